# Optimizing a Trainium2 kernel written in Bass

```python
import math
import jax
import jax.numpy as jnp
from jax import lax
import numpy as np

D_MODEL = 1024
BATCH = 8
SEQ = 4096
DEPTH = 1

HEAD_DIM = 64
DIL_PAIRS = ((128, 1), (512, 4), (2048, 16))
DIL_HEADS_PER_GROUP = 4
DIL_HEADS = DIL_HEADS_PER_GROUP * len(DIL_PAIRS)
DIL_WIDTH = DIL_HEADS * HEAD_DIM
DIL_OUT = DIL_HEADS_PER_GROUP * HEAD_DIM
DIL_QBLOCK = 64
NA_HEADS = 8
NA_WIDTH = NA_HEADS * HEAD_DIM
GRID_W = 64
NA_KH_MAX = 8
NA_KW = 16
NA_QC = 16
NA_KC = NA_QC + NA_KW
N_EXPERTS = 16
EC_CAPACITY_FACTOR = 2
EXPERT_FF = 1024
ROPE_THETA = 10000.0
EPS = 1e-6
SPLIT_SIZES = (DIL_WIDTH, DIL_WIDTH, DIL_WIDTH, NA_WIDTH, NA_WIDTH, NA_WIDTH, D_MODEL, D_MODEL)
IN_COLS = sum(SPLIT_SIZES)
SPLIT_POINTS = tuple(sum(SPLIT_SIZES[:i + 1]) for i in range(len(SPLIT_SIZES) - 1))

kernel_name = 'hybrid_dilated_neighbourhood_ec_block'


def rms_norm(t, g):
    tf = t.astype(jnp.float32)
    y = tf * lax.rsqrt(jnp.mean(tf * tf, axis=-1, keepdims=True) + EPS)
    return (y * g.astype(jnp.float32)).astype(t.dtype)


def rotary(t, pos):
    half = t.shape[-1] // 2
    inv = ROPE_THETA ** (-jnp.arange(half, dtype=jnp.float32) / half)
    ang = pos[:, None] * inv[None, :]
    cos = jnp.cos(ang)[None, :, None, :]
    sin = jnp.sin(ang)[None, :, None, :]
    tf = t.astype(jnp.float32)
    t1, t2 = tf[..., :half], tf[..., half:]
    return jnp.concatenate([t1 * cos - t2 * sin, t2 * cos + t1 * sin], axis=-1).astype(t.dtype)


def dilated_group_attention(q, k, v, window, dilation):
    B, S, H, hd = q.shape
    d = dilation
    R = (window // 2) // d
    L = S // d
    qb = math.gcd(L, DIL_QBLOCK)
    nb = L // qb
    kw = qb + 2 * R

    def to_sub(t):
        return t.reshape(B, L, d, H, hd).transpose(0, 2, 3, 1, 4)

    pad = ((0, 0), (0, 0), (0, 0), (R, R), (0, 0))
    qs = to_sub(q).reshape(B, d, H, nb, qb, hd)
    ks = jnp.pad(to_sub(k), pad)
    vs = jnp.pad(to_sub(v), pad)
    kidx = jnp.arange(nb)[:, None] * qb + jnp.arange(kw)[None, :]
    kb = ks[:, :, :, kidx]
    vb = vs[:, :, :, kidx]
    s = jnp.einsum('brhnqc,brhnkc->brhnqk', qs, kb).astype(jnp.float32) * (hd ** -0.5)
    u_q = jnp.arange(nb)[:, None] * qb + jnp.arange(qb)[None, :]
    u_k = kidx - R
    off = u_k[:, None, :] - u_q[:, :, None]
    valid = (jnp.abs(off) <= R) & (u_k[:, None, :] >= 0) & (u_k[:, None, :] < L)
    s = jnp.where(valid, s, -jnp.inf)
    m = jnp.max(s, axis=-1, keepdims=True)
    p = jnp.exp(s - m)
    den = jnp.sum(p, axis=-1, keepdims=True)
    out = jnp.einsum('brhnqk,brhnkc->brhnqc', p, vb.astype(jnp.float32)) / den
    lse = (m + jnp.log(den))[..., 0]
    out = out.reshape(B, d, H, L, hd).transpose(0, 3, 1, 2, 4).reshape(B, S, H, hd)
    lse = lse.reshape(B, d, H, L).transpose(0, 3, 1, 2).reshape(B, S, H)
    return out.astype(q.dtype), lse


def neighbourhood_attention(q, k, v, rpb):
    B, S, H, hd = q.shape
    rows = S // GRID_W
    kh = min(NA_KH_MAX, rows)
    ncb = GRID_W // NA_QC
    qg = q.reshape(B, rows, GRID_W, H, hd)
    kg = k.reshape(B, rows, GRID_W, H, hd)
    vg = v.reshape(B, rows, GRID_W, H, hd)
    qcol = jnp.arange(GRID_W).reshape(ncb, NA_QC)
    kc_start = jnp.clip(jnp.arange(ncb) * NA_QC - NA_KW // 2, 0, GRID_W - NA_KC)
    kcol = kc_start[:, None] + jnp.arange(NA_KC)[None, :]
    ws = jnp.clip(qcol - NA_KW // 2, 0, GRID_W - NA_KW)
    col_ok = (kcol[:, None, :] >= ws[:, :, None]) & (kcol[:, None, :] < ws[:, :, None] + NA_KW)
    dc_idx = jnp.clip(kcol[:, None, :] - qcol[:, :, None] + NA_KW - 1, 0, 2 * NA_KW - 2)
    scale = hd ** -0.5

    def row_block(r):
        rs = jnp.clip(r - kh // 2, 0, rows - kh)
        q_r = lax.dynamic_index_in_dim(qg, r, axis=1, keepdims=False)
        k_r = lax.dynamic_slice_in_dim(kg, rs, kh, axis=1)
        v_r = lax.dynamic_slice_in_dim(vg, rs, kh, axis=1)
        kb = k_r[:, :, kcol]
        vb = v_r[:, :, kcol]
        qb = q_r.reshape(B, ncb, NA_QC, H, hd)
        s = jnp.einsum('bnqhc,bjnkhc->bhnqjk', qb, kb).astype(jnp.float32) * scale
        dr_idx = rs + jnp.arange(kh) - r + NA_KH_MAX - 1
        bias = rpb[:, dr_idx[:, None, None, None], dc_idx[None]]
        s = s + bias.transpose(0, 2, 3, 1, 4).astype(jnp.float32)[None]
        s = jnp.where(col_ok[:, :, None, :], s, -jnp.inf)
        p = jax.nn.softmax(s.reshape(B, H, ncb, NA_QC, kh * NA_KC), axis=-1)
        p = p.reshape(B, H, ncb, NA_QC, kh, NA_KC)
        o = jnp.einsum('bhnqjk,bjnkhc->bnqhc', p, vb.astype(jnp.float32))
        return o.reshape(B, GRID_W, H, hd).astype(q.dtype)

    out = lax.map(row_block, jnp.arange(rows))
    return out.transpose(1, 0, 2, 3, 4).reshape(B, S, H, hd)


def expert_choice_ffn(h, w_router, w_gate, w_up, w_down):
    B, S, D = h.shape
    cap = (EC_CAPACITY_FACTOR * S) // N_EXPERTS
    logits = jnp.einsum('bsd,de->bse', h, w_router).astype(jnp.float32)
    aff = jax.nn.softmax(logits, axis=-1)
    gates, idx = lax.top_k(aff.transpose(0, 2, 1), cap)
    flat = jnp.arange(B)[:, None, None] * S + idx
    h_flat = h.reshape(B * S, D)
    xin = h_flat[flat]
    a = jnp.einsum('becd,edf->becf', xin, w_gate)
    u = jnp.einsum('becd,edf->becf', xin, w_up)
    y = jnp.einsum('becf,efd->becd', jax.nn.silu(a) * u, w_down)
    y = y * gates[..., None].astype(y.dtype)
    out = jnp.zeros((B * S, D), y.dtype).at[flat.reshape(-1)].add(y.reshape(-1, D))
    return out.reshape(B, S, D)


def setup_inputs(seed: int = 0) -> dict:
    key = jax.random.key(seed)
    ks = jax.random.split(key, 16)
    f32 = jnp.float32

    def nrm(k, shape, scale):
        return jax.random.normal(k, shape, f32) * scale

    def gain(k, shape):
        return 1.0 + 0.05 * jax.random.normal(k, shape, f32)

    L = DEPTH
    return {
        'x': nrm(ks[0], (BATCH, SEQ, D_MODEL), 1.0),
        'norm1_g': gain(ks[1], (L, D_MODEL)),
        'w_in': nrm(ks[2], (L, D_MODEL, IN_COLS), D_MODEL ** -0.5),
        'dil_q_norm_g': gain(ks[3], (L, HEAD_DIM)),
        'dil_k_norm_g': gain(ks[4], (L, HEAD_DIM)),
        'na_q_norm_g': gain(ks[5], (L, HEAD_DIM)),
        'na_k_norm_g': gain(ks[6], (L, HEAD_DIM)),
        'na_rpb': nrm(ks[7], (L, NA_HEADS, 2 * NA_KH_MAX - 1, 2 * NA_KW - 1), 0.1),
        'w_dil_branch': nrm(ks[8], (L, DIL_OUT, D_MODEL), DIL_OUT ** -0.5),
        'w_na_branch': nrm(ks[9], (L, NA_WIDTH, D_MODEL), NA_WIDTH ** -0.5),
        'w_out': nrm(ks[10], (L, D_MODEL, D_MODEL), D_MODEL ** -0.5),
        'norm2_g': gain(ks[11], (L, D_MODEL)),
        'w_router': nrm(ks[12], (L, D_MODEL, N_EXPERTS), D_MODEL ** -0.5),
        'w_gate': nrm(ks[13], (L, N_EXPERTS, D_MODEL, EXPERT_FF), D_MODEL ** -0.5),
        'w_up': nrm(ks[14], (L, N_EXPERTS, D_MODEL, EXPERT_FF), D_MODEL ** -0.5),
        'w_down': nrm(ks[15], (L, N_EXPERTS, EXPERT_FF, D_MODEL), EXPERT_FF ** -0.5),
    }


def reference(x, norm1_g, w_in, dil_q_norm_g, dil_k_norm_g, na_q_norm_g, na_k_norm_g, na_rpb,
              w_dil_branch, w_na_branch, w_out, norm2_g, w_router, w_gate, w_up, w_down):
    B, S, _ = x.shape
    pos = jnp.arange(S, dtype=jnp.float32)
    for l in range(DEPTH):
        h = rms_norm(x, norm1_g[l])
        proj = jnp.einsum('bsd,dc->bsc', h, w_in[l])
        qa, ka, va, qn, kn, vn, ga, gn = jnp.split(proj, SPLIT_POINTS, axis=-1)

        qa = rotary(rms_norm(qa.reshape(B, S, DIL_HEADS, HEAD_DIM), dil_q_norm_g[l]), pos)
        ka = rotary(rms_norm(ka.reshape(B, S, DIL_HEADS, HEAD_DIM), dil_k_norm_g[l]), pos)
        va = va.reshape(B, S, DIL_HEADS, HEAD_DIM)
        outs = []
        lses = []
        for g, (window, dilation) in enumerate(DIL_PAIRS):
            hs = slice(g * DIL_HEADS_PER_GROUP, (g + 1) * DIL_HEADS_PER_GROUP)
            o_g, lse_g = dilated_group_attention(qa[:, :, hs], ka[:, :, hs], va[:, :, hs], window, dilation)
            outs.append(o_g.astype(jnp.float32))
            lses.append(lse_g)
        wts = jax.nn.softmax(jnp.stack(lses, axis=0), axis=0)
        y_a = jnp.sum(wts[..., None] * jnp.stack(outs, axis=0), axis=0).astype(x.dtype)
        y_a = jnp.einsum('bsc,cd->bsd', y_a.reshape(B, S, DIL_OUT), w_dil_branch[l])

        qn = rms_norm(qn.reshape(B, S, NA_HEADS, HEAD_DIM), na_q_norm_g[l])
        kn = rms_norm(kn.reshape(B, S, NA_HEADS, HEAD_DIM), na_k_norm_g[l])
        vn = vn.reshape(B, S, NA_HEADS, HEAD_DIM)
        y_b = neighbourhood_attention(qn, kn, vn, na_rpb[l])
        y_b = jnp.einsum('bsc,cd->bsd', y_b.reshape(B, S, NA_WIDTH), w_na_branch[l])

        merged = jax.nn.sigmoid(ga) * y_a + jax.nn.sigmoid(gn) * y_b
        x = x + jnp.einsum('bsd,de->bse', merged, w_out[l])

        h2 = rms_norm(x, norm2_g[l])
        x = x + expert_choice_ffn(h2, w_router[l], w_gate[l], w_up[l], w_down[l])
    return x
```

```python
import numpy as np
from contextlib import ExitStack
import concourse.bass as bass
import concourse.mybir as mybir
from concourse.bass_utils import run_bass_kernel_spmd

F32 = mybir.dt.float32
BF16 = mybir.dt.bfloat16
I32 = mybir.dt.int32
U32 = mybir.dt.uint32
U16 = mybir.dt.uint16
AF = mybir.ActivationFunctionType
ALU = mybir.AluOpType
AX = mybir.AxisListType

S = 4096
D = 1024
NT = 32
EPS = 1e-6
NE = 16
CAP = 512
N_BISECT = 30

QUEUES = ['pe', 'act', 'dve', 'pool', 'sp']
RING = {'sp': 16, 'act': 8, 'pool': 12}


class Buf:
    def __init__(self, name, ap=None):
        self.name = name
        self.ap = ap
        self.w = []
        self.r = []
        self.pw = []
        self.pr = []
        self.wgroup = None


class Prog:
    def __init__(self, nc, es):
        self.nc = nc
        self.es = es
        self.ops = []
        self.bar = []

    def sbuf(self, name, shape, dtype):
        self.uid = getattr(self, 'uid', 0) + 1
        t = self.es.enter_context(self.nc.sbuf_tensor("sb%d_%s" % (self.uid, name), list(shape), dtype))
        return Buf(name, t)

    def psum(self, name, shape, dtype):
        self.uid = getattr(self, 'uid', 0) + 1
        t = self.es.enter_context(self.nc.psum_tensor("ps%d_%s" % (self.uid, name), list(shape), dtype))
        b = Buf(name, t)
        b.psum = True
        return b

    def dram_buf(self, name):
        return Buf(name, None)

    def views(self, name, buf, n):
        assert not getattr(buf, 'psum', False), "PSUM banks must be tracked as a whole (bank collisions)"
        return [Buf("%s_%d" % (name, i), buf.ap) for i in range(n)]

    def barrier(self):
        last = {}
        deps = set()
        for i, o in enumerate(self.ops):
            if o['dma']:
                deps.add(i)
            else:
                last[o['q']] = i
        deps.update(last.values())
        self.bar = sorted(deps)

    def op(self, q, fn, reads=(), writes=(), dma=False, group=None):
        i = len(self.ops)
        raw = set()
        oth = set()
        excl = [b for b in reads if getattr(b, 'psum', False)]
        if excl:
            reads = [b for b in reads if not getattr(b, 'psum', False)]
            writes = list(writes) + [b for b in excl if b not in writes]
        for b in reads:
            raw.update(b.w)
        for b in writes:
            if group is not None and b.wgroup == group:
                oth.update(b.pw)
                oth.update(b.pr)
                oth.update(b.r)
            else:
                oth.update(b.w)
                oth.update(b.r)
        for b in reads:
            b.r.append(i)
        for b in writes:
            if group is not None and b.wgroup == group:
                b.w.append(i)
            else:
                b.pw = b.w
                b.pr = [x for x in b.r if x != i]
                b.w = [i]
                b.r = []
                b.wgroup = group
        deps = set()
        for d in raw | oth:
            if d == i:
                continue
            o = self.ops[d]
            if o['q'] == q and not o['dma'] and not dma:
                if q != 'pe':
                    deps.add(d)
                continue
            deps.add(d)
        for d in self.bar:
            o = self.ops[d]
            if o['q'] == q and not o['dma'] and not dma:
                continue
            deps.add(d)
        self.ops.append(dict(q=q, fn=fn, deps=deps, dma=dma))
        return i

    def dma(self, q, out, in_, reads=(), writes=(), group=None, **kw):
        return self.op(q, lambda e: e.dma_start(out=out, in_=in_, **kw), reads, writes, dma=True, group=group)

    def mm(self, out, lhsT, rhs, start, stop, reads, writes, group=None):
        return self.op('pe', lambda e: e.matmul(out, lhsT=lhsT, rhs=rhs, start=start, stop=stop), reads, writes,
                       group=group)

    def tr(self, out, in_, ident, reads, writes, group=None):
        return self.op('pe', lambda e: e.transpose(out, in_, ident), reads, writes, group=group)

    def act(self, out, in_, func, reads, writes, group=None, **kw):
        return self.op('act', lambda e: e.activation(out=out, in_=in_, func=func, **kw), reads, writes, group=group)

    def tt(self, q, out, in0, in1, op, reads, writes, group=None):
        return self.op(q, lambda e: e.tensor_tensor(out=out, in0=in0, in1=in1, op=op), reads, writes, group=group)

    def ts(self, q, out, in0, s1, s2, op0, op1, reads, writes, group=None, **kw):
        if op1 is None:
            return self.op(q, lambda e: e.tensor_scalar(out=out, in0=in0, scalar1=s1, scalar2=None, op0=op0, **kw),
                           reads, writes, group=group)
        return self.op(q, lambda e: e.tensor_scalar(out=out, in0=in0, scalar1=s1, scalar2=s2, op0=op0, op1=op1, **kw),
                       reads, writes, group=group)

    def stt(self, out, in0, scalar, in1, op0, op1, reads, writes):
        return self.op('dve', lambda e: e.scalar_tensor_tensor(out=out, in0=in0, scalar=scalar, in1=in1,
                                                                op0=op0, op1=op1), reads, writes)

    def cp(self, q, out, in_, reads, writes, group=None):
        if q == 'act':
            return self.op(q, lambda e: e.copy(out=out, in_=in_), reads, writes, group=group)
        return self.op(q, lambda e: e.tensor_copy(out=out, in_=in_), reads, writes, group=group)

    def emit(self):
        nc = self.nc
        es = self.es
        ops = self.ops
        flagged = [False] * len(ops)
        for o in ops:
            for d in o['deps']:
                flagged[d] = True
        prog_sem = {q: es.enter_context(nc.semaphore("pg_" + q)) for q in ['pe', 'act', 'dve', 'pool']}
        ring_sem = {q: [es.enter_context(nc.semaphore("rg_%s_%d" % (q, k))) for k in range(n)]
                    for q, n in RING.items()}
        cnt = {q: 0 for q in QUEUES}
        dcnt = {q: 0 for q in QUEUES}
        done = [None] * len(ops)
        prev_ring = [None] * len(ops)
        for i, o in enumerate(ops):
            q = o['q']
            if o['dma']:
                k = dcnt[q]
                dcnt[q] += 1
                R = RING[q]
                sem = ring_sem[q][k % R]
                val = 16 * (k // R + 1)
                done[i] = (sem, val)
                if val > 16:
                    prev_ring[i] = (sem, val - 16)
            elif flagged[i]:
                cnt[q] += 1
                done[i] = (prog_sem[q], cnt[q])
        final = {}
        for q, sems in ring_sem.items():
            for k, s in enumerate(sems):
                n = (dcnt[q] - k + RING[q] - 1) // RING[q]
                if n > 0:
                    final[id(s)] = (s, 16 * n)
        self.stats = dict(cnt=cnt, dcnt=dcnt, nops=len(ops))

        def run_queue(q, eng):
            waited = {}
            for i, o in enumerate(ops):
                if o['q'] != q:
                    continue
                need = {}
                for d in o['deps']:
                    s, v = done[d]
                    if need.get(id(s), (None, 0))[1] < v:
                        need[id(s)] = (s, v)
                if prev_ring[i] is not None:
                    s, v = prev_ring[i]
                    if need.get(id(s), (None, 0))[1] < v:
                        need[id(s)] = (s, v)
                for k, (s, v) in need.items():
                    if waited.get(k, 0) >= v:
                        continue
                    eng.wait_ge(s, v)
                    waited[k] = v
                ins = o['fn'](eng)
                if done[i] is not None:
                    ins.then_inc(done[i][0], 16 if o['dma'] else 1)
            if q == 'sp':
                for k, (s, v) in final.items():
                    if waited.get(k, 0) < v:
                        eng.wait_ge(s, v)

        with nc.Block() as block:
            @block.tensor
            def _(e):
                run_queue('pe', e)

            @block.scalar
            def _(e):
                run_queue('act', e)

            @block.vector
            def _(e):
                run_queue('dve', e)

            @block.gpsimd
            def _(e):
                run_queue('pool', e)

            @block.sync
            def _(e):
                run_queue('sp', e)


NA_VARIANTS = [(2, [0, 1, 2, 3, 4]), (0, [0, 1, 2, 3]), (1, [0, 1, 2, 3]), (30, [28, 29, 30, 31]),
               (31, [28, 29, 30, 31])]
NA_TILE0 = [0, 5, 9, 13, 17]
C_IDENT, C_BONES, C_RMAT, C_ONES, C_TRIU = 0, 128, 256, 384, 512
C_DMASK = 640
C_IOTA = C_DMASK + 2048
C_RESET = C_IOTA + 512
C_TOK = C_RESET + 512
C_TOTAL = C_TOK + 64


def na_block_info(b):
    if 2 <= b <= 29:
        return list(range(b - 2, b + 3)), NA_TILE0[0]
    v = {0: 1, 1: 2, 30: 3, 31: 4}[b]
    return NA_VARIANTS[v][1], NA_TILE0[v]


def host_consts():
    c = np.zeros((128, C_TOTAL), np.float32)
    p = np.arange(128)
    c[:, C_IDENT:C_IDENT + 128] = np.eye(128)
    c[:, C_BONES:C_BONES + 128] = ((p[:, None] // 64) == (p[None, :] // 64)) / 64.0
    R = np.zeros((128, 128), np.float32)
    for i in range(128):
        if i % 64 < 32:
            R[i + 32, i] = -1.0
        else:
            R[i - 32, i] = 1.0
    c[:, C_RMAT:C_RMAT + 128] = R
    c[:, C_ONES:C_ONES + 128] = 1.0
    c[:, C_TRIU:C_TRIU + 128] = (p[:, None] < p[None, :])
    pk = p[:, None]
    pq = p[None, :]
    mA = (pk >= pq)
    mB = (pk <= pq)
    first = np.concatenate([mA & (pk >= 64), mB], axis=1)
    mid = np.concatenate([mA, mB], axis=1)
    last = np.concatenate([mA, mB & (pk < 64)], axis=1)
    c[:, C_DMASK:C_DMASK + 2048] = (np.concatenate([first, mid, mid, mid, mid, last, first, last], axis=1) - 1.0) * 30000.0
    c[:, C_IOTA:C_IOTA + 512] = np.arange(512)[None, :]
    rs = np.ones(512, np.float32)
    rs[0::32] = 0.0
    c[:, C_RESET:C_RESET + 512] = rs[None, :]
    tok = np.arange(32)[None, :] * 128 + p[:, None]
    c[:, C_TOK:C_TOK + 32] = tok // 64
    c[:, C_TOK + 32:C_TOK + 64] = tok % 64
    inv = (10000.0 ** (-np.arange(32, dtype=np.float32) / 32)).astype(np.float32)
    pos = np.arange(S, dtype=np.float32)
    ang = (pos[:, None] * inv[None, :]).astype(np.float32)
    cos = np.cos(ang).astype(np.float32).T
    sin = np.sin(ang).astype(np.float32).T
    cosT = np.tile(cos, (4, 1))
    sinT = np.tile(sin, (4, 1))
    mask = np.zeros((21, 128, 128), np.float32)
    dri = np.zeros((21, 128, 128), np.int64)
    dci = np.zeros((21, 128, 128), np.int64)
    t = 0
    for b, kbs in NA_VARIANTS:
        for kb in kbs:
            kr = 2 * kb + p // 64
            kc = p % 64
            r = 2 * b + p // 64
            qc = p % 64
            rs_ = np.clip(r - 4, 0, 56)
            ws = np.clip(qc - 8, 0, 48)
            valid = ((kr[:, None] >= rs_[None, :]) & (kr[:, None] < rs_[None, :] + 8) &
                     (kc[:, None] >= ws[None, :]) & (kc[:, None] < ws[None, :] + 16))
            mask[t] = valid
            dri[t] = np.clip(kr[:, None] - r[None, :] + 7, 0, 14)
            dci[t] = np.clip(kc[:, None] - qc[None, :] + 15, 0, 30)
            t += 1
    namask = np.ascontiguousarray(mask.transpose(1, 0, 2))
    return c, np.ascontiguousarray(cosT), np.ascontiguousarray(sinT), namask, dri, dci


W_SEGS = [('qa', 0, 768), ('ka', 768, 768), ('va', 1536, 768), ('qn', 2304, 512), ('kn', 2816, 512),
          ('vn', 3328, 512), ('ga', 3840, 1024), ('gn', 4864, 1024)]
QK_ROW = {'qa': 0, 'ka': 768, 'qn': 1536, 'kn': 2048}
V_COL = {'va': 0, 'vn': 768}
SG_ROW = {'ga': 0, 'gn': 1024}
GV_COL = {'qa': 0, 'ka': 1, 'qn': 2, 'kn': 3}
CW = 256


def emit_phase_F(P, ring, cst, ones_f, triu_f, ident_f, lg_all, idx_all, gate_all, banks, es_persist, es_tmp,
                 idx_v, gate_v):
    P.es = es_persist
    vals = P.sbuf("vals", [128, NE, NT, 5], BF16)
    shb = P.sbuf("shb", [128, 512], BF16)
    slb = P.sbuf("slb", [128, 512], BF16)
    iob = P.sbuf("iob", [128, 128], BF16)
    He = ring("He", 2, [128, NT, 128], BF16)
    Le = ring("Le", 2, [128, NT, 4], BF16)
    LVe = ring("LVe", 2, [128, NT, 4, 5], BF16)
    idf = ring("idf", 2, [128, 4], F32)
    csb = ring("csb", 2, [128, 20], F32)
    P.es = es_tmp
    totp, ppp, ccp, cpsb = banks
    mx = P.sbuf("mx", [128, NT], F32)
    sh = P.sbuf("sh", [128, NT, NE], F32)
    sm = P.sbuf("sm", [128, NT], F32)
    affT = P.sbuf("affT", [128, NE, NT], F32)
    lo = P.sbuf("lo", [128, NE], F32)
    hi = P.sbuf("hi", [128, NE], F32)
    mid = P.sbuf("mid", [128, NE], F32)
    cmpb = P.sbuf("cmpb", [128, NE, NT], F32)
    cntp = P.sbuf("cntp", [128, NE], F32)
    mge = P.sbuf("mge", [128, NE], U32)
    mlt = P.sbuf("mlt", [128, NE], U32)
    P.op('dve', lambda e: e.tensor_reduce(out=mx.ap[:], in_=lg_all.ap[:], axis=AX.X, op=ALU.max), [lg_all], [mx])
    P.tt('dve', sh.ap[:], lg_all.ap[:], mx.ap[:, :, None].to_broadcast([128, NT, NE]), ALU.subtract, [lg_all, mx], [sh])
    P.act(sh.ap[:], sh.ap[:], AF.Exp, [sh], [sh])
    P.op('dve', lambda e: e.tensor_reduce(out=sm.ap[:], in_=sh.ap[:], axis=AX.X, op=ALU.add), [sh], [sm])
    P.op('dve', lambda e: e.reciprocal(out=sm.ap[:], in_=sm.ap[:]), [sm], [sm])
    P.tt('dve', affT.ap[:].rearrange("p e i -> p i e"), sh.ap[:], sm.ap[:, :, None].to_broadcast([128, NT, NE]),
         ALU.mult, [sh, sm], [affT])
    P.op('dve', lambda e: e.memset(lo.ap[:], 0.0), [], [lo])
    P.op('dve', lambda e: e.memset(hi.ap[:], 1.0), [], [hi])
    for itb in range(N_BISECT):
        P.tt('dve', mid.ap[:], lo.ap[:], hi.ap[:], ALU.add, [lo, hi], [mid])
        P.ts('dve', mid.ap[:], mid.ap[:], 0.5, None, ALU.mult, None, [mid], [mid])
        P.tt('dve', cmpb.ap[:], affT.ap[:], mid.ap[:, :, None].to_broadcast([128, NE, NT]), ALU.is_ge, [affT, mid], [cmpb])
        P.op('dve', lambda e: e.tensor_reduce(out=cntp.ap[:], in_=cmpb.ap[:], axis=AX.X, op=ALU.add), [cmpb], [cntp])
        P.mm(totp.ap[:, 0:NE], ones_f, cntp.ap[:], True, True, [cntp, cst], [totp])
        P.ts('dve', mge.ap[:], totp.ap[:, 0:NE], float(CAP), None, ALU.is_ge, None, [totp], [mge])
        P.ts('dve', mlt.ap[:], totp.ap[:, 0:NE], float(CAP), None, ALU.is_lt, None, [totp], [mlt])
        P.op('dve', lambda e: e.copy_predicated(out=lo.ap[:], mask=mge.ap[:], data=mid.ap[:]), [mge, mid, lo], [lo])
        P.op('dve', lambda e: e.copy_predicated(out=hi.ap[:], mask=mlt.ap[:], data=mid.ap[:]), [mlt, mid, hi], [hi])
    sel = P.sbuf("sel", [128, 512], F32)
    cc = P.sbuf("cc", [128, 512], F32)
    csum = P.sbuf("csum", [128, 512], F32)
    slot = P.sbuf("slot", [128, 512], F32)
    ltb = P.sbuf("ltb", [128, 512], F32)
    slotm = P.sbuf("slotm", [128, 512], F32)
    affF = affT.ap[:].rearrange("p e i -> p (e i)")
    P.tt('dve', sel.ap[:].rearrange("p (e i) -> p e i", i=NT), affT.ap[:],
         lo.ap[:, :, None].to_broadcast([128, NE, NT]), ALU.is_ge, [affT, lo], [sel])
    P.mm(ppp.ap[:], triu_f, sel.ap[:], True, True, [sel, cst], [ppp])
    P.mm(ccp.ap[:], ones_f, sel.ap[:], True, True, [sel, cst], [ccp])
    P.cp('act', cc.ap[:], ccp.ap[:], [ccp], [cc])
    P.op('dve', lambda e: e.tensor_tensor_scan(out=csum.ap[:], data0=cst.ap[:, C_RESET:C_RESET + 512], data1=cc.ap[:],
                                               initial=0.0, op0=ALU.mult, op1=ALU.add), [cc, cst], [csum])
    P.tt('dve', slot.ap[:], csum.ap[:], cc.ap[:], ALU.subtract, [csum, cc], [slot])
    P.tt('dve', slot.ap[:], ppp.ap[:], slot.ap[:], ALU.add, [ppp, slot], [slot])
    P.ts('dve', ltb.ap[:], slot.ap[:], float(CAP), None, ALU.is_lt, None, [slot], [ltb])
    P.tt('dve', ltb.ap[:], ltb.ap[:], sel.ap[:], ALU.mult, [ltb, sel], [ltb])
    P.stt(slotm.ap[:], slot.ap[:], 1.0, ltb.ap[:], ALU.add, ALU.mult, [slot, ltb], [slotm])
    P.ts('dve', slotm.ap[:], slotm.ap[:], -1.0, None, ALU.add, None, [slotm], [slotm])
    a1 = P.sbuf("a1", [128, 512], BF16)
    r1 = P.sbuf("r1", [128, 512], F32)
    r2 = P.sbuf("r2", [128, 512], F32)
    P.cp('dve', a1.ap[:], affF, [affT], [a1])
    P.tt('dve', r1.ap[:], affF, a1.ap[:], ALU.subtract, [affT, a1], [r1])
    P.cp('dve', vals.ap[:, :, :, 2], a1.ap[:].rearrange("p (e i) -> p e i", i=NT), [a1], [vals], group='vals')
    P.cp('dve', a1.ap[:], r1.ap[:], [r1], [a1])
    P.tt('dve', r2.ap[:], r1.ap[:], a1.ap[:], ALU.subtract, [r1, a1], [r2])
    P.cp('dve', vals.ap[:, :, :, 3], a1.ap[:].rearrange("p (e i) -> p e i", i=NT), [a1], [vals], group='vals')
    P.cp('dve', vals.ap[:, :, :, 4], r2.ap[:].rearrange("p (e i) -> p e i", i=NT), [r2], [vals], group='vals')
    P.cp('dve', vals.ap[:, :, :, 0], cst.ap[:, C_TOK:C_TOK + 32][:, None, :].to_broadcast([128, NE, NT]), [cst], [vals],
         group='vals')
    P.cp('dve', vals.ap[:, :, :, 1], cst.ap[:, C_TOK + 32:C_TOK + 64][:, None, :].to_broadcast([128, NE, NT]), [cst],
         [vals], group='vals')
    sli = P.sbuf("sli", [128, 512], I32)
    shi = P.sbuf("shi", [128, 512], I32)
    P.cp('dve', iob.ap[:], cst.ap[:, C_IOTA:C_IOTA + 128], [cst], [iob])
    P.cp('dve', sli.ap[:], slotm.ap[:], [slotm], [sli])
    P.ts('dve', shi.ap[:], sli.ap[:], 2, None, ALU.arith_shift_right, None, [sli], [shi])
    P.cp('dve', shb.ap[:], shi.ap[:], [shi], [shb])
    P.ts('dve', shi.ap[:], sli.ap[:], 3, None, ALU.bitwise_and, None, [sli, shb], [shi])
    P.cp('dve', slb.ap[:], shi.ap[:], [shi], [slb])

    def compact(e_):
        k_ = e_ % 2
        esl = slice(e_ * NT, (e_ + 1) * NT)
        P.tt('dve', He[k_].ap[:], iob.ap[:, None, :].to_broadcast([128, NT, 128]),
             shb.ap[:, esl, None].to_broadcast([128, NT, 128]), ALU.is_equal, [iob, shb], [He[k_]])
        P.tt('dve', Le[k_].ap[:], iob.ap[:, None, 0:4].to_broadcast([128, NT, 4]),
             slb.ap[:, esl, None].to_broadcast([128, NT, 4]), ALU.is_equal, [iob, slb], [Le[k_]])
        P.tt('dve', LVe[k_].ap[:], Le[k_].ap[:, :, :, None].to_broadcast([128, NT, 4, 5]),
             vals.ap[:, e_, :, None, :].to_broadcast([128, NT, 4, 5]), ALU.mult, [Le[k_], vals], [LVe[k_]])
        for i in range(NT):
            P.mm(cpsb.ap[:, 0:20], He[k_].ap[:, i, :], LVe[k_].ap[:, i].rearrange("p a b -> p (a b)"), i == 0, i == NT - 1,
                 [He[k_], LVe[k_]], [cpsb])
        cs_ = csb[k_]
        P.cp('act', cs_.ap[:], cpsb.ap[:, 0:20], [cpsb], [cs_])
        c3 = cs_.ap[:].rearrange("p (a b) -> p a b", b=5)
        f_ = idf[k_]
        P.stt(f_.ap[:], c3[:, :, 0], 64.0, c3[:, :, 1], ALU.mult, ALU.add, [cs_], [f_])
        P.cp('dve', idx_all.ap[:, e_, :], f_.ap[:], [f_], [idx_v[e_]])
        P.tt('dve', f_.ap[:], c3[:, :, 2], c3[:, :, 3], ALU.add, [cs_], [f_])
        P.tt('dve', gate_all.ap[:, e_, :], c3[:, :, 4], f_.ap[:], ALU.add, [cs_, f_], [gate_v[e_]])

    return dict(compact=compact)


def build_program(debug=False, stop_after=None):
    nc = bass.Bass("TRN2", target_bir_lowering=False)

    def din(name, shape, dt=F32):
        return nc.dram_tensor(name, list(shape), dt, kind="ExternalInput").ap()

    skind = "ExternalOutput" if debug else "Internal"

    def dscr(name, shape, dt):
        return nc.dram_tensor(name, list(shape), dt, kind=skind).ap()

    x_d = din("x", [S, D])
    win_d = din("w_in", [D, 5888])
    g1_d = din("g1", [128, 8])
    gv_d = din("gv", [128, 4])
    g2_d = din("g2rep", [128, D])
    cst_d = din("consts", [128, C_TOTAL])
    cos_d = din("cosT", [128, S])
    sin_d = din("sinT", [128, S])
    namask_d = din("namask", [128, 21, 128])
    nabias_d = din("nabias", [8, 128, 21, 128])
    wa_d = din("w_dil_branch", [256, D])
    wb_d = din("w_na_branch", [512, D])
    wo_d = din("w_out", [D, D])
    wr_d = din("w_router", [D, NE])
    need_moe = stop_after in (None, 'G')
    if need_moe:
        wg_d = din("w_gate", [NE, D, D])
        wu_d = din("w_up", [NE, D, D])
        wd_d = din("w_down", [NE, D, D])
    out_d = nc.dram_tensor("out", [S, D], F32, kind="ExternalOutput").ap()

    qkT_d = dscr("qkT", [2560, S], BF16)
    sgT_d = dscr("sgT", [2048, S], BF16)
    v_d = dscr("vtok", [S, 1300], BF16)
    dout_d = dscr("dout", [3, S, 260], F32)
    h2_d = dscr("h2", [S, D], BF16)

    with ExitStack() as es:
        P = Prog(nc, es)
        qkT_b = P.dram_buf("qkT")
        sgT_b = P.dram_buf("sgT")
        v_b = P.dram_buf("v")
        dout_b = P.dram_buf("dout")
        h2_b = P.dram_buf("h2")
        out_b = P.dram_buf("out")

        cst = P.sbuf("cst", [128, C_TOTAL], F32)
        cstb = P.sbuf("cstb", [128, C_IOTA], BF16)
        g1 = P.sbuf("g1", [128, 8], F32)
        gv = P.sbuf("gv", [128, 4], F32)
        idx_all = P.sbuf("idx_all", [128, NE, 4], I32)
        gate_all = P.sbuf("gate_all", [128, NE, 4], F32)
        lg_all = P.sbuf("lg_all", [128, NT, NE], F32)
        idx_v = P.views("idx_v", idx_all, NE)
        gate_v = P.views("gate_v", gate_all, NE)
        P.dma('sp', cst.ap[:], cst_d, writes=[cst])
        P.dma('sp', g1.ap[:], g1_d, writes=[g1])
        P.dma('sp', gv.ap[:], gv_d, writes=[gv])
        P.cp('dve', cstb.ap[:], cst.ap[:, 0:C_IOTA], [cst], [cstb])
        ident_b = cstb.ap[:, C_IDENT:C_IDENT + 128]
        bones_b = cstb.ap[:, C_BONES:C_BONES + 128]
        rmat_b = cstb.ap[:, C_RMAT:C_RMAT + 128]
        ident_f = cst.ap[:, C_IDENT:C_IDENT + 128]
        ones_f = cst.ap[:, C_ONES:C_ONES + 128]
        triu_f = cst.ap[:, C_TRIU:C_TRIU + 128]

        def ring(name, n, shape, dt, psum=False):
            return [(P.psum if psum else P.sbuf)("%s%d" % (name, i), shape, dt) for i in range(n)]

        def pipeline(items, stages, skews):
            N = len(items)
            for n in range(N + max(skews)):
                for f, sk in zip(stages, skews):
                    i = n - sk
                    if 0 <= i < N:
                        f(items[i])

        with ExitStack() as es2:
            P.es = es2
            hT = P.sbuf("hT", [128, 8, S], BF16)
            hTv = P.views("hTv", hT, NT)
            cosT = P.sbuf("cosT", [128, S], F32)
            sinT = P.sbuf("sinT", [128, S], F32)
            P.dma('act', cosT.ap[:], cos_d, writes=[cosT])
            P.dma('act', sinT.ap[:], sin_d, writes=[sinT])
            wbf = ring("wbf", 4, [128, 8, CW], BF16)
            chunks = []
            for (nm, c0, wd) in W_SEGS:
                for o in range(0, wd, CW):
                    chunks.append((nm, c0 + o, o))

            def load_wchunk(ci):
                if ci >= len(chunks):
                    return
                nm, col0, off = chunks[ci]
                wb_ = wbf[ci % 4]
                P.dma('pool', wb_.ap[:], win_d[:, col0:col0 + CW].rearrange("(c p) n -> p c n", p=128), [], [wb_])

            load_wchunk(0)
            load_wchunk(1)
            load_wchunk(2)
            with ExitStack() as esA:
                P.es = esA
                xt = ring("xt", 4, [128, D], F32)
                junk = P.sbuf("junk", [128, D], BF16)
                ss = ring("ss", 3, [128, 1], F32)
                rs = ring("rs", 3, [128, 1], F32)
                rr = ring("rr", 3, [128, 1], F32)
                xn = ring("xn", 3, [128, D], BF16)
                tp = ring("tp", 2, [128, 8, 128], BF16, psum=True)

                def stA_load(t):
                    P.dma('sp', xt[t % 4].ap[:], x_d[t * 128:(t + 1) * 128, :], writes=[xt[t % 4]])

                def stA_norm(t):
                    k = t % 3
                    x_ = xt[t % 4]
                    P.act(junk.ap[:], x_.ap[:], AF.Square, [x_], [junk, ss[k]], accum_out=ss[k].ap[:])
                    P.act(rs[k].ap[:], ss[k].ap[:], AF.Sqrt, [ss[k]], [rs[k]], scale=1.0 / D, bias=EPS)
                    P.op('dve', lambda e, o=rr[k].ap[:], i=rs[k].ap[:]: e.reciprocal(out=o, in_=i), [rs[k]], [rr[k]])
                    P.ts('dve', xn[k].ap[:], x_.ap[:], rr[k].ap[:, 0:1], None, ALU.mult, None, [x_, rr[k]], [xn[k]])

                def stA_tr(t):
                    k = t % 3
                    for c in range(8):
                        P.tr(tp[t % 2].ap[:, c, :], xn[k].ap[:, c * 128:(c + 1) * 128], ident_b, [xn[k], cstb], [tp[t % 2]])

                def stA_evac(t):
                    P.tt('dve', hT.ap[:, :, t * 128:(t + 1) * 128], tp[t % 2].ap[:],
                         g1.ap[:, :, None].to_broadcast([128, 8, 128]), ALU.mult, [tp[t % 2], g1], [hTv[t]])

                pipeline(list(range(NT)), [stA_load, stA_norm, stA_tr, stA_evac], [0, 1, 2, 3])
            P.es = es2
            P.barrier()

            psA = ring("psA", 3, [128, 512], F32, psum=True)
            msps = ring("msps", 2, [128, 512], F32, psum=True)
            rotps = ring("rotps", 2, [128, 512], F32, psum=True)
            sqb = ring("sqb", 3, [128, 512], BF16)
            lnb = ring("lnb", 2, [128, 512], F32)
            rstd = ring("rstd", 2, [128, 512], F32)
            qnb = ring("qnb", 3, [128, 512], BF16)
            t1 = ring("t1", 2, [128, 512], F32)
            t2 = ring("t2", 2, [128, 512], F32)
            stg = ring("stg", 4, [128, 512], BF16)
            vst = ring("vst", 2, [128, 4, CW // 64, 65], BF16)
            for v_ in vst:
                P.op('pool', lambda e, o=v_.ap[:]: e.memset(o, 1.0), [], [v_])
            itemsB = []
            for ci, (nm, col0, off) in enumerate(chunks):
                if nm in ('va', 'vn'):
                    for t in range(NT):
                        itemsB.append(dict(n=len(itemsB), ci=ci, nm=nm, off=off, kind='v', t=t, first=(t == 0)))
                else:
                    for j in range(CW // 128):
                        for tb in range(8):
                            itemsB.append(dict(n=len(itemsB), ci=ci, nm=nm, off=off,
                                               kind=('g' if nm in ('ga', 'gn') else ('n' if nm in ('qn', 'kn') else 'd')),
                                               j=j, tb=tb, first=(j == 0 and tb == 0)))

            def stB_main(it_):
                n, ci, nm, off = it_['n'], it_['ci'], it_['nm'], it_['off']
                if it_['first']:
                    load_wchunk(ci + 3)
                wb_ = wbf[ci % 4]
                ps = psA[n % 3]
                if it_['kind'] == 'v':
                    t = it_['t']
                    a = t % 4
                    vc = V_COL[nm] + off
                    for c in range(8):
                        P.mm(ps.ap[:, 0:CW], hT.ap[:, c, t * 128:(t + 1) * 128], wb_.ap[:, c, :], c == 0, c == 7,
                             [hTv[t], wb_], [ps])
                    vs = vst[(t // 4) % 2]
                    P.cp('act' if t % 2 == 0 else 'dve', vs.ap[:, a, :, 0:64], ps.ap[:, 0:CW].rearrange("p (h c) -> p h c", c=64),
                         [ps], [vs], group=('vs', ci, t // 4))
                    if a == 3:
                        t0 = t - 3
                        vc65 = (vc // 64) * 65
                        wd65 = (CW // 64) * 65
                        P.dma('sp', v_d[t0 * 128:(t0 + 4) * 128, vc65:vc65 + wd65].rearrange("(a p) c -> p a c", p=128),
                              vs.ap[:].rearrange("p a h c -> p a (h c)"), [vs], [v_b], group='fill')
                    return
                j, tb = it_['j'], it_['tb']
                tsl = slice(tb * 512, (tb + 1) * 512)
                hr = [hTv[4 * tb + a] for a in range(4)]
                for c in range(8):
                    P.mm(ps.ap[:], wb_.ap[:, c, j * 128:(j + 1) * 128], hT.ap[:, c, tsl], c == 0, c == 7, hr + [wb_], [ps])
                if it_['kind'] == 'g':
                    st = stg[n % 4]
                    P.act(st.ap[:], ps.ap[:], AF.Sigmoid, [ps], [st])
                    r0 = SG_ROW[nm] + off + j * 128
                    P.dma('sp', sgT_d[r0:r0 + 128, tsl], st.ap[:], [st], [sgT_b], group='fill')
                else:
                    P.act(sqb[n % 3].ap[:], ps.ap[:], AF.Square, [ps], [sqb[n % 3]])

            def stB_norm(it_):
                if it_['kind'] not in ('n', 'd'):
                    return
                n, nm, off, j, tb = it_['n'], it_['nm'], it_['off'], it_['j'], it_['tb']
                tsl = slice(tb * 512, (tb + 1) * 512)
                ps = psA[n % 3]
                kk = n % 2
                gcol = gv.ap[:, GV_COL[nm]:GV_COL[nm] + 1]
                P.mm(msps[kk].ap[:], bones_b, sqb[n % 3].ap[:], True, True, [sqb[n % 3], cstb], [msps[kk]])
                P.act(lnb[kk].ap[:], msps[kk].ap[:], AF.Ln, [msps[kk]], [lnb[kk]], bias=EPS)
                P.act(rstd[kk].ap[:], lnb[kk].ap[:], AF.Exp, [lnb[kk]], [rstd[kk]], scale=-0.5)
                if it_['kind'] == 'n':
                    st = stg[n % 4]
                    r0 = QK_ROW[nm] + off + j * 128
                    P.stt(st.ap[:], ps.ap[:], gcol, rstd[kk].ap[:], ALU.mult, ALU.mult, [ps, gv, rstd[kk]], [st])
                    P.dma('sp', qkT_d[r0:r0 + 128, tsl], st.ap[:], [st], [qkT_b], group='fill')
                else:
                    P.stt(qnb[n % 3].ap[:], ps.ap[:], gcol, rstd[kk].ap[:], ALU.mult, ALU.mult, [ps, gv, rstd[kk]], [qnb[n % 3]])

            def stB_rot(it_):
                if it_['kind'] != 'd':
                    return
                n, nm, off, j, tb = it_['n'], it_['nm'], it_['off'], it_['j'], it_['tb']
                tsl = slice(tb * 512, (tb + 1) * 512)
                kk = n % 2
                q_ = qnb[n % 3]
                st = stg[n % 4]
                r0 = QK_ROW[nm] + off + j * 128
                P.mm(rotps[kk].ap[:], rmat_b, q_.ap[:], True, True, [q_, cstb], [rotps[kk]])
                P.tt('pool', t1[kk].ap[:], q_.ap[:], cosT.ap[:, tsl], ALU.mult, [q_, cosT], [t1[kk]])
                P.tt('dve', t2[kk].ap[:], rotps[kk].ap[:], sinT.ap[:, tsl], ALU.mult, [rotps[kk], sinT], [t2[kk]])
                P.tt('dve', st.ap[:], t1[kk].ap[:], t2[kk].ap[:], ALU.add, [t1[kk], t2[kk]], [st])
                P.dma('sp', qkT_d[r0:r0 + 128, tsl], st.ap[:], [st], [qkT_b], group='fill')

            pipeline(itemsB, [stB_main, stB_norm, stB_rot], [0, 1, 2])
        P.es = es
        P.barrier()
        if stop_after == 'B':
            P.emit()
            return nc, P

        def dmask(v):
            return cstb.ap[:, C_DMASK + v * 256:C_DMASK + (v + 1) * 256]

        with ExitStack() as es3:
            P.es = es3
            q2n = ring("q2n", 2, [128, S], BF16)
            k2n = ring("k2n", 2, [128, S], BF16)
            qP = ring("qP", 2, [128, S], BF16)
            kP = ring("kP", 2, [128, 6144], BF16)
            Vg = ring("Vg", 2, [128, 48, 4, 65], BF16)
            ost = ring("ost", 2, [128, 32, 2, 65], F32)
            pt = ring("pt", 5, [128, 512], BF16)
            sps = ring("sps", 4, [128, 512], F32, psum=True)
            po = ring("po", 3, [128, 512], F32, psum=True)
            for v_ in Vg:
                P.op('pool', lambda e, o=v_.ap[:]: e.memset(o, 1.0), [], [v_])
            for k_ in kP:
                P.op('pool', lambda e, o=k_.ap[:]: e.memset(o, 0.0), [], [k_])
            GD = [1, 4, 16]

            def dmask2(v):
                return cstb.ap[:, C_DMASK + v * 512:C_DMASK + (v + 1) * 512]

            def load_V(g):
                d = GD[g]
                L = S // d
                nblk = L // 128
                vg = Vg[g % 2]
                cs = slice(g * 260, (g + 1) * 260)
                for r in range(d):
                    v_r = v_d.rearrange("(u r) c -> r u c", r=d)[r]
                    b0 = r * (nblk + 1)
                    P.dma('sp', vg.ap[:, b0 + 1:b0 + nblk].rearrange("p k h c -> p k (h c)"),
                          v_r[64:64 + 128 * (nblk - 1), cs].rearrange("(k p) c -> p k c", p=128),
                          [v_b], [vg], group=('vg', g))
                    P.dma('sp', vg.ap[64:128, b0].rearrange("p h c -> p (h c)"), v_r[0:64, cs], [v_b], [vg], group=('vg', g))
                    P.dma('sp', vg.ap[0:64, b0 + nblk].rearrange("p h c -> p (h c)"), v_r[L - 64:L, cs], [v_b], [vg],
                          group=('vg', g))

            pairs = [(g, hp) for g in range(3) for hp in range(2)]

            def load_pair(pi):
                g, hp = pairs[pi]
                k_ = pi % 2
                row = (4 * g + 2 * hp) * 64
                if g == 0:
                    P.dma('sp', qP[k_].ap[:], qkT_d[row:row + 128, :], [qkT_b], [qP[k_]])
                    P.dma('sp', kP[k_].ap[:, 64:64 + S], qkT_d[768 + row:768 + row + 128, :], [qkT_b], [kP[k_]])
                    return
                P.dma('sp', q2n[k_].ap[:], qkT_d[row:row + 128, :], [qkT_b], [q2n[k_]])
                P.dma('sp', k2n[k_].ap[:], qkT_d[768 + row:768 + row + 128, :], [qkT_b], [k2n[k_]])

            def permute_pair(pi):
                g, hp = pairs[pi]
                if g == 0:
                    return
                d = GD[g]
                L = S // d
                k_ = pi % 2
                if pi >= 2 and GD[pairs[pi - 2][0]] != d:
                    P.op('pool', lambda e, o=kP[k_].ap[:]: e.memset(o, 0.0), [], [kP[k_]])
                P.cp('pool', qP[k_].ap[:].rearrange("p (r u) -> p r u", r=d),
                     q2n[k_].ap[:].rearrange("p (u r) -> p r u", r=d), [q2n[k_]], [qP[k_]])
                P.cp('act', kP[k_].ap[:, 0:d * (L + 128)].rearrange("p (r u) -> p r u", r=d)[:, :, 64:64 + L],
                     k2n[k_].ap[:].rearrange("p (u r) -> p r u", r=d), [k2n[k_]], [kP[k_]])

            def store_pair(pi):
                g, hp = pairs[pi]
                d = GD[g]
                nblk = (S // d) // 128
                os_ = ost[pi % 2]
                for r in range(d):
                    P.dma('sp', dout_d[g].rearrange("(j p r) c -> r p j c", p=128, r=d)[r][:, :, hp * 130:hp * 130 + 130],
                          os_.ap[:, r * nblk:(r + 1) * nblk].rearrange("p j h c -> p j (h c)"),
                          [os_], [dout_b], group='fill')

            items = []
            for pi, (g, hp) in enumerate(pairs):
                d = GD[g]
                L = S // d
                nblk = L // 128
                cnt_pair = d * (nblk // 2) * 2
                ci = 0
                for r in range(d):
                    for jj in range(nblk // 2):
                        for s_ in range(2):
                            items.append(dict(n=len(items), pi=pi, g=g, hp=hp, d=d, L=L, nblk=nblk, r=r, jj=jj, s=s_,
                                              first=(ci == 0), mid=(ci == cnt_pair // 2), last=(ci == cnt_pair - 1)))
                            ci += 1
            load_pair(0)
            load_V(0)
            load_pair(1)
            load_V(1)
            permute_pair(0)

            def stC_qk(it_):
                n = it_['n']
                k_ = it_['pi'] % 2
                if it_['mid'] and it_['pi'] + 1 < len(pairs):
                    permute_pair(it_['pi'] + 1)
                if it_['first'] and it_['pi'] >= 1 and it_['pi'] + 1 < len(pairs):
                    load_pair(it_['pi'] + 1)
                sp_ = sps[n % 4]
                L, r, s_ = it_['L'], it_['r'], it_['s']
                bs = slice(64 * s_, 64 * s_ + 64)
                for a in range(2):
                    j = 2 * it_['jj'] + a
                    kb0 = r * (L + 128) + 128 * j
                    col = a * 256
                    qsl = qP[k_].ap[bs, r * L + 128 * j:r * L + 128 * j + 128]
                    P.mm(sp_.ap[:, col:col + 128], kP[k_].ap[bs, kb0:kb0 + 128], qsl, a == 0, False, [kP[k_], qP[k_]], [sp_])
                    P.mm(sp_.ap[:, col + 128:col + 256], kP[k_].ap[bs, kb0 + 128:kb0 + 256], qsl, False, False,
                         [kP[k_], qP[k_]], [sp_])
                jj, nblk = it_['jj'], it_['nblk']
                f0 = (jj == 0)
                l1 = (2 * jj + 1 == nblk - 1)
                var = 3 if (f0 and l1) else (0 if f0 else (2 if l1 else 1))
                P.mm(sp_.ap[:], ident_b, dmask2(var), False, True, [cstb], [sp_])

            def stC_exp(it_):
                n = it_['n']
                sp_ = sps[n % 4]
                p_ = pt[n % 5]
                P.act(p_.ap[:], sp_.ap[:], AF.Exp, [sp_], [p_], scale=0.125)

            def stC_pv(it_):
                n = it_['n']
                g = it_['g']
                p_ = pt[n % 5]
                o_ = po[n % 3]
                vg = Vg[g % 2]
                os_ = ost[it_['pi'] % 2]
                r, nblk, s_ = it_['r'], it_['nblk'], it_['s']
                hh = 2 * it_['hp'] + s_
                for a in range(2):
                    j = 2 * it_['jj'] + a
                    b0 = r * (nblk + 1) + j
                    oc = a * 65
                    P.mm(o_.ap[:, oc:oc + 65], p_.ap[:, a * 256:a * 256 + 128], vg.ap[:, b0, hh, :], True, False, [p_, vg], [o_])
                    P.mm(o_.ap[:, oc:oc + 65], p_.ap[:, a * 256 + 128:a * 256 + 256], vg.ap[:, b0 + 1, hh, :], False, True,
                         [p_, vg], [o_])
                j0 = r * nblk + 2 * it_['jj']
                P.cp('act' if n % 2 == 0 else 'dve', os_.ap[:, j0:j0 + 2, s_, :],
                     o_.ap[:, 0:130].rearrange("p (h c) -> p h c", c=65), [o_], [os_], group=('ost', it_['pi']))
                if it_['last']:
                    store_pair(it_['pi'])
                    if g == 0 and it_['hp'] == 1:
                        load_V(2)

            pipeline(items, [stC_qk, stC_exp, stC_pv], [0, 0, 2])
        P.es = es
        P.barrier()
        if stop_after == 'C':
            P.emit()
            return nc, P

        esDE = ExitStack()
        es.enter_context(esDE)
        P.es = esDE
        attnB = P.sbuf("attnB", [128, NT, 512], BF16)
        attnBv = P.views("attnBv", attnB, NT)
        PA = P.sbuf("PA", [128, 2, D], BF16)
        PB = P.sbuf("PB", [128, 4, D], BF16)
        WO = P.sbuf("WO", [128, 8, D], BF16)

        def load_branch_weights(after):
            P.dma('pool', PA.ap[:], wa_d.rearrange("(c p) n -> p c n", p=128), after, [PA])
            P.dma('pool', PB.ap[:], wb_d.rearrange("(c p) n -> p c n", p=128), after, [PB])
            for c in range(8):
                P.dma('pool', WO.ap[:, c, :], wo_d[c * 128:(c + 1) * 128, :], after, [WO], group='wo')
        with ExitStack() as es4:
            P.es = es4
            q2 = ring("q2", 2, [128, S], BF16)
            k2 = ring("k2", 2, [128, S], BF16)
            Vn = P.sbuf("Vn", [128, NT, 8, 65], BF16)
            nmf = P.sbuf("nmf", [128, 21, 128], F32)
            nbf = ring("nbf", 1, [128, 21, 128], F32)
            Eh = ring("Eh", 3, [128, 21 * 128], BF16)
            ptn = ring("ptn", 4, [128, 640], BF16)
            rdn = ring("rdn", 4, [128, 1], F32)
            spn = ring("spn", 2, [128, 1024], F32, psum=True)
            pon = ring("pon", 3, [128, 512], F32, psum=True)
            P.dma('sp', nmf.ap[:], namask_d, [], [nmf])
            for q4 in range(4):
                P.dma('act', Vn.ap[:, q4 * 8:(q4 + 1) * 8].rearrange("p k h c -> p k (h c)"),
                      v_d[q4 * 1024:(q4 + 1) * 1024, 12 * 65:20 * 65].rearrange("(k p) c -> p k c", p=128), [v_b], [Vn],
                      group='vn')

            def loadD_pair(hp):
                k_ = hp % 2
                P.dma('sp', q2[k_].ap[:], qkT_d[1536 + hp * 128:1536 + hp * 128 + 128, :], [qkT_b], [q2[k_]])
                P.dma('sp', k2[k_].ap[:], qkT_d[2048 + hp * 128:2048 + hp * 128 + 128, :], [qkT_b], [k2[k_]])

            negm = P.sbuf("negm", [128, 21, 128], F32)
            P.ts('pool', negm.ap[:], nmf.ap[:], -1.0, 30000.0, ALU.add, ALU.mult, [nmf], [negm])

            def make_E(h):
                e_ = 0
                P.dma('sp', nbf[e_].ap[:], nabias_d[h], [], [nbf[e_]])
                P.stt(Eh[h % 3].ap[:].rearrange("p (t q) -> p t q", q=128), nbf[e_].ap[:], 8.0, negm.ap[:], ALU.mult, ALU.add,
                      [nbf[e_], negm], [Eh[h % 3]])

            itemsD = []
            for hp in range(4):
                for s_ in range(2):
                    for b in range(NT):
                        itemsD.append(dict(n=len(itemsD), hp=hp, s=s_, h=2 * hp + s_, b=b))
            loadD_pair(0)
            loadD_pair(1)
            make_E(0)
            make_E(1)

            def stD_qk(it_):
                n, hp, s_, h, b = it_['n'], it_['hp'], it_['s'], it_['h'], it_['b']
                k_ = hp % 2
                if b == 0 and h + 2 < 8:
                    make_E(h + 2)
                if b == 0 and s_ == 0 and 1 <= hp < 3:
                    loadD_pair(hp + 1)
                bs = slice(64 * s_, 64 * s_ + 64)
                kbs, tile0 = na_block_info(b)
                nk = len(kbs)
                sp_ = spn[n % 2]
                qsl = q2[k_].ap[bs, b * 128:(b + 1) * 128]
                for i, kb in enumerate(kbs):
                    P.mm(sp_.ap[:, i * 128:(i + 1) * 128], k2[k_].ap[bs, kb * 128:(kb + 1) * 128], qsl, i == 0 or i == 4, False,
                         [k2[k_], q2[k_]], [sp_])
                n1 = min(nk, 4) * 128
                P.mm(sp_.ap[:, 0:n1], ident_b, Eh[h % 3].ap[:, tile0 * 128:tile0 * 128 + n1], False, True, [Eh[h % 3], cstb], [sp_])
                if nk == 5:
                    P.mm(sp_.ap[:, 512:640], ident_b, Eh[h % 3].ap[:, (tile0 + 4) * 128:(tile0 + 5) * 128], False, True,
                         [Eh[h % 3], cstb], [sp_])

            def stD_exp(it_):
                n, h, b = it_['n'], it_['h'], it_['b']
                kbs, tile0 = na_block_info(b)
                nk = len(kbs)
                sp_ = spn[n % 2]
                p_ = ptn[n % 4]
                n1 = min(nk, 4) * 128
                P.act(p_.ap[:, 0:n1], sp_.ap[:, 0:n1], AF.Exp, [sp_], [p_], scale=0.125)
                if nk == 5:
                    P.act(p_.ap[:, 512:640], sp_.ap[:, 512:640], AF.Exp, [sp_], [p_], scale=0.125)

            def stD_pv(it_):
                n, h, b = it_['n'], it_['h'], it_['b']
                kbs, tile0 = na_block_info(b)
                nk = len(kbs)
                p_ = ptn[n % 4]
                o_ = pon[n % 3]
                ocol = 0
                rd_ = rdn[n % 4]
                for i, kb in enumerate(kbs):
                    P.mm(o_.ap[:, ocol:ocol + 65], p_.ap[:, i * 128:(i + 1) * 128], Vn.ap[:, kb, h, :], i == 0,
                         i == nk - 1, [p_, Vn], [o_])
                P.op('dve', lambda e, o=rd_.ap[:], i_=o_.ap[:, ocol + 64:ocol + 65]: e.reciprocal(out=o, in_=i_),
                     [o_], [rd_])
                P.act(attnB.ap[:, b, h * 64:(h + 1) * 64], o_.ap[:, ocol:ocol + 64], AF.Copy, [o_, rd_], [attnBv[b]],
                      scale=rd_.ap[:, 0:1])
                if n == 24:
                    load_branch_weights([attnBv[b]])

            pipeline(itemsD, [stD_qk, stD_exp, stD_pv], [0, 0, 2])
        P.es = esDE
        P.barrier()
        if debug:
            dbgB_d = nc.dram_tensor("dbg_attnB", [128, NT, 512], BF16, kind="ExternalOutput").ap()
            P.dma('sp', dbgB_d, attnB.ap[:], attnBv, [])
        if stop_after == 'D':
            P.emit()
            return nc, P

        with ExitStack() as es5:
            P.es = es5
            wr = P.sbuf("wr", [128, 8, NE], F32)
            g2r = P.sbuf("g2r", [128, D], F32)
            P.dma('sp', wr.ap[:], wr_d.rearrange("(c p) e -> p c e", p=128), [], [wr])
            P.dma('sp', g2r.ap[:], g2_d, [], [g2r])
            d3 = ring("d3", 3, [128, 3, 260], F32)
            s01 = ring("s01", 3, [128, 260], F32)
            rd4 = ring("rd4", 3, [128, 4], F32)
            yAt = ring("yAt", 3, [128, 4, 64], BF16)
            tpE = ring("tpE", 1, [128, 8, 128], BF16, psum=True)
            yT = ring("yT", 2, [128, 6, 512], BF16)
            sg = ring("sg", 2, [128, 16, 512], BF16)
            psE = ring("psE", 2, [128, 512], F32, psum=True)
            psX = ring("psX", 2, [128, 512], F32, psum=True)
            lgp = P.psum("lgp", [128, 512], F32)
            m1 = ring("m1", 2, [128, 512], F32)
            m2 = ring("m2", 2, [128, 512], F32)
            mT = ring("mT", 2, [128, 8, 512], BF16)
            xt2 = ring("xt2", 2, [128, D], F32)
            x1 = ring("x1", 2, [128, D], F32)
            junk2 = P.sbuf("junk2", [128, D], BF16)
            ss2 = ring("ss2", 3, [128, 1], F32)
            rs2 = ring("rs2", 3, [128, 1], F32)
            rr2 = ring("rr2", 3, [128, 1], F32)
            h2f = ring("h2f", 2, [128, D], F32)
            h2b = ring("h2b", 2, [128, D], BF16)
            tpF = ring("tpF", 2, [128, 4, 128], F32, psum=True)
            h2T = ring("h2T", 2, [128, 8, 128], F32)
            pe_i = [0]

            def nxt():
                p_ = psE[pe_i[0] % 3]
                pe_i[0] += 1
                return p_

            def T0(t):
                k_ = t % 3
                P.dma('sp', d3[k_].ap[:], dout_d[:, t * 128:(t + 1) * 128, :].rearrange("g p c -> p g c"), [dout_b], [d3[k_]])

            def SGL(tb):
                P.dma('act', sg[tb % 2].ap[:], sgT_d[:, tb * 512:(tb + 1) * 512].rearrange("(o p) t -> p o t", p=128),
                      [sgT_b], [sg[tb % 2]])

            def T1(t):
                k_ = t % 3
                P.tt('pool', s01[k_].ap[:], d3[k_].ap[:, 0, :], d3[k_].ap[:, 1, :], ALU.add, [d3[k_]], [s01[k_]])
                P.tt('pool', s01[k_].ap[:], s01[k_].ap[:], d3[k_].ap[:, 2, :], ALU.add, [s01[k_], d3[k_]], [s01[k_]])

            def T2(t):
                k_ = t % 3
                s3 = s01[k_].ap[:].rearrange("p (h c) -> p h c", c=65)
                P.op('dve', lambda e, o=rd4[k_].ap[:], i_=s3[:, :, 64]: e.reciprocal(out=o, in_=i_), [s01[k_]], [rd4[k_]])
                P.tt('dve', yAt[k_].ap[:], s3[:, :, 0:64], rd4[k_].ap[:, :, None].to_broadcast([128, 4, 64]), ALU.mult,
                     [s01[k_], rd4[k_]], [yAt[k_]])

            def T3(t):
                tb, a = t // 4, t % 4
                y_ = yT[tb % 2]
                k_ = t % 3
                yA2 = yAt[k_].ap[:].rearrange("p h c -> p (h c)")
                tp_ = tpE[0]
                for c in range(2):
                    P.tr(tp_.ap[:, c, :], yA2[:, c * 128:(c + 1) * 128], ident_b, [yAt[k_], cstb], [tp_])
                for c in range(4):
                    P.tr(tp_.ap[:, 2 + c, :], attnB.ap[:, t, c * 128:(c + 1) * 128], ident_b, [attnBv[t], cstb], [tp_])
                P.cp('act', y_.ap[:, :, a * 128:(a + 1) * 128], tp_.ap[:, 0:6, :], [tp_], [y_], group=('yT', tb))

            def E2a(tb, j):
                ob, hf = j // 2, j % 2
                y_ = yT[tb % 2]
                bk = psE[j % 2]
                osl = slice(ob * 128, (ob + 1) * 128)
                hsl = slice(hf * 256, (hf + 1) * 256)
                for c in range(2):
                    P.mm(bk.ap[:, 0:256], PA.ap[:, c, osl], y_.ap[:, c, hsl], c == 0, c == 1, [PA, y_], [bk])
                for c in range(4):
                    P.mm(bk.ap[:, 256:512], PB.ap[:, c, osl], y_.ap[:, 2 + c, hsl], c == 0, c == 3, [PB, y_], [bk])

            def E2b(tb, j):
                ob, hf = j // 2, j % 2
                sg_ = sg[tb % 2]
                bk = psE[j % 2]
                hsl = slice(hf * 256, (hf + 1) * 256)
                k_ = j % 2
                P.tt('dve', m1[k_].ap[:, 0:256], bk.ap[:, 0:256], sg_.ap[:, ob, hsl], ALU.mult, [bk, sg_], [m1[k_]])
                P.tt('dve', m2[k_].ap[:, 0:256], bk.ap[:, 256:512], sg_.ap[:, 8 + ob, hsl], ALU.mult, [bk, sg_], [m2[k_]])

            def E2c(tb, j):
                ob, hf = j // 2, j % 2
                m_ = mT[tb % 2]
                k_ = j % 2
                hsl = slice(hf * 256, (hf + 1) * 256)
                P.tt('pool', m_.ap[:, ob, hsl], m1[k_].ap[:, 0:256], m2[k_].ap[:, 0:256], ALU.add, [m1[k_], m2[k_]], [m_],
                     group=('mT', tb))

            def XL(t):
                P.dma('sp', xt2[t % 2].ap[:], x_d[t * 128:(t + 1) * 128, :], [], [xt2[t % 2]])

            def X0(t):
                tb, a = t // 4, t % 4
                m_ = mT[tb % 2]
                for ch in range(2):
                    px = psX[(2 * t + ch) % 2]
                    csl = slice(ch * 512, (ch + 1) * 512)
                    for c in range(8):
                        P.mm(px.ap[:], m_.ap[:, c, a * 128:(a + 1) * 128], WO.ap[:, c, csl], c == 0, c == 7, [m_, WO], [px])

            def X0b(t):
                for ch in range(2):
                    px = psX[(2 * t + ch) % 2]
                    csl = slice(ch * 512, (ch + 1) * 512)
                    P.tt('dve', x1[t % 2].ap[:, csl], px.ap[:], xt2[t % 2].ap[:, csl], ALU.add, [px, xt2[t % 2]], [x1[t % 2]],
                         group=('x1', t))
                P.dma('sp', out_d[t * 128:(t + 1) * 128, :], x1[t % 2].ap[:], [x1[t % 2]], [out_b], group='fill')

            def X0c(t):
                k_ = t % 3
                P.act(junk2.ap[:], x1[t % 2].ap[:], AF.Square, [x1[t % 2]], [junk2, ss2[k_]], accum_out=ss2[k_].ap[:])
                P.act(rs2[k_].ap[:], ss2[k_].ap[:], AF.Sqrt, [ss2[k_]], [rs2[k_]], scale=1.0 / D, bias=EPS)

            def X1(t):
                k_ = t % 3
                P.op('dve', lambda e, o=rr2[k_].ap[:], i_=rs2[k_].ap[:]: e.reciprocal(out=o, in_=i_), [rs2[k_]], [rr2[k_]])
                P.stt(h2f[t % 2].ap[:], x1[t % 2].ap[:], rr2[k_].ap[:, 0:1], g2r.ap[:], ALU.mult, ALU.mult,
                      [x1[t % 2], rr2[k_], g2r], [h2f[t % 2]])

            def X1b(t):
                P.cp('act', h2b[t % 2].ap[:], h2f[t % 2].ap[:], [h2f[t % 2]], [h2b[t % 2]])
                P.dma('sp', h2_d[t * 128:(t + 1) * 128, :], h2b[t % 2].ap[:], [h2b[t % 2]], [h2_b], group='fill')
                for hf in range(2):
                    tf = tpF[hf]
                    for c in range(4):
                        cc_ = hf * 4 + c
                        P.tr(tf.ap[:, c, :], h2f[t % 2].ap[:, cc_ * 128:(cc_ + 1) * 128], ident_f, [h2f[t % 2], cst], [tf])

            def X2(t):
                for hf in range(2):
                    tf = tpF[hf]
                    P.cp('act', h2T[t % 2].ap[:, hf * 4:hf * 4 + 4, :], tf.ap[:], [tf], [h2T[t % 2]], group=('h2T', t))

            def X3(t):
                for c in range(8):
                    P.mm(lgp.ap[:, t * NE:(t + 1) * NE], h2T[t % 2].ap[:, c, :], wr.ap[:, c, :], c == 0, c == 7, [h2T[t % 2], wr], [lgp])

            ev = []
            for t in range(NT):
                tb, a = t // 4, t % 4
                ev += [(4 * t, 0, T0, (t,)), (4 * t + 4, 1, T1, (t,)), (4 * t + 8, 2, T2, (t,)), (4 * t + 12, 3, T3, (t,))]
                x0 = 16 * tb + 48 + 4 * a
                ev += [(x0 - 4, 7, XL, (t,)), (x0, 8, X0, (t,)), (x0 + 2, 9, X0b, (t,)), (x0 + 4, 10, X0c, (t,)),
                       (x0 + 8, 11, X1, (t,)), (x0 + 12, 12, X1b, (t,)), (x0 + 16, 13, X2, (t,)), (x0 + 20, 14, X3, (t,))]
            for tb in range(8):
                ev.append((16 * tb + 14 if tb >= 2 else 0, 0.5, SGL, (tb,)))
                for j in range(16):
                    u = 16 * tb + 28 + j
                    ev += [(u, 4, E2a, (tb, j)), (u + 1, 5, E2b, (tb, j)), (u + 2, 6, E2c, (tb, j))]
            ev.sort(key=lambda x: (x[0], -x[1]))
            for _, _, f_, args_ in ev:
                f_(*args_)
            P.cp('dve', lg_all.ap[:].rearrange("p t e -> p (t e)"), lgp.ap[:], [lgp], [lg_all])
        P.es = es
        esDE.close()
        P.barrier()
        if debug:
            dbgL_d = nc.dram_tensor("dbg_lg", [128, NT, NE], F32, kind="ExternalOutput").ap()
            P.dma('sp', dbgL_d, lg_all.ap[:], [lg_all], [])
        if stop_after == 'E':
            P.emit()
            return nc, P

        with ExitStack() as es7:
            P.es = es7
            NWB = 6
            wbg = ring("wbg", NWB, [128, 8, 512], BF16)
            xg = ring("xg", 12, [128, D], BF16)
            xinT = ring("xinT", 2, [128, 8, 512], BF16)
            sa = ring("sa", 3, [128, 512], F32)
            actT = ring("actT", 2, [128, 8, 512], BF16)
            yst = ring("yst", 4, [128, D], F32)
            tpG = ring("tpG", 2, [128, 8, 128], BF16, psum=True)
            psG = ring("psG", 5, [128, 512], F32, psum=True)
            cpsb = P.psum("cpsb", [128, 512], F32)
            es6 = ExitStack()
            es7.enter_context(es6)
            fdbg = emit_phase_F(P, ring, cst, ones_f, triu_f, ident_f, lg_all, idx_all, gate_all,
                                banks=[psG[0], psG[1], psG[2], cpsb], es_persist=es7, es_tmp=es6,
                                idx_v=idx_v, gate_v=gate_v)
            compact = fdbg['compact']
            P.es = es7
            es6.close()
            if stop_after == 'F':
                for e_ in range(NE):
                    compact(e_)
                dbgI_d = nc.dram_tensor("dbg_idx", [128, NE, 4], I32, kind="ExternalOutput").ap()
                dbgG_d = nc.dram_tensor("dbg_gate", [128, NE, 4], F32, kind="ExternalOutput").ap()
                P.dma('sp', dbgI_d, idx_all.ap[:], idx_v, [])
                P.dma('sp', dbgG_d, gate_all.ap[:], gate_v, [])
                P.emit()
                return nc, P
            pi_ = [0]
            wchunks = []
            for e_ in range(NE):
                for fh in range(2):
                    wchunks.append((e_, 'g', fh))
                    wchunks.append((e_, 'u', fh))
                for ch in range(2):
                    wchunks.append((e_, 'd', ch))
            PF = 4

            def load_w(k):
                if k >= len(wchunks):
                    return
                e_, kind, h_ = wchunks[k]
                src_t = {'g': wg_d, 'u': wu_d, 'd': wd_d}[kind]
                hsl = slice(h_ * 512, (h_ + 1) * 512)
                wb_ = wbg[k % NWB]
                P.dma('pool', wb_.ap[:], src_t[e_].rearrange("(c p) f -> p c f", p=128)[:, :, hsl], [], [wb_])

            def nps():
                p_ = psG[pi_[0] % 5]
                pi_[0] += 1
                return p_

            def prep_gather(e_):
                if e_ >= NE:
                    return
                for sc in range(4):
                    g_ = xg[(e_ * 4 + sc) % 12]
                    P.op('pool', lambda e, o=g_.ap[:], ix=idx_all.ap[:, e_, sc:sc + 1]: e.indirect_dma_start(
                        out=o, out_offset=None, in_=h2_d, in_offset=bass.IndirectOffsetOnAxis(ap=ix, axis=0)),
                        [idx_v[e_], h2_b], [g_], dma=True)

            def prep_tr(e_):
                if e_ >= NE:
                    return
                xT = xinT[e_ % 2]
                for sc in range(4):
                    g_ = xg[(e_ * 4 + sc) % 12]
                    tp_ = tpG[sc % 2]
                    for c in range(8):
                        P.tr(tp_.ap[:, c, :], g_.ap[:, c * 128:(c + 1) * 128], ident_b, [g_, cstb], [tp_])
                    P.cp('act' if sc % 2 == 0 else 'dve', xT.ap[:, :, sc * 128:(sc + 1) * 128], tp_.ap[:], [tp_], [xT],
                         group=('xT', e_))

            for k in range(PF):
                load_w(k)
            compact(0)
            prep_gather(0)
            compact(1)
            prep_gather(1)
            prep_tr(0)
            for k, (e_, kind, h_) in enumerate(wchunks):
                load_w(k + PF)
                xT = xinT[e_ % 2]
                aT = actT[e_ % 2]
                if kind == 'g':
                    if h_ == 0 and e_ + 2 < NE:
                        compact(e_ + 2)
                        prep_gather(e_ + 2)
                    continue
                if kind == 'u':
                    fh = h_
                    wg_ = wbg[(k - 1) % NWB]
                    wu_ = wbg[k % NWB]
                    for fc in range(4):
                        pa = nps()
                        pu = nps()
                        for c in range(8):
                            P.mm(pa.ap[:], wg_.ap[:, c, fc * 128:(fc + 1) * 128], xT.ap[:, c, :], c == 0, c == 7, [wg_, xT], [pa])
                        for c in range(8):
                            P.mm(pu.ap[:], wu_.ap[:, c, fc * 128:(fc + 1) * 128], xT.ap[:, c, :], c == 0, c == 7, [wu_, xT], [pu])
                        s__ = sa[fc % 3]
                        P.act(s__.ap[:], pa.ap[:], AF.Silu, [pa], [s__])
                        P.tt('dve', aT.ap[:, fh * 4 + fc, :], s__.ap[:], pu.ap[:], ALU.mult, [s__, pu], [aT], group=('aT', e_))
                    if fh == 1:
                        prep_tr(e_ + 1)
                    continue
                ch = h_
                csl = slice(ch * 512, (ch + 1) * 512)
                wd_ = wbg[k % NWB]
                for sc in range(4):
                    py = nps()
                    y_ = yst[sc]
                    for fc in range(8):
                        P.mm(py.ap[:], aT.ap[:, fc, sc * 128:(sc + 1) * 128], wd_.ap[:, fc, :], fc == 0, fc == 7, [aT, wd_], [py])
                    P.act(y_.ap[:, csl], py.ap[:], AF.Copy, [py, gate_v[e_]], [y_], group=('y', e_, sc),
                          scale=gate_all.ap[:, e_, sc:sc + 1])
                if ch == 1:
                    for sc in range(4):
                        y_ = yst[sc]
                        P.op('pool', lambda e, i_=y_.ap[:], ix=idx_all.ap[:, e_, sc:sc + 1]: e.indirect_dma_start(
                            out=out_d, out_offset=bass.IndirectOffsetOnAxis(ap=ix, axis=0), in_=i_, in_offset=None,
                            compute_op=ALU.add), [idx_v[e_], y_], [out_b], dma=True, group=('scat', e_))
        P.es = es

        P.emit()
    return nc, P


_CACHE = {}


def kernel(x, norm1_g, w_in, dil_q_norm_g, dil_k_norm_g, na_q_norm_g, na_k_norm_g, na_rpb,
           w_dil_branch, w_na_branch, w_out, norm2_g, w_router, w_gate, w_up, w_down, _debug=False,
           _stop_after=None, _cores=8):
    x = np.asarray(x, np.float32)
    consts, cosT, sinT, namask, dri, dci = host_consts()
    f = lambda a: np.ascontiguousarray(np.asarray(a, np.float32))
    g1 = f(np.asarray(norm1_g)[0].reshape(8, 128).T)
    gv = f(np.stack([np.tile(np.asarray(g)[0], 2) for g in (dil_q_norm_g, dil_k_norm_g, na_q_norm_g, na_k_norm_g)], axis=1))
    g2rep = f(np.broadcast_to(np.asarray(norm2_g)[0][None, :], (128, D)))
    rpb = np.asarray(na_rpb, np.float32)[0]
    nabias = f(rpb[:, dri, dci].transpose(0, 2, 1, 3))
    shared = dict(w_in=f(w_in[0]), g1=g1, gv=gv, g2rep=g2rep, consts=consts, cosT=cosT, sinT=sinT,
                  namask=namask, nabias=nabias, w_dil_branch=f(w_dil_branch[0]), w_na_branch=f(w_na_branch[0]),
                  w_out=f(w_out[0]), w_router=f(w_router[0]), w_gate=f(w_gate[0]), w_up=f(w_up[0]),
                  w_down=f(w_down[0]))
    key = (_debug, _stop_after)
    import time as _time
    _t0 = _time.time()
    if key not in _CACHE:
        _CACHE[key] = build_program(debug=_debug, stop_after=_stop_after)
    nc, P = _CACHE[key]
    if _debug:
        print("build_program s:", _time.time() - _t0, P.stats, flush=True)
    if _stop_after not in (None, 'G'):
        for k_ in ("w_gate", "w_up", "w_down"):
            shared.pop(k_)
    in_maps = []
    for c in range(_cores):
        m = dict(shared)
        m["x"] = np.ascontiguousarray(x[c])
        in_maps.append(m)
    _t0 = _time.time()
    res = run_bass_kernel_spmd(nc, in_maps, core_ids=list(range(_cores)))
    if _debug:
        print("run s:", _time.time() - _t0, flush=True)
        return res.results
    return np.stack([r["out"] for r in res.results], axis=0)
```

```python
import numpy as np
from contextlib import ExitStack
import concourse.bass as bass
import concourse.mybir as mybir
from concourse.bass_utils import run_bass_kernel_spmd

F32 = mybir.dt.float32
BF16 = mybir.dt.bfloat16
I32 = mybir.dt.int32
U32 = mybir.dt.uint32
U16 = mybir.dt.uint16
AF = mybir.ActivationFunctionType
ALU = mybir.AluOpType
AX = mybir.AxisListType

S = 4096
D = 1024
NT = 32
EPS = 1e-6
NE = 16
CAP = 512
N_BISECT = 30

QUEUES = ['pe', 'act', 'dve', 'pool', 'sp']
RING = {'sp': 16, 'act': 8, 'pool': 12}


class Buf:
    def __init__(self, name, ap=None):
        self.name = name
        self.ap = ap
        self.w = []
        self.r = []
        self.pw = []
        self.pr = []
        self.wgroup = None


class Prog:
    def __init__(self, nc, es):
        self.nc = nc
        self.es = es
        self.ops = []
        self.bar = []

    def sbuf(self, name, shape, dtype):
        self.uid = getattr(self, 'uid', 0) + 1
        t = self.es.enter_context(self.nc.sbuf_tensor("sb%d_%s" % (self.uid, name), list(shape), dtype))
        return Buf(name, t)

    def psum(self, name, shape, dtype):
        self.uid = getattr(self, 'uid', 0) + 1
        t = self.es.enter_context(self.nc.psum_tensor("ps%d_%s" % (self.uid, name), list(shape), dtype))
        b = Buf(name, t)
        b.psum = True
        return b

    def dram_buf(self, name):
        return Buf(name, None)

    def views(self, name, buf, n):
        assert not getattr(buf, 'psum', False), "PSUM banks must be tracked as a whole (bank collisions)"
        return [Buf("%s_%d" % (name, i), buf.ap) for i in range(n)]

    def barrier(self):
        last = {}
        deps = set()
        for i, o in enumerate(self.ops):
            if o['dma']:
                deps.add(i)
            else:
                last[o['q']] = i
        deps.update(last.values())
        self.bar = sorted(deps)

    def op(self, q, fn, reads=(), writes=(), dma=False, group=None):
        i = len(self.ops)
        raw = set()
        oth = set()
        excl = [b for b in reads if getattr(b, 'psum', False)]
        if excl:
            reads = [b for b in reads if not getattr(b, 'psum', False)]
            writes = list(writes) + [b for b in excl if b not in writes]
        for b in reads:
            raw.update(b.w)
        for b in writes:
            if group is not None and b.wgroup == group:
                oth.update(b.pw)
                oth.update(b.pr)
                oth.update(b.r)
            else:
                oth.update(b.w)
                oth.update(b.r)
        for b in reads:
            b.r.append(i)
        for b in writes:
            if group is not None and b.wgroup == group:
                b.w.append(i)
            else:
                b.pw = b.w
                b.pr = [x for x in b.r if x != i]
                b.w = [i]
                b.r = []
                b.wgroup = group
        deps = set()
        for d in raw | oth:
            if d == i:
                continue
            o = self.ops[d]
            if o['q'] == q and not o['dma'] and not dma:
                if q != 'pe':
                    deps.add(d)
                continue
            deps.add(d)
        for d in self.bar:
            o = self.ops[d]
            if o['q'] == q and not o['dma'] and not dma:
                continue
            deps.add(d)
        self.ops.append(dict(q=q, fn=fn, deps=deps, dma=dma))
        return i

    def dma(self, q, out, in_, reads=(), writes=(), group=None, **kw):
        return self.op(q, lambda e: e.dma_start(out=out, in_=in_, **kw), reads, writes, dma=True, group=group)

    def mm(self, out, lhsT, rhs, start, stop, reads, writes, group=None):
        return self.op('pe', lambda e: e.matmul(out, lhsT=lhsT, rhs=rhs, start=start, stop=stop), reads, writes,
                       group=group)

    def tr(self, out, in_, ident, reads, writes, group=None):
        return self.op('pe', lambda e: e.transpose(out, in_, ident), reads, writes, group=group)

    def act(self, out, in_, func, reads, writes, group=None, **kw):
        return self.op('act', lambda e: e.activation(out=out, in_=in_, func=func, **kw), reads, writes, group=group)

    def tt(self, q, out, in0, in1, op, reads, writes, group=None):
        return self.op(q, lambda e: e.tensor_tensor(out=out, in0=in0, in1=in1, op=op), reads, writes, group=group)

    def ts(self, q, out, in0, s1, s2, op0, op1, reads, writes, group=None, **kw):
        if op1 is None:
            return self.op(q, lambda e: e.tensor_scalar(out=out, in0=in0, scalar1=s1, scalar2=None, op0=op0, **kw),
                           reads, writes, group=group)
        return self.op(q, lambda e: e.tensor_scalar(out=out, in0=in0, scalar1=s1, scalar2=s2, op0=op0, op1=op1, **kw),
                       reads, writes, group=group)

    def stt(self, out, in0, scalar, in1, op0, op1, reads, writes):
        return self.op('dve', lambda e: e.scalar_tensor_tensor(out=out, in0=in0, scalar=scalar, in1=in1,
                                                                op0=op0, op1=op1), reads, writes)

    def cp(self, q, out, in_, reads, writes, group=None):
        if q == 'act':
            return self.op(q, lambda e: e.copy(out=out, in_=in_), reads, writes, group=group)
        return self.op(q, lambda e: e.tensor_copy(out=out, in_=in_), reads, writes, group=group)

    def emit(self):
        nc = self.nc
        es = self.es
        ops = self.ops
        flagged = [False] * len(ops)
        for o in ops:
            for d in o['deps']:
                flagged[d] = True
        prog_sem = {q: es.enter_context(nc.semaphore("pg_" + q)) for q in ['pe', 'act', 'dve', 'pool']}
        ring_sem = {q: [es.enter_context(nc.semaphore("rg_%s_%d" % (q, k))) for k in range(n)]
                    for q, n in RING.items()}
        cnt = {q: 0 for q in QUEUES}
        dcnt = {q: 0 for q in QUEUES}
        done = [None] * len(ops)
        prev_ring = [None] * len(ops)
        for i, o in enumerate(ops):
            q = o['q']
            if o['dma']:
                k = dcnt[q]
                dcnt[q] += 1
                R = RING[q]
                sem = ring_sem[q][k % R]
                val = 16 * (k // R + 1)
                done[i] = (sem, val)
                if val > 16:
                    prev_ring[i] = (sem, val - 16)
            elif flagged[i]:
                cnt[q] += 1
                done[i] = (prog_sem[q], cnt[q])
        final = {}
        for q, sems in ring_sem.items():
            for k, s in enumerate(sems):
                n = (dcnt[q] - k + RING[q] - 1) // RING[q]
                if n > 0:
                    final[id(s)] = (s, 16 * n)
        self.stats = dict(cnt=cnt, dcnt=dcnt, nops=len(ops))

        def run_queue(q, eng):
            waited = {}
            for i, o in enumerate(ops):
                if o['q'] != q:
                    continue
                need = {}
                for d in o['deps']:
                    s, v = done[d]
                    if need.get(id(s), (None, 0))[1] < v:
                        need[id(s)] = (s, v)
                if prev_ring[i] is not None:
                    s, v = prev_ring[i]
                    if need.get(id(s), (None, 0))[1] < v:
                        need[id(s)] = (s, v)
                for k, (s, v) in need.items():
                    if waited.get(k, 0) >= v:
                        continue
                    eng.wait_ge(s, v)
                    waited[k] = v
                ins = o['fn'](eng)
                if done[i] is not None:
                    ins.then_inc(done[i][0], 16 if o['dma'] else 1)
            if q == 'sp':
                for k, (s, v) in final.items():
                    if waited.get(k, 0) < v:
                        eng.wait_ge(s, v)

        with nc.Block() as block:
            @block.tensor
            def _(e):
                run_queue('pe', e)

            @block.scalar
            def _(e):
                run_queue('act', e)

            @block.vector
            def _(e):
                run_queue('dve', e)

            @block.gpsimd
            def _(e):
                run_queue('pool', e)

            @block.sync
            def _(e):
                run_queue('sp', e)


NA_VARIANTS = [(2, [0, 1, 2, 3, 4]), (0, [0, 1, 2, 3]), (1, [0, 1, 2, 3]), (30, [28, 29, 30, 31]),
               (31, [28, 29, 30, 31])]
NA_TILE0 = [0, 5, 9, 13, 17]
C_IDENT, C_BONES, C_RMAT, C_ONES, C_TRIU = 0, 128, 256, 384, 512
C_DMASK = 640
C_IOTA = C_DMASK + 2048
C_RESET = C_IOTA + 512
C_TOK = C_RESET + 512
C_TOTAL = C_TOK + 64


def na_block_info(b):
    if 2 <= b <= 29:
        return list(range(b - 2, b + 3)), NA_TILE0[0]
    v = {0: 1, 1: 2, 30: 3, 31: 4}[b]
    return NA_VARIANTS[v][1], NA_TILE0[v]


def host_consts():
    c = np.zeros((128, C_TOTAL), np.float32)
    p = np.arange(128)
    c[:, C_IDENT:C_IDENT + 128] = np.eye(128)
    c[:, C_BONES:C_BONES + 128] = ((p[:, None] // 64) == (p[None, :] // 64)) / 64.0
    R = np.zeros((128, 128), np.float32)
    for i in range(128):
        if i % 64 < 32:
            R[i + 32, i] = -1.0
        else:
            R[i - 32, i] = 1.0
    c[:, C_RMAT:C_RMAT + 128] = R
    c[:, C_ONES:C_ONES + 128] = 1.0
    c[:, C_TRIU:C_TRIU + 128] = (p[:, None] < p[None, :])
    pk = p[:, None]
    pq = p[None, :]
    mA = (pk >= pq)
    mB = (pk <= pq)
    first = np.concatenate([mA & (pk >= 64), mB], axis=1)
    mid = np.concatenate([mA, mB], axis=1)
    last = np.concatenate([mA, mB & (pk < 64)], axis=1)
    c[:, C_DMASK:C_DMASK + 2048] = (np.concatenate([first, mid, mid, mid, mid, last, first, last], axis=1) - 1.0) * 30000.0
    c[:, C_IOTA:C_IOTA + 512] = np.arange(512)[None, :]
    rs = np.ones(512, np.float32)
    rs[0::32] = 0.0
    c[:, C_RESET:C_RESET + 512] = rs[None, :]
    tok = np.arange(32)[None, :] * 128 + p[:, None]
    c[:, C_TOK:C_TOK + 32] = tok // 64
    c[:, C_TOK + 32:C_TOK + 64] = tok % 64
    inv = (10000.0 ** (-np.arange(32, dtype=np.float32) / 32)).astype(np.float32)
    pos = np.arange(S, dtype=np.float32)
    ang = (pos[:, None] * inv[None, :]).astype(np.float32)
    cos = np.cos(ang).astype(np.float32).T
    sin = np.sin(ang).astype(np.float32).T
    cosT = np.tile(cos, (4, 1))
    sinT = np.tile(sin, (4, 1))
    mask = np.zeros((21, 128, 128), np.float32)
    dri = np.zeros((21, 128, 128), np.int64)
    dci = np.zeros((21, 128, 128), np.int64)
    t = 0
    for b, kbs in NA_VARIANTS:
        for kb in kbs:
            kr = 2 * kb + p // 64
            kc = p % 64
            r = 2 * b + p // 64
            qc = p % 64
            rs_ = np.clip(r - 4, 0, 56)
            ws = np.clip(qc - 8, 0, 48)
            valid = ((kr[:, None] >= rs_[None, :]) & (kr[:, None] < rs_[None, :] + 8) &
                     (kc[:, None] >= ws[None, :]) & (kc[:, None] < ws[None, :] + 16))
            mask[t] = valid
            dri[t] = np.clip(kr[:, None] - r[None, :] + 7, 0, 14)
            dci[t] = np.clip(kc[:, None] - qc[None, :] + 15, 0, 30)
            t += 1
    namask = np.ascontiguousarray(mask.transpose(1, 0, 2))
    return c, np.ascontiguousarray(cosT), np.ascontiguousarray(sinT), namask, dri, dci


W_SEGS = [('qa', 0, 768), ('ka', 768, 768), ('va', 1536, 768), ('qn', 2304, 512), ('kn', 2816, 512),
          ('vn', 3328, 512), ('ga', 3840, 1024), ('gn', 4864, 1024)]
QK_ROW = {'qa': 0, 'ka': 768, 'qn': 1536, 'kn': 2048}
V_COL = {'va': 0, 'vn': 768}
SG_ROW = {'ga': 0, 'gn': 1024}
GV_COL = {'qa': 0, 'ka': 1, 'qn': 2, 'kn': 3}
CW = 256


def emit_phase_F(P, ring, cst, ones_f, triu_f, ident_f, lg_all, idx_all, gate_all, banks, es_persist, es_tmp,
                 idx_v, gate_v):
    P.es = es_persist
    vals = P.sbuf("vals", [128, NE, NT, 5], BF16)
    shb = P.sbuf("shb", [128, 512], BF16)
    slb = P.sbuf("slb", [128, 512], BF16)
    iob = P.sbuf("iob", [128, 128], BF16)
    He = ring("He", 2, [128, NT, 128], BF16)
    Le = ring("Le", 2, [128, NT, 4], BF16)
    LVe = ring("LVe", 2, [128, NT, 4, 5], BF16)
    idf = ring("idf", 2, [128, 4], F32)
    csb = ring("csb", 2, [128, 20], F32)
    P.es = es_tmp
    totp, ppp, ccp, cpsb = banks
    mx = P.sbuf("mx", [128, NT], F32)
    sh = P.sbuf("sh", [128, NT, NE], F32)
    sm = P.sbuf("sm", [128, NT], F32)
    affT = P.sbuf("affT", [128, NE, NT], F32)
    lo = P.sbuf("lo", [128, NE], F32)
    hi = P.sbuf("hi", [128, NE], F32)
    mid = P.sbuf("mid", [128, NE], F32)
    cmpb = P.sbuf("cmpb", [128, NE, NT], F32)
    cntp = P.sbuf("cntp", [128, NE], F32)
    mge = P.sbuf("mge", [128, NE], U32)
    mlt = P.sbuf("mlt", [128, NE], U32)
    P.op('dve', lambda e: e.tensor_reduce(out=mx.ap[:], in_=lg_all.ap[:], axis=AX.X, op=ALU.max), [lg_all], [mx])
    P.tt('dve', sh.ap[:], lg_all.ap[:], mx.ap[:, :, None].to_broadcast([128, NT, NE]), ALU.subtract, [lg_all, mx], [sh])
    P.act(sh.ap[:], sh.ap[:], AF.Exp, [sh], [sh])
    P.op('dve', lambda e: e.tensor_reduce(out=sm.ap[:], in_=sh.ap[:], axis=AX.X, op=ALU.add), [sh], [sm])
    P.op('dve', lambda e: e.reciprocal(out=sm.ap[:], in_=sm.ap[:]), [sm], [sm])
    P.tt('dve', affT.ap[:].rearrange("p e i -> p i e"), sh.ap[:], sm.ap[:, :, None].to_broadcast([128, NT, NE]),
         ALU.mult, [sh, sm], [affT])
    P.op('dve', lambda e: e.memset(lo.ap[:], 0.0), [], [lo])
    P.op('dve', lambda e: e.memset(hi.ap[:], 1.0), [], [hi])
    for itb in range(N_BISECT):
        P.tt('dve', mid.ap[:], lo.ap[:], hi.ap[:], ALU.add, [lo, hi], [mid])
        P.ts('dve', mid.ap[:], mid.ap[:], 0.5, None, ALU.mult, None, [mid], [mid])
        P.tt('dve', cmpb.ap[:], affT.ap[:], mid.ap[:, :, None].to_broadcast([128, NE, NT]), ALU.is_ge, [affT, mid], [cmpb])
        P.op('dve', lambda e: e.tensor_reduce(out=cntp.ap[:], in_=cmpb.ap[:], axis=AX.X, op=ALU.add), [cmpb], [cntp])
        P.mm(totp.ap[:, 0:NE], ones_f, cntp.ap[:], True, True, [cntp, cst], [totp])
        P.ts('dve', mge.ap[:], totp.ap[:, 0:NE], float(CAP), None, ALU.is_ge, None, [totp], [mge])
        P.ts('dve', mlt.ap[:], totp.ap[:, 0:NE], float(CAP), None, ALU.is_lt, None, [totp], [mlt])
        P.op('dve', lambda e: e.copy_predicated(out=lo.ap[:], mask=mge.ap[:], data=mid.ap[:]), [mge, mid, lo], [lo])
        P.op('dve', lambda e: e.copy_predicated(out=hi.ap[:], mask=mlt.ap[:], data=mid.ap[:]), [mlt, mid, hi], [hi])
    sel = P.sbuf("sel", [128, 512], F32)
    cc = P.sbuf("cc", [128, 512], F32)
    csum = P.sbuf("csum", [128, 512], F32)
    slot = P.sbuf("slot", [128, 512], F32)
    ltb = P.sbuf("ltb", [128, 512], F32)
    slotm = P.sbuf("slotm", [128, 512], F32)
    affF = affT.ap[:].rearrange("p e i -> p (e i)")
    P.tt('dve', sel.ap[:].rearrange("p (e i) -> p e i", i=NT), affT.ap[:],
         lo.ap[:, :, None].to_broadcast([128, NE, NT]), ALU.is_ge, [affT, lo], [sel])
    P.mm(ppp.ap[:], triu_f, sel.ap[:], True, True, [sel, cst], [ppp])
    P.mm(ccp.ap[:], ones_f, sel.ap[:], True, True, [sel, cst], [ccp])
    P.cp('act', cc.ap[:], ccp.ap[:], [ccp], [cc])
    P.op('dve', lambda e: e.tensor_tensor_scan(out=csum.ap[:], data0=cst.ap[:, C_RESET:C_RESET + 512], data1=cc.ap[:],
                                               initial=0.0, op0=ALU.mult, op1=ALU.add), [cc, cst], [csum])
    P.tt('dve', slot.ap[:], csum.ap[:], cc.ap[:], ALU.subtract, [csum, cc], [slot])
    P.tt('dve', slot.ap[:], ppp.ap[:], slot.ap[:], ALU.add, [ppp, slot], [slot])
    P.ts('dve', ltb.ap[:], slot.ap[:], float(CAP), None, ALU.is_lt, None, [slot], [ltb])
    P.tt('dve', ltb.ap[:], ltb.ap[:], sel.ap[:], ALU.mult, [ltb, sel], [ltb])
    P.stt(slotm.ap[:], slot.ap[:], 1.0, ltb.ap[:], ALU.add, ALU.mult, [slot, ltb], [slotm])
    P.ts('dve', slotm.ap[:], slotm.ap[:], -1.0, None, ALU.add, None, [slotm], [slotm])
    a1 = P.sbuf("a1", [128, 512], BF16)
    r1 = P.sbuf("r1", [128, 512], F32)
    r2 = P.sbuf("r2", [128, 512], F32)
    P.cp('dve', a1.ap[:], affF, [affT], [a1])
    P.tt('dve', r1.ap[:], affF, a1.ap[:], ALU.subtract, [affT, a1], [r1])
    P.cp('dve', vals.ap[:, :, :, 2], a1.ap[:].rearrange("p (e i) -> p e i", i=NT), [a1], [vals], group='vals')
    P.cp('dve', a1.ap[:], r1.ap[:], [r1], [a1])
    P.tt('dve', r2.ap[:], r1.ap[:], a1.ap[:], ALU.subtract, [r1, a1], [r2])
    P.cp('dve', vals.ap[:, :, :, 3], a1.ap[:].rearrange("p (e i) -> p e i", i=NT), [a1], [vals], group='vals')
    P.cp('dve', vals.ap[:, :, :, 4], r2.ap[:].rearrange("p (e i) -> p e i", i=NT), [r2], [vals], group='vals')
    P.cp('dve', vals.ap[:, :, :, 0], cst.ap[:, C_TOK:C_TOK + 32][:, None, :].to_broadcast([128, NE, NT]), [cst], [vals],
         group='vals')
    P.cp('dve', vals.ap[:, :, :, 1], cst.ap[:, C_TOK + 32:C_TOK + 64][:, None, :].to_broadcast([128, NE, NT]), [cst],
         [vals], group='vals')
    sli = P.sbuf("sli", [128, 512], I32)
    shi = P.sbuf("shi", [128, 512], I32)
    P.cp('dve', iob.ap[:], cst.ap[:, C_IOTA:C_IOTA + 128], [cst], [iob])
    P.cp('dve', sli.ap[:], slotm.ap[:], [slotm], [sli])
    P.ts('dve', shi.ap[:], sli.ap[:], 2, None, ALU.arith_shift_right, None, [sli], [shi])
    P.cp('dve', shb.ap[:], shi.ap[:], [shi], [shb])
    P.ts('dve', shi.ap[:], sli.ap[:], 3, None, ALU.bitwise_and, None, [sli, shb], [shi])
    P.cp('dve', slb.ap[:], shi.ap[:], [shi], [slb])

    def compact(e_):
        k_ = e_ % 2
        esl = slice(e_ * NT, (e_ + 1) * NT)
        P.tt('dve', He[k_].ap[:], iob.ap[:, None, :].to_broadcast([128, NT, 128]),
             shb.ap[:, esl, None].to_broadcast([128, NT, 128]), ALU.is_equal, [iob, shb], [He[k_]])
        P.tt('dve', Le[k_].ap[:], iob.ap[:, None, 0:4].to_broadcast([128, NT, 4]),
             slb.ap[:, esl, None].to_broadcast([128, NT, 4]), ALU.is_equal, [iob, slb], [Le[k_]])
        P.tt('dve', LVe[k_].ap[:], Le[k_].ap[:, :, :, None].to_broadcast([128, NT, 4, 5]),
             vals.ap[:, e_, :, None, :].to_broadcast([128, NT, 4, 5]), ALU.mult, [Le[k_], vals], [LVe[k_]])
        for i in range(NT):
            P.mm(cpsb.ap[:, 0:20], He[k_].ap[:, i, :], LVe[k_].ap[:, i].rearrange("p a b -> p (a b)"), i == 0, i == NT - 1,
                 [He[k_], LVe[k_]], [cpsb])
        cs_ = csb[k_]
        P.cp('act', cs_.ap[:], cpsb.ap[:, 0:20], [cpsb], [cs_])
        c3 = cs_.ap[:].rearrange("p (a b) -> p a b", b=5)
        f_ = idf[k_]
        P.stt(f_.ap[:], c3[:, :, 0], 64.0, c3[:, :, 1], ALU.mult, ALU.add, [cs_], [f_])
        P.cp('dve', idx_all.ap[:, e_, :], f_.ap[:], [f_], [idx_v[e_]])
        P.tt('dve', f_.ap[:], c3[:, :, 2], c3[:, :, 3], ALU.add, [cs_], [f_])
        P.tt('dve', gate_all.ap[:, e_, :], c3[:, :, 4], f_.ap[:], ALU.add, [cs_, f_], [gate_v[e_]])

    return dict(compact=compact)


def build_program(debug=False, stop_after=None):
    nc = bass.Bass("TRN2", target_bir_lowering=False)

    def din(name, shape, dt=F32):
        return nc.dram_tensor(name, list(shape), dt, kind="ExternalInput").ap()

    skind = "ExternalOutput" if debug else "Internal"

    def dscr(name, shape, dt):
        return nc.dram_tensor(name, list(shape), dt, kind=skind).ap()

    x_d = din("x", [S, D])
    win_d = din("w_in", [D, 5888])
    g1_d = din("g1", [128, 8])
    gv_d = din("gv", [128, 4])
    g2_d = din("g2rep", [128, D])
    cst_d = din("consts", [128, C_TOTAL])
    cos_d = din("cosT", [128, S])
    sin_d = din("sinT", [128, S])
    namask_d = din("namask", [128, 21, 128])
    nabias_d = din("nabias", [8, 128, 21, 128])
    wa_d = din("w_dil_branch", [256, D])
    wb_d = din("w_na_branch", [512, D])
    wo_d = din("w_out", [D, D])
    wr_d = din("w_router", [D, NE])
    need_moe = stop_after in (None, 'G')
    if need_moe:
        wg_d = din("w_gate", [NE, D, D])
        wu_d = din("w_up", [NE, D, D])
        wd_d = din("w_down", [NE, D, D])
    out_d = nc.dram_tensor("out", [S, D], F32, kind="ExternalOutput").ap()

    qkT_d = dscr("qkT", [2560, S], BF16)
    sgT_d = dscr("sgT", [2048, S], BF16)
    v_d = dscr("vtok", [S, 1300], BF16)
    dout_d = dscr("dout", [3, S, 260], F32)
    h2_d = dscr("h2", [S, D], BF16)

    with ExitStack() as es:
        P = Prog(nc, es)
        qkT_b = P.dram_buf("qkT")
        sgT_b = P.dram_buf("sgT")
        v_b = P.dram_buf("v")
        dout_b = P.dram_buf("dout")
        h2_b = P.dram_buf("h2")
        out_b = P.dram_buf("out")

        cst = P.sbuf("cst", [128, C_TOTAL], F32)
        cstb = P.sbuf("cstb", [128, C_IOTA], BF16)
        g1 = P.sbuf("g1", [128, 8], F32)
        gv = P.sbuf("gv", [128, 4], F32)
        idx_all = P.sbuf("idx_all", [128, NE, 4], I32)
        gate_all = P.sbuf("gate_all", [128, NE, 4], F32)
        lg_all = P.sbuf("lg_all", [128, NT, NE], F32)
        idx_v = P.views("idx_v", idx_all, NE)
        gate_v = P.views("gate_v", gate_all, NE)
        P.dma('sp', cst.ap[:], cst_d, writes=[cst])
        P.dma('sp', g1.ap[:], g1_d, writes=[g1])
        P.dma('sp', gv.ap[:], gv_d, writes=[gv])
        P.cp('dve', cstb.ap[:], cst.ap[:, 0:C_IOTA], [cst], [cstb])
        ident_b = cstb.ap[:, C_IDENT:C_IDENT + 128]
        bones_b = cstb.ap[:, C_BONES:C_BONES + 128]
        rmat_b = cstb.ap[:, C_RMAT:C_RMAT + 128]
        ident_f = cst.ap[:, C_IDENT:C_IDENT + 128]
        ones_f = cst.ap[:, C_ONES:C_ONES + 128]
        triu_f = cst.ap[:, C_TRIU:C_TRIU + 128]

        def ring(name, n, shape, dt, psum=False):
            return [(P.psum if psum else P.sbuf)("%s%d" % (name, i), shape, dt) for i in range(n)]

        def pipeline(items, stages, skews):
            N = len(items)
            for n in range(N + max(skews)):
                for f, sk in zip(stages, skews):
                    i = n - sk
                    if 0 <= i < N:
                        f(items[i])

        with ExitStack() as es2:
            P.es = es2
            hT = P.sbuf("hT", [128, 8, S], BF16)
            hTv = P.views("hTv", hT, NT)
            cosT = P.sbuf("cosT", [128, S], F32)
            sinT = P.sbuf("sinT", [128, S], F32)
            P.dma('act', cosT.ap[:], cos_d, writes=[cosT])
            P.dma('act', sinT.ap[:], sin_d, writes=[sinT])
            wbf = ring("wbf", 4, [128, 8, CW], BF16)
            chunks = []
            for (nm, c0, wd) in W_SEGS:
                for o in range(0, wd, CW):
                    chunks.append((nm, c0 + o, o))

            def load_wchunk(ci):
                if ci >= len(chunks):
                    return
                nm, col0, off = chunks[ci]
                wb_ = wbf[ci % 4]
                P.dma('pool', wb_.ap[:], win_d[:, col0:col0 + CW].rearrange("(c p) n -> p c n", p=128), [], [wb_])

            load_wchunk(0)
            load_wchunk(1)
            load_wchunk(2)
            with ExitStack() as esA:
                P.es = esA
                xt = ring("xt", 4, [128, D], F32)
                junk = P.sbuf("junk", [128, D], BF16)
                ss = ring("ss", 3, [128, 1], F32)
                rs = ring("rs", 3, [128, 1], F32)
                rr = ring("rr", 3, [128, 1], F32)
                xn = ring("xn", 3, [128, D], BF16)
                tp = ring("tp", 2, [128, 8, 128], BF16, psum=True)

                def stA_load(t):
                    P.dma('sp', xt[t % 4].ap[:], x_d[t * 128:(t + 1) * 128, :], writes=[xt[t % 4]])

                def stA_norm(t):
                    k = t % 3
                    x_ = xt[t % 4]
                    P.act(junk.ap[:], x_.ap[:], AF.Square, [x_], [junk, ss[k]], accum_out=ss[k].ap[:])
                    P.act(rs[k].ap[:], ss[k].ap[:], AF.Sqrt, [ss[k]], [rs[k]], scale=1.0 / D, bias=EPS)
                    P.op('dve', lambda e, o=rr[k].ap[:], i=rs[k].ap[:]: e.reciprocal(out=o, in_=i), [rs[k]], [rr[k]])
                    P.ts('dve', xn[k].ap[:], x_.ap[:], rr[k].ap[:, 0:1], None, ALU.mult, None, [x_, rr[k]], [xn[k]])

                def stA_tr(t):
                    k = t % 3
                    for c in range(8):
                        P.tr(tp[t % 2].ap[:, c, :], xn[k].ap[:, c * 128:(c + 1) * 128], ident_b, [xn[k], cstb], [tp[t % 2]])

                def stA_evac(t):
                    P.tt('dve', hT.ap[:, :, t * 128:(t + 1) * 128], tp[t % 2].ap[:],
                         g1.ap[:, :, None].to_broadcast([128, 8, 128]), ALU.mult, [tp[t % 2], g1], [hTv[t]])

                pipeline(list(range(NT)), [stA_load, stA_norm, stA_tr, stA_evac], [0, 1, 2, 3])
            P.es = es2
            P.barrier()

            psA = ring("psA", 3, [128, 512], F32, psum=True)
            msps = ring("msps", 2, [128, 512], F32, psum=True)
            rotps = ring("rotps", 2, [128, 512], F32, psum=True)
            sqb = ring("sqb", 3, [128, 512], BF16)
            lnb = ring("lnb", 2, [128, 512], F32)
            rstd = ring("rstd", 2, [128, 512], F32)
            qnb = ring("qnb", 3, [128, 512], BF16)
            t1 = ring("t1", 2, [128, 512], F32)
            t2 = ring("t2", 2, [128, 512], F32)
            stg = ring("stg", 4, [128, 512], BF16)
            vst = ring("vst", 2, [128, 4, CW // 64, 65], BF16)
            for v_ in vst:
                P.op('pool', lambda e, o=v_.ap[:]: e.memset(o, 1.0), [], [v_])
            itemsB = []
            for ci, (nm, col0, off) in enumerate(chunks):
                if nm in ('va', 'vn'):
                    for t in range(NT):
                        itemsB.append(dict(n=len(itemsB), ci=ci, nm=nm, off=off, kind='v', t=t, first=(t == 0)))
                else:
                    for j in range(CW // 128):
                        for tb in range(8):
                            itemsB.append(dict(n=len(itemsB), ci=ci, nm=nm, off=off,
                                               kind=('g' if nm in ('ga', 'gn') else ('n' if nm in ('qn', 'kn') else 'd')),
                                               j=j, tb=tb, first=(j == 0 and tb == 0)))

            def stB_main(it_):
                n, ci, nm, off = it_['n'], it_['ci'], it_['nm'], it_['off']
                if it_['first']:
                    load_wchunk(ci + 3)
                wb_ = wbf[ci % 4]
                ps = psA[n % 3]
                if it_['kind'] == 'v':
                    t = it_['t']
                    a = t % 4
                    vc = V_COL[nm] + off
                    for c in range(8):
                        P.mm(ps.ap[:, 0:CW], hT.ap[:, c, t * 128:(t + 1) * 128], wb_.ap[:, c, :], c == 0, c == 7,
                             [hTv[t], wb_], [ps])
                    vs = vst[(t // 4) % 2]
                    P.cp('act' if t % 2 == 0 else 'dve', vs.ap[:, a, :, 0:64], ps.ap[:, 0:CW].rearrange("p (h c) -> p h c", c=64),
                         [ps], [vs], group=('vs', ci, t // 4))
                    if a == 3:
                        t0 = t - 3
                        vc65 = (vc // 64) * 65
                        wd65 = (CW // 64) * 65
                        P.dma('sp', v_d[t0 * 128:(t0 + 4) * 128, vc65:vc65 + wd65].rearrange("(a p) c -> p a c", p=128),
                              vs.ap[:].rearrange("p a h c -> p a (h c)"), [vs], [v_b], group='fill')
                    return
                j, tb = it_['j'], it_['tb']
                tsl = slice(tb * 512, (tb + 1) * 512)
                hr = [hTv[4 * tb + a] for a in range(4)]
                for c in range(8):
                    P.mm(ps.ap[:], wb_.ap[:, c, j * 128:(j + 1) * 128], hT.ap[:, c, tsl], c == 0, c == 7, hr + [wb_], [ps])
                if it_['kind'] == 'g':
                    st = stg[n % 4]
                    P.act(st.ap[:], ps.ap[:], AF.Sigmoid, [ps], [st])
                    r0 = SG_ROW[nm] + off + j * 128
                    P.dma('sp', sgT_d[r0:r0 + 128, tsl], st.ap[:], [st], [sgT_b], group='fill')
                else:
                    P.act(sqb[n % 3].ap[:], ps.ap[:], AF.Square, [ps], [sqb[n % 3]])

            def stB_norm(it_):
                if it_['kind'] not in ('n', 'd'):
                    return
                n, nm, off, j, tb = it_['n'], it_['nm'], it_['off'], it_['j'], it_['tb']
                tsl = slice(tb * 512, (tb + 1) * 512)
                ps = psA[n % 3]
                kk = n % 2
                gcol = gv.ap[:, GV_COL[nm]:GV_COL[nm] + 1]
                P.mm(msps[kk].ap[:], bones_b, sqb[n % 3].ap[:], True, True, [sqb[n % 3], cstb], [msps[kk]])
                P.act(lnb[kk].ap[:], msps[kk].ap[:], AF.Ln, [msps[kk]], [lnb[kk]], bias=EPS)
                P.act(rstd[kk].ap[:], lnb[kk].ap[:], AF.Exp, [lnb[kk]], [rstd[kk]], scale=-0.5)
                if it_['kind'] == 'n':
                    st = stg[n % 4]
                    r0 = QK_ROW[nm] + off + j * 128
                    P.stt(st.ap[:], ps.ap[:], gcol, rstd[kk].ap[:], ALU.mult, ALU.mult, [ps, gv, rstd[kk]], [st])
                    P.dma('sp', qkT_d[r0:r0 + 128, tsl], st.ap[:], [st], [qkT_b], group='fill')
                else:
                    P.stt(qnb[n % 3].ap[:], ps.ap[:], gcol, rstd[kk].ap[:], ALU.mult, ALU.mult, [ps, gv, rstd[kk]], [qnb[n % 3]])

            def stB_rot(it_):
                if it_['kind'] != 'd':
                    return
                n, nm, off, j, tb = it_['n'], it_['nm'], it_['off'], it_['j'], it_['tb']
                tsl = slice(tb * 512, (tb + 1) * 512)
                kk = n % 2
                q_ = qnb[n % 3]
                st = stg[n % 4]
                r0 = QK_ROW[nm] + off + j * 128
                P.mm(rotps[kk].ap[:], rmat_b, q_.ap[:], True, True, [q_, cstb], [rotps[kk]])
                P.tt('pool', t1[kk].ap[:], q_.ap[:], cosT.ap[:, tsl], ALU.mult, [q_, cosT], [t1[kk]])
                P.tt('dve', t2[kk].ap[:], rotps[kk].ap[:], sinT.ap[:, tsl], ALU.mult, [rotps[kk], sinT], [t2[kk]])
                P.tt('dve', st.ap[:], t1[kk].ap[:], t2[kk].ap[:], ALU.add, [t1[kk], t2[kk]], [st])
                P.dma('sp', qkT_d[r0:r0 + 128, tsl], st.ap[:], [st], [qkT_b], group='fill')

            pipeline(itemsB, [stB_main, stB_norm, stB_rot], [0, 1, 2])
        P.es = es
        P.barrier()
        if stop_after == 'B':
            P.emit()
            return nc, P

        def dmask(v):
            return cstb.ap[:, C_DMASK + v * 256:C_DMASK + (v + 1) * 256]

        with ExitStack() as es3:
            P.es = es3
            q2n = ring("q2n", 2, [128, S], BF16)
            k2n = ring("k2n", 2, [128, S], BF16)
            qP = ring("qP", 2, [128, S], BF16)
            kP = ring("kP", 2, [128, 6144], BF16)
            Vg = ring("Vg", 2, [128, 48, 4, 65], BF16)
            ost = ring("ost", 2, [128, 32, 2, 65], F32)
            pt = ring("pt", 5, [128, 512], BF16)
            sps = ring("sps", 4, [128, 512], F32, psum=True)
            po = ring("po", 3, [128, 512], F32, psum=True)
            for v_ in Vg:
                P.op('pool', lambda e, o=v_.ap[:]: e.memset(o, 1.0), [], [v_])
            for k_ in kP:
                P.op('pool', lambda e, o=k_.ap[:]: e.memset(o, 0.0), [], [k_])
            GD = [1, 4, 16]

            def dmask2(v):
                return cstb.ap[:, C_DMASK + v * 512:C_DMASK + (v + 1) * 512]

            def load_V(g):
                d = GD[g]
                L = S // d
                nblk = L // 128
                vg = Vg[g % 2]
                cs = slice(g * 260, (g + 1) * 260)
                for r in range(d):
                    v_r = v_d.rearrange("(u r) c -> r u c", r=d)[r]
                    b0 = r * (nblk + 1)
                    P.dma('sp', vg.ap[:, b0 + 1:b0 + nblk].rearrange("p k h c -> p k (h c)"),
                          v_r[64:64 + 128 * (nblk - 1), cs].rearrange("(k p) c -> p k c", p=128),
                          [v_b], [vg], group=('vg', g))
                    P.dma('sp', vg.ap[64:128, b0].rearrange("p h c -> p (h c)"), v_r[0:64, cs], [v_b], [vg], group=('vg', g))
                    P.dma('sp', vg.ap[0:64, b0 + nblk].rearrange("p h c -> p (h c)"), v_r[L - 64:L, cs], [v_b], [vg],
                          group=('vg', g))

            pairs = [(g, hp) for g in range(3) for hp in range(2)]

            def load_pair(pi):
                g, hp = pairs[pi]
                k_ = pi % 2
                row = (4 * g + 2 * hp) * 64
                if g == 0:
                    P.dma('sp', qP[k_].ap[:], qkT_d[row:row + 128, :], [qkT_b], [qP[k_]])
                    P.dma('sp', kP[k_].ap[:, 64:64 + S], qkT_d[768 + row:768 + row + 128, :], [qkT_b], [kP[k_]])
                    return
                P.dma('sp', q2n[k_].ap[:], qkT_d[row:row + 128, :], [qkT_b], [q2n[k_]])
                P.dma('sp', k2n[k_].ap[:], qkT_d[768 + row:768 + row + 128, :], [qkT_b], [k2n[k_]])

            def permute_pair(pi):
                g, hp = pairs[pi]
                if g == 0:
                    return
                d = GD[g]
                L = S // d
                k_ = pi % 2
                if pi >= 2 and GD[pairs[pi - 2][0]] != d:
                    P.op('pool', lambda e, o=kP[k_].ap[:]: e.memset(o, 0.0), [], [kP[k_]])
                P.cp('pool', qP[k_].ap[:].rearrange("p (r u) -> p r u", r=d),
                     q2n[k_].ap[:].rearrange("p (u r) -> p r u", r=d), [q2n[k_]], [qP[k_]])
                P.cp('act', kP[k_].ap[:, 0:d * (L + 128)].rearrange("p (r u) -> p r u", r=d)[:, :, 64:64 + L],
                     k2n[k_].ap[:].rearrange("p (u r) -> p r u", r=d), [k2n[k_]], [kP[k_]])

            def store_pair(pi):
                g, hp = pairs[pi]
                d = GD[g]
                nblk = (S // d) // 128
                os_ = ost[pi % 2]
                for r in range(d):
                    P.dma('sp', dout_d[g].rearrange("(j p r) c -> r p j c", p=128, r=d)[r][:, :, hp * 130:hp * 130 + 130],
                          os_.ap[:, r * nblk:(r + 1) * nblk].rearrange("p j h c -> p j (h c)"),
                          [os_], [dout_b], group='fill')

            items = []
            for pi, (g, hp) in enumerate(pairs):
                d = GD[g]
                L = S // d
                nblk = L // 128
                cnt_pair = d * (nblk // 2) * 2
                ci = 0
                for r in range(d):
                    for jj in range(nblk // 2):
                        for s_ in range(2):
                            items.append(dict(n=len(items), pi=pi, g=g, hp=hp, d=d, L=L, nblk=nblk, r=r, jj=jj, s=s_,
                                              first=(ci == 0), mid=(ci == cnt_pair // 2), last=(ci == cnt_pair - 1)))
                            ci += 1
            load_pair(0)
            load_V(0)
            load_pair(1)
            load_V(1)
            permute_pair(0)

            def stC_qk(it_):
                n = it_['n']
                k_ = it_['pi'] % 2
                if it_['mid'] and it_['pi'] + 1 < len(pairs):
                    permute_pair(it_['pi'] + 1)
                if it_['first'] and it_['pi'] >= 1 and it_['pi'] + 1 < len(pairs):
                    load_pair(it_['pi'] + 1)
                sp_ = sps[n % 4]
                L, r, s_ = it_['L'], it_['r'], it_['s']
                bs = slice(64 * s_, 64 * s_ + 64)
                for a in range(2):
                    j = 2 * it_['jj'] + a
                    kb0 = r * (L + 128) + 128 * j
                    col = a * 256
                    qsl = qP[k_].ap[bs, r * L + 128 * j:r * L + 128 * j + 128]
                    P.mm(sp_.ap[:, col:col + 128], kP[k_].ap[bs, kb0:kb0 + 128], qsl, a == 0, False, [kP[k_], qP[k_]], [sp_])
                    P.mm(sp_.ap[:, col + 128:col + 256], kP[k_].ap[bs, kb0 + 128:kb0 + 256], qsl, False, False,
                         [kP[k_], qP[k_]], [sp_])
                jj, nblk = it_['jj'], it_['nblk']
                f0 = (jj == 0)
                l1 = (2 * jj + 1 == nblk - 1)
                var = 3 if (f0 and l1) else (0 if f0 else (2 if l1 else 1))
                P.mm(sp_.ap[:], ident_b, dmask2(var), False, True, [cstb], [sp_])

            def stC_exp(it_):
                n = it_['n']
                sp_ = sps[n % 4]
                p_ = pt[n % 5]
                P.act(p_.ap[:], sp_.ap[:], AF.Exp, [sp_], [p_], scale=0.125)

            def stC_pv(it_):
                n = it_['n']
                g = it_['g']
                p_ = pt[n % 5]
                o_ = po[n % 3]
                vg = Vg[g % 2]
                os_ = ost[it_['pi'] % 2]
                r, nblk, s_ = it_['r'], it_['nblk'], it_['s']
                hh = 2 * it_['hp'] + s_
                for a in range(2):
                    j = 2 * it_['jj'] + a
                    b0 = r * (nblk + 1) + j
                    oc = a * 65
                    P.mm(o_.ap[:, oc:oc + 65], p_.ap[:, a * 256:a * 256 + 128], vg.ap[:, b0, hh, :], True, False, [p_, vg], [o_])
                    P.mm(o_.ap[:, oc:oc + 65], p_.ap[:, a * 256 + 128:a * 256 + 256], vg.ap[:, b0 + 1, hh, :], False, True,
                         [p_, vg], [o_])
                j0 = r * nblk + 2 * it_['jj']
                P.cp('act' if n % 2 == 0 else 'dve', os_.ap[:, j0:j0 + 2, s_, :],
                     o_.ap[:, 0:130].rearrange("p (h c) -> p h c", c=65), [o_], [os_], group=('ost', it_['pi']))
                if it_['last']:
                    store_pair(it_['pi'])
                    if g == 0 and it_['hp'] == 1:
                        load_V(2)

            pipeline(items, [stC_qk, stC_exp, stC_pv], [0, 0, 2])
        P.es = es
        P.barrier()
        if stop_after == 'C':
            P.emit()
            return nc, P

        esDE = ExitStack()
        es.enter_context(esDE)
        P.es = esDE
        attnB = P.sbuf("attnB", [128, NT, 512], BF16)
        attnBv = P.views("attnBv", attnB, NT)
        PA = P.sbuf("PA", [128, 2, D], BF16)
        PB = P.sbuf("PB", [128, 4, D], BF16)
        WO = P.sbuf("WO", [128, 8, D], BF16)

        def load_branch_weights(after):
            P.dma('pool', PA.ap[:], wa_d.rearrange("(c p) n -> p c n", p=128), after, [PA])
            P.dma('pool', PB.ap[:], wb_d.rearrange("(c p) n -> p c n", p=128), after, [PB])
            for c in range(8):
                P.dma('pool', WO.ap[:, c, :], wo_d[c * 128:(c + 1) * 128, :], after, [WO], group='wo')
        with ExitStack() as es4:
            P.es = es4
            q2 = ring("q2", 2, [128, S], BF16)
            k2 = ring("k2", 2, [128, S], BF16)
            Vn = P.sbuf("Vn", [128, NT, 8, 65], BF16)
            nmf = P.sbuf("nmf", [128, 21, 128], F32)
            nbf = ring("nbf", 1, [128, 21, 128], F32)
            Eh = ring("Eh", 3, [128, 21 * 128], BF16)
            ptn = ring("ptn", 4, [128, 640], BF16)
            rdn = ring("rdn", 4, [128, 1], F32)
            spn = ring("spn", 2, [128, 1024], F32, psum=True)
            pon = ring("pon", 3, [128, 512], F32, psum=True)
            P.dma('sp', nmf.ap[:], namask_d, [], [nmf])
            for q4 in range(4):
                P.dma('act', Vn.ap[:, q4 * 8:(q4 + 1) * 8].rearrange("p k h c -> p k (h c)"),
                      v_d[q4 * 1024:(q4 + 1) * 1024, 12 * 65:20 * 65].rearrange("(k p) c -> p k c", p=128), [v_b], [Vn],
                      group='vn')

            def loadD_pair(hp):
                k_ = hp % 2
                P.dma('sp', q2[k_].ap[:], qkT_d[1536 + hp * 128:1536 + hp * 128 + 128, :], [qkT_b], [q2[k_]])
                P.dma('sp', k2[k_].ap[:], qkT_d[2048 + hp * 128:2048 + hp * 128 + 128, :], [qkT_b], [k2[k_]])

            negm = P.sbuf("negm", [128, 21, 128], F32)
            P.ts('pool', negm.ap[:], nmf.ap[:], -1.0, 30000.0, ALU.add, ALU.mult, [nmf], [negm])

            def make_E(h):
                e_ = 0
                P.dma('sp', nbf[e_].ap[:], nabias_d[h], [], [nbf[e_]])
                P.stt(Eh[h % 3].ap[:].rearrange("p (t q) -> p t q", q=128), nbf[e_].ap[:], 8.0, negm.ap[:], ALU.mult, ALU.add,
                      [nbf[e_], negm], [Eh[h % 3]])

            itemsD = []
            for hp in range(4):
                for s_ in range(2):
                    for b in range(NT):
                        itemsD.append(dict(n=len(itemsD), hp=hp, s=s_, h=2 * hp + s_, b=b))
            loadD_pair(0)
            loadD_pair(1)
            make_E(0)
            make_E(1)

            def stD_qk(it_):
                n, hp, s_, h, b = it_['n'], it_['hp'], it_['s'], it_['h'], it_['b']
                k_ = hp % 2
                if b == 0 and h + 2 < 8:
                    make_E(h + 2)
                if b == 0 and s_ == 0 and 1 <= hp < 3:
                    loadD_pair(hp + 1)
                bs = slice(64 * s_, 64 * s_ + 64)
                kbs, tile0 = na_block_info(b)
                nk = len(kbs)
                sp_ = spn[n % 2]
                qsl = q2[k_].ap[bs, b * 128:(b + 1) * 128]
                for i, kb in enumerate(kbs):
                    P.mm(sp_.ap[:, i * 128:(i + 1) * 128], k2[k_].ap[bs, kb * 128:(kb + 1) * 128], qsl, i == 0 or i == 4, False,
                         [k2[k_], q2[k_]], [sp_])
                n1 = min(nk, 4) * 128
                P.mm(sp_.ap[:, 0:n1], ident_b, Eh[h % 3].ap[:, tile0 * 128:tile0 * 128 + n1], False, True, [Eh[h % 3], cstb], [sp_])
                if nk == 5:
                    P.mm(sp_.ap[:, 512:640], ident_b, Eh[h % 3].ap[:, (tile0 + 4) * 128:(tile0 + 5) * 128], False, True,
                         [Eh[h % 3], cstb], [sp_])

            def stD_exp(it_):
                n, h, b = it_['n'], it_['h'], it_['b']
                kbs, tile0 = na_block_info(b)
                nk = len(kbs)
                sp_ = spn[n % 2]
                p_ = ptn[n % 4]
                n1 = min(nk, 4) * 128
                P.act(p_.ap[:, 0:n1], sp_.ap[:, 0:n1], AF.Exp, [sp_], [p_], scale=0.125)
                if nk == 5:
                    P.act(p_.ap[:, 512:640], sp_.ap[:, 512:640], AF.Exp, [sp_], [p_], scale=0.125)

            def stD_pv(it_):
                n, h, b = it_['n'], it_['h'], it_['b']
                kbs, tile0 = na_block_info(b)
                nk = len(kbs)
                p_ = ptn[n % 4]
                o_ = pon[n % 3]
                ocol = 0
                rd_ = rdn[n % 4]
                for i, kb in enumerate(kbs):
                    P.mm(o_.ap[:, ocol:ocol + 65], p_.ap[:, i * 128:(i + 1) * 128], Vn.ap[:, kb, h, :], i == 0,
                         i == nk - 1, [p_, Vn], [o_])
                P.op('dve', lambda e, o=rd_.ap[:], i_=o_.ap[:, ocol + 64:ocol + 65]: e.reciprocal(out=o, in_=i_),
                     [o_], [rd_])
                P.act(attnB.ap[:, b, h * 64:(h + 1) * 64], o_.ap[:, ocol:ocol + 64], AF.Copy, [o_, rd_], [attnBv[b]],
                      scale=rd_.ap[:, 0:1])
                if n == 24:
                    load_branch_weights([attnBv[b]])

            pipeline(itemsD, [stD_qk, stD_exp, stD_pv], [0, 0, 2])
        P.es = esDE
        P.barrier()
        if debug:
            dbgB_d = nc.dram_tensor("dbg_attnB", [128, NT, 512], BF16, kind="ExternalOutput").ap()
            P.dma('sp', dbgB_d, attnB.ap[:], attnBv, [])
        if stop_after == 'D':
            P.emit()
            return nc, P

        with ExitStack() as es5:
            P.es = es5
            wr = P.sbuf("wr", [128, 8, NE], F32)
            g2r = P.sbuf("g2r", [128, D], F32)
            P.dma('sp', wr.ap[:], wr_d.rearrange("(c p) e -> p c e", p=128), [], [wr])
            P.dma('sp', g2r.ap[:], g2_d, [], [g2r])
            d3 = ring("d3", 3, [128, 3, 260], F32)
            s01 = ring("s01", 3, [128, 260], F32)
            rd4 = ring("rd4", 3, [128, 4], F32)
            yAt = ring("yAt", 3, [128, 4, 64], BF16)
            tpE = ring("tpE", 1, [128, 8, 128], BF16, psum=True)
            yT = ring("yT", 2, [128, 6, 512], BF16)
            sg = ring("sg", 2, [128, 16, 512], BF16)
            psE = ring("psE", 2, [128, 512], F32, psum=True)
            psX = ring("psX", 2, [128, 512], F32, psum=True)
            lgp = P.psum("lgp", [128, 512], F32)
            m1 = ring("m1", 2, [128, 512], F32)
            m2 = ring("m2", 2, [128, 512], F32)
            mT = ring("mT", 2, [128, 8, 512], BF16)
            xt2 = ring("xt2", 2, [128, D], F32)
            x1 = ring("x1", 2, [128, D], F32)
            junk2 = P.sbuf("junk2", [128, D], BF16)
            ss2 = ring("ss2", 3, [128, 1], F32)
            rs2 = ring("rs2", 3, [128, 1], F32)
            rr2 = ring("rr2", 3, [128, 1], F32)
            h2f = ring("h2f", 2, [128, D], F32)
            h2b = ring("h2b", 2, [128, D], BF16)
            tpF = ring("tpF", 2, [128, 4, 128], F32, psum=True)
            h2T = ring("h2T", 2, [128, 8, 128], F32)
            pe_i = [0]

            def nxt():
                p_ = psE[pe_i[0] % 3]
                pe_i[0] += 1
                return p_

            def T0(t):
                k_ = t % 3
                P.dma('sp', d3[k_].ap[:], dout_d[:, t * 128:(t + 1) * 128, :].rearrange("g p c -> p g c"), [dout_b], [d3[k_]])

            def SGL(tb):
                P.dma('act', sg[tb % 2].ap[:], sgT_d[:, tb * 512:(tb + 1) * 512].rearrange("(o p) t -> p o t", p=128),
                      [sgT_b], [sg[tb % 2]])

            def T1(t):
                k_ = t % 3
                P.tt('pool', s01[k_].ap[:], d3[k_].ap[:, 0, :], d3[k_].ap[:, 1, :], ALU.add, [d3[k_]], [s01[k_]])
                P.tt('pool', s01[k_].ap[:], s01[k_].ap[:], d3[k_].ap[:, 2, :], ALU.add, [s01[k_], d3[k_]], [s01[k_]])

            def T2(t):
                k_ = t % 3
                s3 = s01[k_].ap[:].rearrange("p (h c) -> p h c", c=65)
                P.op('dve', lambda e, o=rd4[k_].ap[:], i_=s3[:, :, 64]: e.reciprocal(out=o, in_=i_), [s01[k_]], [rd4[k_]])
                P.tt('dve', yAt[k_].ap[:], s3[:, :, 0:64], rd4[k_].ap[:, :, None].to_broadcast([128, 4, 64]), ALU.mult,
                     [s01[k_], rd4[k_]], [yAt[k_]])

            def T3(t):
                tb, a = t // 4, t % 4
                y_ = yT[tb % 2]
                k_ = t % 3
                yA2 = yAt[k_].ap[:].rearrange("p h c -> p (h c)")
                tp_ = tpE[0]
                for c in range(2):
                    P.tr(tp_.ap[:, c, :], yA2[:, c * 128:(c + 1) * 128], ident_b, [yAt[k_], cstb], [tp_])
                for c in range(4):
                    P.tr(tp_.ap[:, 2 + c, :], attnB.ap[:, t, c * 128:(c + 1) * 128], ident_b, [attnBv[t], cstb], [tp_])
                P.cp('act', y_.ap[:, :, a * 128:(a + 1) * 128], tp_.ap[:, 0:6, :], [tp_], [y_], group=('yT', tb))

            def E2a(tb, j):
                ob, hf = j // 2, j % 2
                y_ = yT[tb % 2]
                bk = psE[j % 2]
                osl = slice(ob * 128, (ob + 1) * 128)
                hsl = slice(hf * 256, (hf + 1) * 256)
                for c in range(2):
                    P.mm(bk.ap[:, 0:256], PA.ap[:, c, osl], y_.ap[:, c, hsl], c == 0, c == 1, [PA, y_], [bk])
                for c in range(4):
                    P.mm(bk.ap[:, 256:512], PB.ap[:, c, osl], y_.ap[:, 2 + c, hsl], c == 0, c == 3, [PB, y_], [bk])

            def E2b(tb, j):
                ob, hf = j // 2, j % 2
                sg_ = sg[tb % 2]
                bk = psE[j % 2]
                hsl = slice(hf * 256, (hf + 1) * 256)
                k_ = j % 2
                P.tt('dve', m1[k_].ap[:, 0:256], bk.ap[:, 0:256], sg_.ap[:, ob, hsl], ALU.mult, [bk, sg_], [m1[k_]])
                P.tt('dve', m2[k_].ap[:, 0:256], bk.ap[:, 256:512], sg_.ap[:, 8 + ob, hsl], ALU.mult, [bk, sg_], [m2[k_]])

            def E2c(tb, j):
                ob, hf = j // 2, j % 2
                m_ = mT[tb % 2]
                k_ = j % 2
                hsl = slice(hf * 256, (hf + 1) * 256)
                P.tt('pool', m_.ap[:, ob, hsl], m1[k_].ap[:, 0:256], m2[k_].ap[:, 0:256], ALU.add, [m1[k_], m2[k_]], [m_],
                     group=('mT', tb))

            def XL(t):
                P.dma('sp', xt2[t % 2].ap[:], x_d[t * 128:(t + 1) * 128, :], [], [xt2[t % 2]])

            def X0(t):
                tb, a = t // 4, t % 4
                m_ = mT[tb % 2]
                for ch in range(2):
                    px = psX[(2 * t + ch) % 2]
                    csl = slice(ch * 512, (ch + 1) * 512)
                    for c in range(8):
                        P.mm(px.ap[:], m_.ap[:, c, a * 128:(a + 1) * 128], WO.ap[:, c, csl], c == 0, c == 7, [m_, WO], [px])

            def X0b(t):
                for ch in range(2):
                    px = psX[(2 * t + ch) % 2]
                    csl = slice(ch * 512, (ch + 1) * 512)
                    P.tt('dve', x1[t % 2].ap[:, csl], px.ap[:], xt2[t % 2].ap[:, csl], ALU.add, [px, xt2[t % 2]], [x1[t % 2]],
                         group=('x1', t))
                P.dma('sp', out_d[t * 128:(t + 1) * 128, :], x1[t % 2].ap[:], [x1[t % 2]], [out_b], group='fill')

            def X0c(t):
                k_ = t % 3
                P.act(junk2.ap[:], x1[t % 2].ap[:], AF.Square, [x1[t % 2]], [junk2, ss2[k_]], accum_out=ss2[k_].ap[:])
                P.act(rs2[k_].ap[:], ss2[k_].ap[:], AF.Sqrt, [ss2[k_]], [rs2[k_]], scale=1.0 / D, bias=EPS)

            def X1(t):
                k_ = t % 3
                P.op('dve', lambda e, o=rr2[k_].ap[:], i_=rs2[k_].ap[:]: e.reciprocal(out=o, in_=i_), [rs2[k_]], [rr2[k_]])
                P.stt(h2f[t % 2].ap[:], x1[t % 2].ap[:], rr2[k_].ap[:, 0:1], g2r.ap[:], ALU.mult, ALU.mult,
                      [x1[t % 2], rr2[k_], g2r], [h2f[t % 2]])

            def X1b(t):
                P.cp('act', h2b[t % 2].ap[:], h2f[t % 2].ap[:], [h2f[t % 2]], [h2b[t % 2]])
                P.dma('sp', h2_d[t * 128:(t + 1) * 128, :], h2b[t % 2].ap[:], [h2b[t % 2]], [h2_b], group='fill')
                for hf in range(2):
                    tf = tpF[hf]
                    for c in range(4):
                        cc_ = hf * 4 + c
                        P.tr(tf.ap[:, c, :], h2f[t % 2].ap[:, cc_ * 128:(cc_ + 1) * 128], ident_f, [h2f[t % 2], cst], [tf])

            def X2(t):
                for hf in range(2):
                    tf = tpF[hf]
                    P.cp('act', h2T[t % 2].ap[:, hf * 4:hf * 4 + 4, :], tf.ap[:], [tf], [h2T[t % 2]], group=('h2T', t))

            def X3(t):
                for c in range(8):
                    P.mm(lgp.ap[:, t * NE:(t + 1) * NE], h2T[t % 2].ap[:, c, :], wr.ap[:, c, :], c == 0, c == 7, [h2T[t % 2], wr], [lgp])

            ev = []
            for t in range(NT):
                tb, a = t // 4, t % 4
                ev += [(4 * t, 0, T0, (t,)), (4 * t + 2, 1, T1, (t,)), (4 * t + 4, 2, T2, (t,)), (4 * t + 6, 3, T3, (t,))]
                x0 = 16 * tb + 40 + 4 * a
                ev += [(x0 - 4, 7, XL, (t,)), (x0, 8, X0, (t,)), (x0 + 1, 9, X0b, (t,)), (x0 + 2, 10, X0c, (t,)),
                       (x0 + 4, 11, X1, (t,)), (x0 + 6, 12, X1b, (t,)), (x0 + 8, 13, X2, (t,)), (x0 + 10, 14, X3, (t,))]
            for tb in range(8):
                ev.append((16 * tb + 6 if tb >= 2 else 0, 0.5, SGL, (tb,)))
                for j in range(16):
                    u = 16 * tb + 20 + j
                    ev += [(u, 4, E2a, (tb, j)), (u + 1, 5, E2b, (tb, j)), (u + 2, 6, E2c, (tb, j))]
            ev.sort(key=lambda x: (x[0], -x[1]))
            for _, _, f_, args_ in ev:
                f_(*args_)
            P.cp('dve', lg_all.ap[:].rearrange("p t e -> p (t e)"), lgp.ap[:], [lgp], [lg_all])
        P.es = es
        esDE.close()
        P.barrier()
        if debug:
            dbgL_d = nc.dram_tensor("dbg_lg", [128, NT, NE], F32, kind="ExternalOutput").ap()
            P.dma('sp', dbgL_d, lg_all.ap[:], [lg_all], [])
        if stop_after == 'E':
            P.emit()
            return nc, P

        with ExitStack() as es7:
            P.es = es7
            NWB = 6
            wbg = ring("wbg", NWB, [128, 8, 512], BF16)
            xg = ring("xg", 12, [128, D], BF16)
            xinT = ring("xinT", 2, [128, 8, 512], BF16)
            sa = ring("sa", 3, [128, 512], F32)
            actT = ring("actT", 2, [128, 8, 512], BF16)
            yst = ring("yst", 4, [128, D], F32)
            tpG = ring("tpG", 2, [128, 8, 128], BF16, psum=True)
            psG = ring("psG", 5, [128, 512], F32, psum=True)
            cpsb = P.psum("cpsb", [128, 512], F32)
            es6 = ExitStack()
            es7.enter_context(es6)
            fdbg = emit_phase_F(P, ring, cst, ones_f, triu_f, ident_f, lg_all, idx_all, gate_all,
                                banks=[psG[0], psG[1], psG[2], cpsb], es_persist=es7, es_tmp=es6,
                                idx_v=idx_v, gate_v=gate_v)
            compact = fdbg['compact']
            P.es = es7
            es6.close()
            if stop_after == 'F':
                for e_ in range(NE):
                    compact(e_)
                dbgI_d = nc.dram_tensor("dbg_idx", [128, NE, 4], I32, kind="ExternalOutput").ap()
                dbgG_d = nc.dram_tensor("dbg_gate", [128, NE, 4], F32, kind="ExternalOutput").ap()
                P.dma('sp', dbgI_d, idx_all.ap[:], idx_v, [])
                P.dma('sp', dbgG_d, gate_all.ap[:], gate_v, [])
                P.emit()
                return nc, P
            pi_ = [0]
            wchunks = []
            for e_ in range(NE):
                for fh in range(2):
                    wchunks.append((e_, 'g', fh))
                    wchunks.append((e_, 'u', fh))
                for ch in range(2):
                    wchunks.append((e_, 'd', ch))
            PF = 4

            def load_w(k):
                if k >= len(wchunks):
                    return
                e_, kind, h_ = wchunks[k]
                src_t = {'g': wg_d, 'u': wu_d, 'd': wd_d}[kind]
                hsl = slice(h_ * 512, (h_ + 1) * 512)
                wb_ = wbg[k % NWB]
                P.dma('pool', wb_.ap[:], src_t[e_].rearrange("(c p) f -> p c f", p=128)[:, :, hsl], [], [wb_])

            def nps():
                p_ = psG[pi_[0] % 5]
                pi_[0] += 1
                return p_

            def prep_gather(e_):
                if e_ >= NE:
                    return
                for sc in range(4):
                    g_ = xg[(e_ * 4 + sc) % 12]
                    P.op('pool', lambda e, o=g_.ap[:], ix=idx_all.ap[:, e_, sc:sc + 1]: e.indirect_dma_start(
                        out=o, out_offset=None, in_=h2_d, in_offset=bass.IndirectOffsetOnAxis(ap=ix, axis=0)),
                        [idx_v[e_], h2_b], [g_], dma=True)

            def prep_tr(e_):
                if e_ >= NE:
                    return
                xT = xinT[e_ % 2]
                for sc in range(4):
                    g_ = xg[(e_ * 4 + sc) % 12]
                    tp_ = tpG[sc % 2]
                    for c in range(8):
                        P.tr(tp_.ap[:, c, :], g_.ap[:, c * 128:(c + 1) * 128], ident_b, [g_, cstb], [tp_])
                    P.cp('act' if sc % 2 == 0 else 'dve', xT.ap[:, :, sc * 128:(sc + 1) * 128], tp_.ap[:], [tp_], [xT],
                         group=('xT', e_))

            for k in range(PF):
                load_w(k)
            compact(0)
            prep_gather(0)
            compact(1)
            prep_gather(1)
            prep_tr(0)
            for k, (e_, kind, h_) in enumerate(wchunks):
                load_w(k + PF)
                xT = xinT[e_ % 2]
                aT = actT[e_ % 2]
                if kind == 'g':
                    if h_ == 0 and e_ + 2 < NE:
                        compact(e_ + 2)
                        prep_gather(e_ + 2)
                    continue
                if kind == 'u':
                    fh = h_
                    wg_ = wbg[(k - 1) % NWB]
                    wu_ = wbg[k % NWB]
                    for fc in range(4):
                        pa = nps()
                        pu = nps()
                        for c in range(8):
                            P.mm(pa.ap[:], wg_.ap[:, c, fc * 128:(fc + 1) * 128], xT.ap[:, c, :], c == 0, c == 7, [wg_, xT], [pa])
                        for c in range(8):
                            P.mm(pu.ap[:], wu_.ap[:, c, fc * 128:(fc + 1) * 128], xT.ap[:, c, :], c == 0, c == 7, [wu_, xT], [pu])
                        s__ = sa[fc % 3]
                        P.act(s__.ap[:], pa.ap[:], AF.Silu, [pa], [s__])
                        P.tt('dve', aT.ap[:, fh * 4 + fc, :], s__.ap[:], pu.ap[:], ALU.mult, [s__, pu], [aT], group=('aT', e_))
                    if fh == 1:
                        prep_tr(e_ + 1)
                    continue
                ch = h_
                csl = slice(ch * 512, (ch + 1) * 512)
                wd_ = wbg[k % NWB]
                for sc in range(4):
                    py = nps()
                    y_ = yst[sc]
                    for fc in range(8):
                        P.mm(py.ap[:], aT.ap[:, fc, sc * 128:(sc + 1) * 128], wd_.ap[:, fc, :], fc == 0, fc == 7, [aT, wd_], [py])
                    P.act(y_.ap[:, csl], py.ap[:], AF.Copy, [py, gate_v[e_]], [y_], group=('y', e_, sc),
                          scale=gate_all.ap[:, e_, sc:sc + 1])
                if ch == 1:
                    for sc in range(4):
                        y_ = yst[sc]
                        P.op('pool', lambda e, i_=y_.ap[:], ix=idx_all.ap[:, e_, sc:sc + 1]: e.indirect_dma_start(
                            out=out_d, out_offset=bass.IndirectOffsetOnAxis(ap=ix, axis=0), in_=i_, in_offset=None,
                            compute_op=ALU.add), [idx_v[e_], y_], [out_b], dma=True, group=('scat', e_))
        P.es = es

        P.emit()
    return nc, P


_CACHE = {}


def kernel(x, norm1_g, w_in, dil_q_norm_g, dil_k_norm_g, na_q_norm_g, na_k_norm_g, na_rpb,
           w_dil_branch, w_na_branch, w_out, norm2_g, w_router, w_gate, w_up, w_down, _debug=False,
           _stop_after=None, _cores=8):
    x = np.asarray(x, np.float32)
    consts, cosT, sinT, namask, dri, dci = host_consts()
    f = lambda a: np.ascontiguousarray(np.asarray(a, np.float32))
    g1 = f(np.asarray(norm1_g)[0].reshape(8, 128).T)
    gv = f(np.stack([np.tile(np.asarray(g)[0], 2) for g in (dil_q_norm_g, dil_k_norm_g, na_q_norm_g, na_k_norm_g)], axis=1))
    g2rep = f(np.broadcast_to(np.asarray(norm2_g)[0][None, :], (128, D)))
    rpb = np.asarray(na_rpb, np.float32)[0]
    nabias = f(rpb[:, dri, dci].transpose(0, 2, 1, 3))
    shared = dict(w_in=f(w_in[0]), g1=g1, gv=gv, g2rep=g2rep, consts=consts, cosT=cosT, sinT=sinT,
                  namask=namask, nabias=nabias, w_dil_branch=f(w_dil_branch[0]), w_na_branch=f(w_na_branch[0]),
                  w_out=f(w_out[0]), w_router=f(w_router[0]), w_gate=f(w_gate[0]), w_up=f(w_up[0]),
                  w_down=f(w_down[0]))
    key = (_debug, _stop_after)
    import time as _time
    _t0 = _time.time()
    if key not in _CACHE:
        _CACHE[key] = build_program(debug=_debug, stop_after=_stop_after)
    nc, P = _CACHE[key]
    if _debug:
        print("build_program s:", _time.time() - _t0, P.stats, flush=True)
    if _stop_after not in (None, 'G'):
        for k_ in ("w_gate", "w_up", "w_down"):
            shared.pop(k_)
    in_maps = []
    for c in range(_cores):
        m = dict(shared)
        m["x"] = np.ascontiguousarray(x[c])
        in_maps.append(m)
    _t0 = _time.time()
    res = run_bass_kernel_spmd(nc, in_maps, core_ids=list(range(_cores)))
    if _debug:
        print("run s:", _time.time() - _t0, flush=True)
        return res.results
    return np.stack([r["out"] for r in res.results], axis=0)
```

```python
import numpy as np
from contextlib import ExitStack
import concourse.bass as bass
import concourse.mybir as mybir
from concourse.bass_utils import run_bass_kernel_spmd

F32 = mybir.dt.float32
BF16 = mybir.dt.bfloat16
I32 = mybir.dt.int32
U32 = mybir.dt.uint32
U16 = mybir.dt.uint16
AF = mybir.ActivationFunctionType
ALU = mybir.AluOpType
AX = mybir.AxisListType

S = 4096
D = 1024
NT = 32
EPS = 1e-6
NE = 16
CAP = 512
N_BISECT = 28

QUEUES = ['pe', 'act', 'dve', 'pool', 'sp']
RING = {'sp': 16, 'act': 8, 'pool': 12}


class Buf:
    def __init__(self, name, ap=None):
        self.name = name
        self.ap = ap
        self.w = []
        self.r = []
        self.pw = []
        self.pr = []
        self.wgroup = None


class Prog:
    def __init__(self, nc, es):
        self.nc = nc
        self.es = es
        self.ops = []
        self.bar = []

    def sbuf(self, name, shape, dtype):
        self.uid = getattr(self, 'uid', 0) + 1
        t = self.es.enter_context(self.nc.sbuf_tensor("sb%d_%s" % (self.uid, name), list(shape), dtype))
        return Buf(name, t)

    def psum(self, name, shape, dtype):
        self.uid = getattr(self, 'uid', 0) + 1
        t = self.es.enter_context(self.nc.psum_tensor("ps%d_%s" % (self.uid, name), list(shape), dtype))
        b = Buf(name, t)
        b.psum = True
        return b

    def dram_buf(self, name):
        return Buf(name, None)

    def views(self, name, buf, n):
        assert not getattr(buf, 'psum', False), "PSUM banks must be tracked as a whole (bank collisions)"
        return [Buf("%s_%d" % (name, i), buf.ap) for i in range(n)]

    def barrier(self):
        last = {}
        deps = set()
        for i, o in enumerate(self.ops):
            if o['dma']:
                deps.add(i)
            else:
                last[o['q']] = i
        deps.update(last.values())
        self.bar = sorted(deps)

    def op(self, q, fn, reads=(), writes=(), dma=False, group=None):
        i = len(self.ops)
        raw = set()
        oth = set()
        excl = [b for b in reads if getattr(b, 'psum', False)]
        if excl:
            reads = [b for b in reads if not getattr(b, 'psum', False)]
            writes = list(writes) + [b for b in excl if b not in writes]
        for b in reads:
            raw.update(b.w)
        for b in writes:
            if group is not None and b.wgroup == group:
                oth.update(b.pw)
                oth.update(b.pr)
                oth.update(b.r)
            else:
                oth.update(b.w)
                oth.update(b.r)
        for b in reads:
            b.r.append(i)
        for b in writes:
            if group is not None and b.wgroup == group:
                b.w.append(i)
            else:
                b.pw = b.w
                b.pr = [x for x in b.r if x != i]
                b.w = [i]
                b.r = []
                b.wgroup = group
        deps = set()
        for d in raw | oth:
            if d == i:
                continue
            o = self.ops[d]
            if o['q'] == q and not o['dma'] and not dma:
                if q != 'pe':
                    deps.add(d)
                continue
            deps.add(d)
        for d in self.bar:
            o = self.ops[d]
            if o['q'] == q and not o['dma'] and not dma:
                continue
            deps.add(d)
        self.ops.append(dict(q=q, fn=fn, deps=deps, dma=dma))
        return i

    def dma(self, q, out, in_, reads=(), writes=(), group=None, **kw):
        return self.op(q, lambda e: e.dma_start(out=out, in_=in_, **kw), reads, writes, dma=True, group=group)

    def mm(self, out, lhsT, rhs, start, stop, reads, writes, group=None):
        return self.op('pe', lambda e: e.matmul(out, lhsT=lhsT, rhs=rhs, start=start, stop=stop), reads, writes,
                       group=group)

    def tr(self, out, in_, ident, reads, writes, group=None):
        return self.op('pe', lambda e: e.transpose(out, in_, ident), reads, writes, group=group)

    def act(self, out, in_, func, reads, writes, group=None, **kw):
        return self.op('act', lambda e: e.activation(out=out, in_=in_, func=func, **kw), reads, writes, group=group)

    def tt(self, q, out, in0, in1, op, reads, writes, group=None):
        return self.op(q, lambda e: e.tensor_tensor(out=out, in0=in0, in1=in1, op=op), reads, writes, group=group)

    def ts(self, q, out, in0, s1, s2, op0, op1, reads, writes, group=None, **kw):
        if op1 is None:
            return self.op(q, lambda e: e.tensor_scalar(out=out, in0=in0, scalar1=s1, scalar2=None, op0=op0, **kw),
                           reads, writes, group=group)
        return self.op(q, lambda e: e.tensor_scalar(out=out, in0=in0, scalar1=s1, scalar2=s2, op0=op0, op1=op1, **kw),
                       reads, writes, group=group)

    def stt(self, out, in0, scalar, in1, op0, op1, reads, writes):
        return self.op('dve', lambda e: e.scalar_tensor_tensor(out=out, in0=in0, scalar=scalar, in1=in1,
                                                                op0=op0, op1=op1), reads, writes)

    def cp(self, q, out, in_, reads, writes, group=None):
        if q == 'act':
            return self.op(q, lambda e: e.copy(out=out, in_=in_), reads, writes, group=group)
        return self.op(q, lambda e: e.tensor_copy(out=out, in_=in_), reads, writes, group=group)

    def emit(self):
        nc = self.nc
        es = self.es
        ops = self.ops
        flagged = [False] * len(ops)
        for o in ops:
            for d in o['deps']:
                flagged[d] = True
        prog_sem = {q: es.enter_context(nc.semaphore("pg_" + q)) for q in ['pe', 'act', 'dve', 'pool']}
        ring_sem = {q: [es.enter_context(nc.semaphore("rg_%s_%d" % (q, k))) for k in range(n)]
                    for q, n in RING.items()}
        cnt = {q: 0 for q in QUEUES}
        dcnt = {q: 0 for q in QUEUES}
        done = [None] * len(ops)
        prev_ring = [None] * len(ops)
        for i, o in enumerate(ops):
            q = o['q']
            if o['dma']:
                k = dcnt[q]
                dcnt[q] += 1
                R = RING[q]
                sem = ring_sem[q][k % R]
                val = 16 * (k // R + 1)
                done[i] = (sem, val)
                if val > 16:
                    prev_ring[i] = (sem, val - 16)
            elif flagged[i]:
                cnt[q] += 1
                done[i] = (prog_sem[q], cnt[q])
        final = {}
        for q, sems in ring_sem.items():
            for k, s in enumerate(sems):
                n = (dcnt[q] - k + RING[q] - 1) // RING[q]
                if n > 0:
                    final[id(s)] = (s, 16 * n)
        self.stats = dict(cnt=cnt, dcnt=dcnt, nops=len(ops))

        def run_queue(q, eng):
            waited = {}
            for i, o in enumerate(ops):
                if o['q'] != q:
                    continue
                need = {}
                for d in o['deps']:
                    s, v = done[d]
                    if need.get(id(s), (None, 0))[1] < v:
                        need[id(s)] = (s, v)
                if prev_ring[i] is not None:
                    s, v = prev_ring[i]
                    if need.get(id(s), (None, 0))[1] < v:
                        need[id(s)] = (s, v)
                for k, (s, v) in need.items():
                    if waited.get(k, 0) >= v:
                        continue
                    eng.wait_ge(s, v)
                    waited[k] = v
                ins = o['fn'](eng)
                if done[i] is not None:
                    ins.then_inc(done[i][0], 16 if o['dma'] else 1)
            if q == 'sp':
                for k, (s, v) in final.items():
                    if waited.get(k, 0) < v:
                        eng.wait_ge(s, v)

        with nc.Block() as block:
            @block.tensor
            def _(e):
                run_queue('pe', e)

            @block.scalar
            def _(e):
                run_queue('act', e)

            @block.vector
            def _(e):
                run_queue('dve', e)

            @block.gpsimd
            def _(e):
                run_queue('pool', e)

            @block.sync
            def _(e):
                run_queue('sp', e)


NA_VARIANTS = [(2, [0, 1, 2, 3, 4]), (0, [0, 1, 2, 3]), (1, [0, 1, 2, 3]), (30, [28, 29, 30, 31]),
               (31, [28, 29, 30, 31])]
NA_TILE0 = [0, 5, 9, 13, 17]
C_IDENT, C_BONES, C_RMAT, C_ONES, C_TRIU = 0, 128, 256, 384, 512
C_DMASK = 640
C_IOTA = C_DMASK + 2048
C_RESET = C_IOTA + 512
C_TOK = C_RESET + 512
C_TOTAL = C_TOK + 64


def na_block_info(b):
    if 2 <= b <= 29:
        return list(range(b - 2, b + 3)), NA_TILE0[0]
    v = {0: 1, 1: 2, 30: 3, 31: 4}[b]
    return NA_VARIANTS[v][1], NA_TILE0[v]


def host_consts():
    c = np.zeros((128, C_TOTAL), np.float32)
    p = np.arange(128)
    c[:, C_IDENT:C_IDENT + 128] = np.eye(128)
    c[:, C_BONES:C_BONES + 128] = ((p[:, None] // 64) == (p[None, :] // 64)) / 64.0
    R = np.zeros((128, 128), np.float32)
    for i in range(128):
        if i % 64 < 32:
            R[i + 32, i] = -1.0
        else:
            R[i - 32, i] = 1.0
    c[:, C_RMAT:C_RMAT + 128] = R
    c[:, C_ONES:C_ONES + 128] = 1.0
    c[:, C_TRIU:C_TRIU + 128] = (p[:, None] < p[None, :])
    pk = p[:, None]
    pq = p[None, :]
    mA = (pk >= pq)
    mB = (pk <= pq)
    first = np.concatenate([mA & (pk >= 64), mB], axis=1)
    mid = np.concatenate([mA, mB], axis=1)
    last = np.concatenate([mA, mB & (pk < 64)], axis=1)
    c[:, C_DMASK:C_DMASK + 2048] = (np.concatenate([first, mid, mid, mid, mid, last, first, last], axis=1) - 1.0) * 30000.0
    c[:, C_IOTA:C_IOTA + 512] = np.arange(512)[None, :]
    rs = np.ones(512, np.float32)
    rs[0::32] = 0.0
    c[:, C_RESET:C_RESET + 512] = rs[None, :]
    tok = np.arange(32)[None, :] * 128 + p[:, None]
    c[:, C_TOK:C_TOK + 32] = tok // 64
    c[:, C_TOK + 32:C_TOK + 64] = tok % 64
    inv = (10000.0 ** (-np.arange(32, dtype=np.float32) / 32)).astype(np.float32)
    pos = np.arange(S, dtype=np.float32)
    ang = (pos[:, None] * inv[None, :]).astype(np.float32)
    cos = np.cos(ang).astype(np.float32).T
    sin = np.sin(ang).astype(np.float32).T
    cosT = np.tile(cos, (4, 1))
    sinT = np.tile(sin, (4, 1))
    mask = np.zeros((21, 128, 128), np.float32)
    dri = np.zeros((21, 128, 128), np.int64)
    dci = np.zeros((21, 128, 128), np.int64)
    t = 0
    for b, kbs in NA_VARIANTS:
        for kb in kbs:
            kr = 2 * kb + p // 64
            kc = p % 64
            r = 2 * b + p // 64
            qc = p % 64
            rs_ = np.clip(r - 4, 0, 56)
            ws = np.clip(qc - 8, 0, 48)
            valid = ((kr[:, None] >= rs_[None, :]) & (kr[:, None] < rs_[None, :] + 8) &
                     (kc[:, None] >= ws[None, :]) & (kc[:, None] < ws[None, :] + 16))
            mask[t] = valid
            dri[t] = np.clip(kr[:, None] - r[None, :] + 7, 0, 14)
            dci[t] = np.clip(kc[:, None] - qc[None, :] + 15, 0, 30)
            t += 1
    namask = np.ascontiguousarray(mask.transpose(1, 0, 2))
    return c, np.ascontiguousarray(cosT), np.ascontiguousarray(sinT), namask, dri, dci


W_SEGS = [('qa', 0, 768), ('ka', 768, 768), ('va', 1536, 768), ('qn', 2304, 512), ('kn', 2816, 512),
          ('vn', 3328, 512), ('ga', 3840, 1024), ('gn', 4864, 1024)]
QK_ROW = {'qa': 0, 'ka': 768, 'qn': 1536, 'kn': 2048}
V_COL = {'va': 0, 'vn': 768}
SG_ROW = {'ga': 0, 'gn': 1024}
GV_COL = {'qa': 0, 'ka': 1, 'qn': 2, 'kn': 3}
CW = 256


def emit_phase_F(P, ring, cst, ones_f, triu_f, ident_f, lg_all, idx_all, gate_all, banks, es_persist, es_tmp,
                 idx_v, gate_v):
    P.es = es_persist
    vals = P.sbuf("vals", [128, NE, NT, 5], BF16)
    shb = P.sbuf("shb", [128, 512], BF16)
    slb = P.sbuf("slb", [128, 512], BF16)
    iob = P.sbuf("iob", [128, 128], BF16)
    He = ring("He", 2, [128, NT, 128], BF16)
    Le = ring("Le", 2, [128, NT, 4], BF16)
    LVe = ring("LVe", 2, [128, NT, 4, 5], BF16)
    idf = ring("idf", 2, [128, 4], F32)
    csb = ring("csb", 2, [128, 20], F32)
    P.es = es_tmp
    totp, ppp, ccp, cpsb = banks
    mx = P.sbuf("mx", [128, NT], F32)
    sh = P.sbuf("sh", [128, NT, NE], F32)
    sm = P.sbuf("sm", [128, NT], F32)
    affT = P.sbuf("affT", [128, NE, NT], F32)
    lo = P.sbuf("lo", [128, NE], F32)
    hi = P.sbuf("hi", [128, NE], F32)
    mid = P.sbuf("mid", [128, NE], F32)
    cmpb = P.sbuf("cmpb", [128, NE, NT], F32)
    cntp = P.sbuf("cntp", [128, NE], F32)
    mge = P.sbuf("mge", [128, NE], U32)
    mlt = P.sbuf("mlt", [128, NE], U32)
    P.op('dve', lambda e: e.tensor_reduce(out=mx.ap[:], in_=lg_all.ap[:], axis=AX.X, op=ALU.max), [lg_all], [mx])
    P.tt('dve', sh.ap[:], lg_all.ap[:], mx.ap[:, :, None].to_broadcast([128, NT, NE]), ALU.subtract, [lg_all, mx], [sh])
    P.act(sh.ap[:], sh.ap[:], AF.Exp, [sh], [sh])
    P.op('dve', lambda e: e.tensor_reduce(out=sm.ap[:], in_=sh.ap[:], axis=AX.X, op=ALU.add), [sh], [sm])
    P.op('dve', lambda e: e.reciprocal(out=sm.ap[:], in_=sm.ap[:]), [sm], [sm])
    P.tt('dve', affT.ap[:].rearrange("p e i -> p i e"), sh.ap[:], sm.ap[:, :, None].to_broadcast([128, NT, NE]),
         ALU.mult, [sh, sm], [affT])
    P.op('dve', lambda e: e.memset(lo.ap[:], 0.0), [], [lo])
    P.op('dve', lambda e: e.memset(hi.ap[:], 1.0), [], [hi])
    for itb in range(N_BISECT):
        P.tt('dve', mid.ap[:], lo.ap[:], hi.ap[:], ALU.add, [lo, hi], [mid])
        P.ts('dve', mid.ap[:], mid.ap[:], 0.5, None, ALU.mult, None, [mid], [mid])
        P.tt('dve', cmpb.ap[:], affT.ap[:], mid.ap[:, :, None].to_broadcast([128, NE, NT]), ALU.is_ge, [affT, mid], [cmpb])
        P.op('dve', lambda e: e.tensor_reduce(out=cntp.ap[:], in_=cmpb.ap[:], axis=AX.X, op=ALU.add), [cmpb], [cntp])
        P.mm(totp.ap[:, 0:NE], ones_f, cntp.ap[:], True, True, [cntp, cst], [totp])
        P.ts('dve', mge.ap[:], totp.ap[:, 0:NE], float(CAP), None, ALU.is_ge, None, [totp], [mge])
        P.ts('dve', mlt.ap[:], totp.ap[:, 0:NE], float(CAP), None, ALU.is_lt, None, [totp], [mlt])
        P.op('dve', lambda e: e.copy_predicated(out=lo.ap[:], mask=mge.ap[:], data=mid.ap[:]), [mge, mid, lo], [lo])
        P.op('dve', lambda e: e.copy_predicated(out=hi.ap[:], mask=mlt.ap[:], data=mid.ap[:]), [mlt, mid, hi], [hi])
    sel = P.sbuf("sel", [128, 512], F32)
    cc = P.sbuf("cc", [128, 512], F32)
    csum = P.sbuf("csum", [128, 512], F32)
    slot = P.sbuf("slot", [128, 512], F32)
    ltb = P.sbuf("ltb", [128, 512], F32)
    slotm = P.sbuf("slotm", [128, 512], F32)
    affF = affT.ap[:].rearrange("p e i -> p (e i)")
    P.tt('dve', sel.ap[:].rearrange("p (e i) -> p e i", i=NT), affT.ap[:],
         lo.ap[:, :, None].to_broadcast([128, NE, NT]), ALU.is_ge, [affT, lo], [sel])
    P.mm(ppp.ap[:], triu_f, sel.ap[:], True, True, [sel, cst], [ppp])
    P.mm(ccp.ap[:], ones_f, sel.ap[:], True, True, [sel, cst], [ccp])
    P.cp('act', cc.ap[:], ccp.ap[:], [ccp], [cc])
    P.op('dve', lambda e: e.tensor_tensor_scan(out=csum.ap[:], data0=cst.ap[:, C_RESET:C_RESET + 512], data1=cc.ap[:],
                                               initial=0.0, op0=ALU.mult, op1=ALU.add), [cc, cst], [csum])
    P.tt('dve', slot.ap[:], csum.ap[:], cc.ap[:], ALU.subtract, [csum, cc], [slot])
    P.tt('dve', slot.ap[:], ppp.ap[:], slot.ap[:], ALU.add, [ppp, slot], [slot])
    P.ts('dve', ltb.ap[:], slot.ap[:], float(CAP), None, ALU.is_lt, None, [slot], [ltb])
    P.tt('dve', ltb.ap[:], ltb.ap[:], sel.ap[:], ALU.mult, [ltb, sel], [ltb])
    P.stt(slotm.ap[:], slot.ap[:], 1.0, ltb.ap[:], ALU.add, ALU.mult, [slot, ltb], [slotm])
    P.ts('dve', slotm.ap[:], slotm.ap[:], -1.0, None, ALU.add, None, [slotm], [slotm])
    a1 = P.sbuf("a1", [128, 512], BF16)
    r1 = P.sbuf("r1", [128, 512], F32)
    r2 = P.sbuf("r2", [128, 512], F32)
    P.cp('dve', a1.ap[:], affF, [affT], [a1])
    P.tt('dve', r1.ap[:], affF, a1.ap[:], ALU.subtract, [affT, a1], [r1])
    P.cp('dve', vals.ap[:, :, :, 2], a1.ap[:].rearrange("p (e i) -> p e i", i=NT), [a1], [vals], group='vals')
    P.cp('dve', a1.ap[:], r1.ap[:], [r1], [a1])
    P.tt('dve', r2.ap[:], r1.ap[:], a1.ap[:], ALU.subtract, [r1, a1], [r2])
    P.cp('dve', vals.ap[:, :, :, 3], a1.ap[:].rearrange("p (e i) -> p e i", i=NT), [a1], [vals], group='vals')
    P.cp('dve', vals.ap[:, :, :, 4], r2.ap[:].rearrange("p (e i) -> p e i", i=NT), [r2], [vals], group='vals')
    P.cp('dve', vals.ap[:, :, :, 0], cst.ap[:, C_TOK:C_TOK + 32][:, None, :].to_broadcast([128, NE, NT]), [cst], [vals],
         group='vals')
    P.cp('dve', vals.ap[:, :, :, 1], cst.ap[:, C_TOK + 32:C_TOK + 64][:, None, :].to_broadcast([128, NE, NT]), [cst],
         [vals], group='vals')
    sli = P.sbuf("sli", [128, 512], I32)
    shi = P.sbuf("shi", [128, 512], I32)
    P.cp('dve', iob.ap[:], cst.ap[:, C_IOTA:C_IOTA + 128], [cst], [iob])
    P.cp('dve', sli.ap[:], slotm.ap[:], [slotm], [sli])
    P.ts('dve', shi.ap[:], sli.ap[:], 2, None, ALU.arith_shift_right, None, [sli], [shi])
    P.cp('dve', shb.ap[:], shi.ap[:], [shi], [shb])
    P.ts('dve', shi.ap[:], sli.ap[:], 3, None, ALU.bitwise_and, None, [sli, shb], [shi])
    P.cp('dve', slb.ap[:], shi.ap[:], [shi], [slb])

    def compact(e_):
        k_ = e_ % 2
        esl = slice(e_ * NT, (e_ + 1) * NT)
        P.tt('dve', He[k_].ap[:], iob.ap[:, None, :].to_broadcast([128, NT, 128]),
             shb.ap[:, esl, None].to_broadcast([128, NT, 128]), ALU.is_equal, [iob, shb], [He[k_]])
        P.tt('dve', Le[k_].ap[:], iob.ap[:, None, 0:4].to_broadcast([128, NT, 4]),
             slb.ap[:, esl, None].to_broadcast([128, NT, 4]), ALU.is_equal, [iob, slb], [Le[k_]])
        P.tt('dve', LVe[k_].ap[:], Le[k_].ap[:, :, :, None].to_broadcast([128, NT, 4, 5]),
             vals.ap[:, e_, :, None, :].to_broadcast([128, NT, 4, 5]), ALU.mult, [Le[k_], vals], [LVe[k_]])
        for i in range(NT):
            P.mm(cpsb.ap[:, 0:20], He[k_].ap[:, i, :], LVe[k_].ap[:, i].rearrange("p a b -> p (a b)"), i == 0, i == NT - 1,
                 [He[k_], LVe[k_]], [cpsb])
        cs_ = csb[k_]
        P.cp('act', cs_.ap[:], cpsb.ap[:, 0:20], [cpsb], [cs_])
        c3 = cs_.ap[:].rearrange("p (a b) -> p a b", b=5)
        f_ = idf[k_]
        P.stt(f_.ap[:], c3[:, :, 0], 64.0, c3[:, :, 1], ALU.mult, ALU.add, [cs_], [f_])
        P.cp('dve', idx_all.ap[:, e_, :], f_.ap[:], [f_], [idx_v[e_]])
        P.tt('dve', f_.ap[:], c3[:, :, 2], c3[:, :, 3], ALU.add, [cs_], [f_])
        P.tt('dve', gate_all.ap[:, e_, :], c3[:, :, 4], f_.ap[:], ALU.add, [cs_, f_], [gate_v[e_]])

    return dict(compact=compact)


def build_program(debug=False, stop_after=None):
    nc = bass.Bass("TRN2", target_bir_lowering=False)

    def din(name, shape, dt=F32):
        return nc.dram_tensor(name, list(shape), dt, kind="ExternalInput").ap()

    skind = "ExternalOutput" if debug else "Internal"

    def dscr(name, shape, dt):
        return nc.dram_tensor(name, list(shape), dt, kind=skind).ap()

    x_d = din("x", [S, D])
    win_d = din("w_in", [D, 5888])
    g1_d = din("g1", [128, 8])
    gv_d = din("gv", [128, 4])
    g2_d = din("g2rep", [128, D])
    cst_d = din("consts", [128, C_TOTAL])
    cos_d = din("cosT", [128, S])
    sin_d = din("sinT", [128, S])
    namask_d = din("namask", [128, 21, 128])
    nabias_d = din("nabias", [8, 128, 21, 128])
    wa_d = din("w_dil_branch", [256, D])
    wb_d = din("w_na_branch", [512, D])
    wo_d = din("w_out", [D, D])
    wr_d = din("w_router", [D, NE])
    need_moe = stop_after in (None, 'G')
    if need_moe:
        wg_d = din("w_gate", [NE, D, D])
        wu_d = din("w_up", [NE, D, D])
        wd_d = din("w_down", [NE, D, D])
    out_d = nc.dram_tensor("out", [S, D], F32, kind="ExternalOutput").ap()

    qkT_d = dscr("qkT", [2560, S], BF16)
    sgT_d = dscr("sgT", [2048, S], BF16)
    v_d = dscr("vtok", [S, 1300], BF16)
    dout_d = dscr("dout", [3, S, 260], F32)
    h2_d = dscr("h2", [S, D], BF16)

    with ExitStack() as es:
        P = Prog(nc, es)
        qkT_b = P.dram_buf("qkT")
        sgT_b = P.dram_buf("sgT")
        v_b = P.dram_buf("v")
        dout_b = P.dram_buf("dout")
        h2_b = P.dram_buf("h2")
        out_b = P.dram_buf("out")

        cst = P.sbuf("cst", [128, C_TOTAL], F32)
        cstb = P.sbuf("cstb", [128, C_IOTA], BF16)
        g1 = P.sbuf("g1", [128, 8], F32)
        gv = P.sbuf("gv", [128, 4], F32)
        idx_all = P.sbuf("idx_all", [128, NE, 4], I32)
        gate_all = P.sbuf("gate_all", [128, NE, 4], F32)
        lg_all = P.sbuf("lg_all", [128, NT, NE], F32)
        idx_v = P.views("idx_v", idx_all, NE)
        gate_v = P.views("gate_v", gate_all, NE)
        P.dma('sp', cst.ap[:], cst_d, writes=[cst])
        P.dma('sp', g1.ap[:], g1_d, writes=[g1])
        P.dma('sp', gv.ap[:], gv_d, writes=[gv])
        P.cp('dve', cstb.ap[:], cst.ap[:, 0:C_IOTA], [cst], [cstb])
        ident_b = cstb.ap[:, C_IDENT:C_IDENT + 128]
        bones_b = cstb.ap[:, C_BONES:C_BONES + 128]
        rmat_b = cstb.ap[:, C_RMAT:C_RMAT + 128]
        ident_f = cst.ap[:, C_IDENT:C_IDENT + 128]
        ones_f = cst.ap[:, C_ONES:C_ONES + 128]
        triu_f = cst.ap[:, C_TRIU:C_TRIU + 128]

        def ring(name, n, shape, dt, psum=False):
            return [(P.psum if psum else P.sbuf)("%s%d" % (name, i), shape, dt) for i in range(n)]

        def pipeline(items, stages, skews):
            N = len(items)
            for n in range(N + max(skews)):
                for f, sk in zip(stages, skews):
                    i = n - sk
                    if 0 <= i < N:
                        f(items[i])

        with ExitStack() as es2:
            P.es = es2
            hT = P.sbuf("hT", [128, 8, S], BF16)
            hTv = P.views("hTv", hT, NT)
            cosT = P.sbuf("cosT", [128, S], F32)
            sinT = P.sbuf("sinT", [128, S], F32)
            P.dma('act', cosT.ap[:], cos_d, writes=[cosT])
            P.dma('act', sinT.ap[:], sin_d, writes=[sinT])
            wbf = ring("wbf", 4, [128, 8, CW], BF16)
            chunks = []
            for (nm, c0, wd) in W_SEGS:
                for o in range(0, wd, CW):
                    chunks.append((nm, c0 + o, o))

            def load_wchunk(ci):
                if ci >= len(chunks):
                    return
                nm, col0, off = chunks[ci]
                wb_ = wbf[ci % 4]
                P.dma('pool', wb_.ap[:], win_d[:, col0:col0 + CW].rearrange("(c p) n -> p c n", p=128), [], [wb_])

            load_wchunk(0)
            load_wchunk(1)
            load_wchunk(2)
            with ExitStack() as esA:
                P.es = esA
                xt = ring("xt", 4, [128, D], F32)
                junk = P.sbuf("junk", [128, D], BF16)
                ss = ring("ss", 3, [128, 1], F32)
                rs = ring("rs", 3, [128, 1], F32)
                rr = ring("rr", 3, [128, 1], F32)
                xn = ring("xn", 3, [128, D], BF16)
                tp = ring("tp", 2, [128, 8, 128], BF16, psum=True)

                def stA_load(t):
                    P.dma('sp', xt[t % 4].ap[:], x_d[t * 128:(t + 1) * 128, :], writes=[xt[t % 4]])

                def stA_norm(t):
                    k = t % 3
                    x_ = xt[t % 4]
                    P.act(junk.ap[:], x_.ap[:], AF.Square, [x_], [junk, ss[k]], accum_out=ss[k].ap[:])
                    P.act(rs[k].ap[:], ss[k].ap[:], AF.Sqrt, [ss[k]], [rs[k]], scale=1.0 / D, bias=EPS)
                    P.op('dve', lambda e, o=rr[k].ap[:], i=rs[k].ap[:]: e.reciprocal(out=o, in_=i), [rs[k]], [rr[k]])
                    P.ts('pool', xn[k].ap[:], x_.ap[:], rr[k].ap[:, 0:1], 1.0, ALU.mult, ALU.mult, [x_, rr[k]], [xn[k]])

                def stA_tr(t):
                    k = t % 3
                    for c in range(8):
                        P.tr(tp[t % 2].ap[:, c, :], xn[k].ap[:, c * 128:(c + 1) * 128], ident_b, [xn[k], cstb], [tp[t % 2]])

                def stA_evac(t):
                    P.tt('dve', hT.ap[:, :, t * 128:(t + 1) * 128], tp[t % 2].ap[:],
                         g1.ap[:, :, None].to_broadcast([128, 8, 128]), ALU.mult, [tp[t % 2], g1], [hTv[t]])

                pipeline(list(range(NT)), [stA_load, stA_norm, stA_tr, stA_evac], [0, 1, 2, 3])
            P.es = es2
            P.barrier()

            psA = ring("psA", 3, [128, 512], F32, psum=True)
            msps = ring("msps", 2, [128, 512], F32, psum=True)
            rotps = ring("rotps", 2, [128, 512], F32, psum=True)
            sqb = ring("sqb", 3, [128, 512], BF16)
            lnb = ring("lnb", 2, [128, 512], F32)
            rstd = ring("rstd", 2, [128, 512], F32)
            qnb = ring("qnb", 3, [128, 512], BF16)
            t1 = ring("t1", 2, [128, 512], F32)
            t2 = ring("t2", 2, [128, 512], F32)
            stg = ring("stg", 4, [128, 512], BF16)
            vst = ring("vst", 2, [128, 4, CW // 64, 65], BF16)
            for v_ in vst:
                P.op('pool', lambda e, o=v_.ap[:]: e.memset(o, 1.0), [], [v_])
            itemsB = []
            for ci, (nm, col0, off) in enumerate(chunks):
                if nm in ('va', 'vn'):
                    for t in range(NT):
                        itemsB.append(dict(n=len(itemsB), ci=ci, nm=nm, off=off, kind='v', t=t, first=(t == 0)))
                else:
                    for j in range(CW // 128):
                        for tb in range(8):
                            itemsB.append(dict(n=len(itemsB), ci=ci, nm=nm, off=off,
                                               kind=('g' if nm in ('ga', 'gn') else ('n' if nm in ('qn', 'kn') else 'd')),
                                               j=j, tb=tb, first=(j == 0 and tb == 0)))

            def stB_main(it_):
                n, ci, nm, off = it_['n'], it_['ci'], it_['nm'], it_['off']
                if it_['first']:
                    load_wchunk(ci + 3)
                wb_ = wbf[ci % 4]
                ps = psA[n % 3]
                if it_['kind'] == 'v':
                    t = it_['t']
                    a = t % 4
                    vc = V_COL[nm] + off
                    for c in range(8):
                        P.mm(ps.ap[:, 0:CW], hT.ap[:, c, t * 128:(t + 1) * 128], wb_.ap[:, c, :], c == 0, c == 7,
                             [hTv[t], wb_], [ps])
                    vs = vst[(t // 4) % 2]
                    P.cp('act' if t % 2 == 0 else 'dve', vs.ap[:, a, :, 0:64], ps.ap[:, 0:CW].rearrange("p (h c) -> p h c", c=64),
                         [ps], [vs], group=('vs', ci, t // 4))
                    if a == 3:
                        t0 = t - 3
                        vc65 = (vc // 64) * 65
                        wd65 = (CW // 64) * 65
                        P.dma('sp', v_d[t0 * 128:(t0 + 4) * 128, vc65:vc65 + wd65].rearrange("(a p) c -> p a c", p=128),
                              vs.ap[:].rearrange("p a h c -> p a (h c)"), [vs], [v_b], group='fill')
                    return
                j, tb = it_['j'], it_['tb']
                tsl = slice(tb * 512, (tb + 1) * 512)
                hr = [hTv[4 * tb + a] for a in range(4)]
                for c in range(8):
                    P.mm(ps.ap[:], wb_.ap[:, c, j * 128:(j + 1) * 128], hT.ap[:, c, tsl], c == 0, c == 7, hr + [wb_], [ps])
                if it_['kind'] == 'g':
                    st = stg[n % 4]
                    P.act(st.ap[:], ps.ap[:], AF.Sigmoid, [ps], [st])
                    r0 = SG_ROW[nm] + off + j * 128
                    P.dma('sp', sgT_d[r0:r0 + 128, tsl], st.ap[:], [st], [sgT_b], group='fill')
                else:
                    P.act(sqb[n % 3].ap[:], ps.ap[:], AF.Square, [ps], [sqb[n % 3]])

            def stB_norm(it_):
                if it_['kind'] not in ('n', 'd'):
                    return
                n, nm, off, j, tb = it_['n'], it_['nm'], it_['off'], it_['j'], it_['tb']
                tsl = slice(tb * 512, (tb + 1) * 512)
                ps = psA[n % 3]
                kk = n % 2
                gcol = gv.ap[:, GV_COL[nm]:GV_COL[nm] + 1]
                P.mm(msps[kk].ap[:], bones_b, sqb[n % 3].ap[:], True, True, [sqb[n % 3], cstb], [msps[kk]])
                P.act(lnb[kk].ap[:], msps[kk].ap[:], AF.Ln, [msps[kk]], [lnb[kk]], bias=EPS)
                P.act(rstd[kk].ap[:], lnb[kk].ap[:], AF.Exp, [lnb[kk]], [rstd[kk]], scale=-0.5)
                if it_['kind'] == 'n':
                    st = stg[n % 4]
                    r0 = QK_ROW[nm] + off + j * 128
                    P.stt(st.ap[:], ps.ap[:], gcol, rstd[kk].ap[:], ALU.mult, ALU.mult, [ps, gv, rstd[kk]], [st])
                    P.dma('sp', qkT_d[r0:r0 + 128, tsl], st.ap[:], [st], [qkT_b], group='fill')
                else:
                    P.stt(qnb[n % 3].ap[:], ps.ap[:], gcol, rstd[kk].ap[:], ALU.mult, ALU.mult, [ps, gv, rstd[kk]], [qnb[n % 3]])

            def stB_rot(it_):
                if it_['kind'] != 'd':
                    return
                n, nm, off, j, tb = it_['n'], it_['nm'], it_['off'], it_['j'], it_['tb']
                tsl = slice(tb * 512, (tb + 1) * 512)
                kk = n % 2
                q_ = qnb[n % 3]
                st = stg[n % 4]
                r0 = QK_ROW[nm] + off + j * 128
                P.mm(rotps[kk].ap[:], rmat_b, q_.ap[:], True, True, [q_, cstb], [rotps[kk]])
                P.tt('pool', t1[kk].ap[:], q_.ap[:], cosT.ap[:, tsl], ALU.mult, [q_, cosT], [t1[kk]])
                P.tt('dve', t2[kk].ap[:], rotps[kk].ap[:], sinT.ap[:, tsl], ALU.mult, [rotps[kk], sinT], [t2[kk]])
                P.tt('dve', st.ap[:], t1[kk].ap[:], t2[kk].ap[:], ALU.add, [t1[kk], t2[kk]], [st])
                P.dma('sp', qkT_d[r0:r0 + 128, tsl], st.ap[:], [st], [qkT_b], group='fill')

            pipeline(itemsB, [stB_main, stB_norm, stB_rot], [0, 1, 2])
        P.es = es
        P.barrier()
        if stop_after == 'B':
            P.emit()
            return nc, P

        def dmask(v):
            return cstb.ap[:, C_DMASK + v * 256:C_DMASK + (v + 1) * 256]

        with ExitStack() as es3:
            P.es = es3
            q2n = ring("q2n", 2, [128, S], BF16)
            k2n = ring("k2n", 2, [128, S], BF16)
            qP = ring("qP", 2, [128, S], BF16)
            kP = ring("kP", 2, [128, 6144], BF16)
            Vg = ring("Vg", 2, [128, 48, 4, 65], BF16)
            ost = ring("ost", 2, [128, 32, 2, 65], F32)
            pt = ring("pt", 5, [128, 512], BF16)
            sps = ring("sps", 4, [128, 512], F32, psum=True)
            po = ring("po", 3, [128, 512], F32, psum=True)
            for v_ in Vg:
                P.op('pool', lambda e, o=v_.ap[:]: e.memset(o, 1.0), [], [v_])
            for k_ in kP:
                P.op('pool', lambda e, o=k_.ap[:]: e.memset(o, 0.0), [], [k_])
            GD = [1, 4, 16]

            def dmask2(v):
                return cstb.ap[:, C_DMASK + v * 512:C_DMASK + (v + 1) * 512]

            def load_V(g):
                d = GD[g]
                L = S // d
                nblk = L // 128
                vg = Vg[g % 2]
                cs = slice(g * 260, (g + 1) * 260)
                for r in range(d):
                    v_r = v_d.rearrange("(u r) c -> r u c", r=d)[r]
                    b0 = r * (nblk + 1)
                    P.dma('sp', vg.ap[:, b0 + 1:b0 + nblk].rearrange("p k h c -> p k (h c)"),
                          v_r[64:64 + 128 * (nblk - 1), cs].rearrange("(k p) c -> p k c", p=128),
                          [v_b], [vg], group=('vg', g))
                    P.dma('sp', vg.ap[64:128, b0].rearrange("p h c -> p (h c)"), v_r[0:64, cs], [v_b], [vg], group=('vg', g))
                    P.dma('sp', vg.ap[0:64, b0 + nblk].rearrange("p h c -> p (h c)"), v_r[L - 64:L, cs], [v_b], [vg],
                          group=('vg', g))

            pairs = [(g, hp) for g in range(3) for hp in range(2)]

            def load_pair(pi):
                g, hp = pairs[pi]
                k_ = pi % 2
                row = (4 * g + 2 * hp) * 64
                if g == 0:
                    P.dma('sp', qP[k_].ap[:], qkT_d[row:row + 128, :], [qkT_b], [qP[k_]])
                    P.dma('sp', kP[k_].ap[:, 64:64 + S], qkT_d[768 + row:768 + row + 128, :], [qkT_b], [kP[k_]])
                    return
                P.dma('sp', q2n[k_].ap[:], qkT_d[row:row + 128, :], [qkT_b], [q2n[k_]])
                P.dma('sp', k2n[k_].ap[:], qkT_d[768 + row:768 + row + 128, :], [qkT_b], [k2n[k_]])

            def permute_pair(pi):
                g, hp = pairs[pi]
                if g == 0:
                    return
                d = GD[g]
                L = S // d
                k_ = pi % 2
                if pi >= 2 and GD[pairs[pi - 2][0]] != d:
                    P.op('pool', lambda e, o=kP[k_].ap[:]: e.memset(o, 0.0), [], [kP[k_]])
                P.cp('pool', qP[k_].ap[:].rearrange("p (r u) -> p r u", r=d),
                     q2n[k_].ap[:].rearrange("p (u r) -> p r u", r=d), [q2n[k_]], [qP[k_]])
                P.cp('act', kP[k_].ap[:, 0:d * (L + 128)].rearrange("p (r u) -> p r u", r=d)[:, :, 64:64 + L],
                     k2n[k_].ap[:].rearrange("p (u r) -> p r u", r=d), [k2n[k_]], [kP[k_]])

            def store_pair(pi):
                g, hp = pairs[pi]
                d = GD[g]
                nblk = (S // d) // 128
                os_ = ost[pi % 2]
                for r in range(d):
                    P.dma('sp', dout_d[g].rearrange("(j p r) c -> r p j c", p=128, r=d)[r][:, :, hp * 130:hp * 130 + 130],
                          os_.ap[:, r * nblk:(r + 1) * nblk].rearrange("p j h c -> p j (h c)"),
                          [os_], [dout_b], group='fill')

            items = []
            for pi, (g, hp) in enumerate(pairs):
                d = GD[g]
                L = S // d
                nblk = L // 128
                cnt_pair = d * (nblk // 2) * 2
                ci = 0
                for r in range(d):
                    for jj in range(nblk // 2):
                        for s_ in range(2):
                            items.append(dict(n=len(items), pi=pi, g=g, hp=hp, d=d, L=L, nblk=nblk, r=r, jj=jj, s=s_,
                                              first=(ci == 0), mid=(ci == cnt_pair // 2), last=(ci == cnt_pair - 1)))
                            ci += 1
            load_pair(0)
            load_V(0)
            load_pair(1)
            load_V(1)
            permute_pair(0)

            def stC_qk(it_):
                n = it_['n']
                k_ = it_['pi'] % 2
                if it_['mid'] and it_['pi'] + 1 < len(pairs):
                    permute_pair(it_['pi'] + 1)
                if it_['first'] and it_['pi'] >= 1 and it_['pi'] + 1 < len(pairs):
                    load_pair(it_['pi'] + 1)
                sp_ = sps[n % 4]
                L, r, s_ = it_['L'], it_['r'], it_['s']
                bs = slice(64 * s_, 64 * s_ + 64)
                for a in range(2):
                    j = 2 * it_['jj'] + a
                    kb0 = r * (L + 128) + 128 * j
                    col = a * 256
                    qsl = qP[k_].ap[bs, r * L + 128 * j:r * L + 128 * j + 128]
                    P.mm(sp_.ap[:, col:col + 128], kP[k_].ap[bs, kb0:kb0 + 128], qsl, a == 0, False, [kP[k_], qP[k_]], [sp_])
                    P.mm(sp_.ap[:, col + 128:col + 256], kP[k_].ap[bs, kb0 + 128:kb0 + 256], qsl, False, False,
                         [kP[k_], qP[k_]], [sp_])
                jj, nblk = it_['jj'], it_['nblk']
                f0 = (jj == 0)
                l1 = (2 * jj + 1 == nblk - 1)
                var = 3 if (f0 and l1) else (0 if f0 else (2 if l1 else 1))
                P.mm(sp_.ap[:], ident_b, dmask2(var), False, True, [cstb], [sp_])

            def stC_exp(it_):
                n = it_['n']
                sp_ = sps[n % 4]
                p_ = pt[n % 5]
                P.act(p_.ap[:], sp_.ap[:], AF.Exp, [sp_], [p_], scale=0.125)

            def stC_pv(it_):
                n = it_['n']
                g = it_['g']
                p_ = pt[n % 5]
                o_ = po[n % 3]
                vg = Vg[g % 2]
                os_ = ost[it_['pi'] % 2]
                r, nblk, s_ = it_['r'], it_['nblk'], it_['s']
                hh = 2 * it_['hp'] + s_
                for a in range(2):
                    j = 2 * it_['jj'] + a
                    b0 = r * (nblk + 1) + j
                    oc = a * 65
                    P.mm(o_.ap[:, oc:oc + 65], p_.ap[:, a * 256:a * 256 + 128], vg.ap[:, b0, hh, :], True, False, [p_, vg], [o_])
                    P.mm(o_.ap[:, oc:oc + 65], p_.ap[:, a * 256 + 128:a * 256 + 256], vg.ap[:, b0 + 1, hh, :], False, True,
                         [p_, vg], [o_])
                j0 = r * nblk + 2 * it_['jj']
                P.cp('act' if n % 2 == 0 else 'dve', os_.ap[:, j0:j0 + 2, s_, :],
                     o_.ap[:, 0:130].rearrange("p (h c) -> p h c", c=65), [o_], [os_], group=('ost', it_['pi']))
                if it_['last']:
                    store_pair(it_['pi'])
                    if g == 0 and it_['hp'] == 1:
                        load_V(2)

            pipeline(items, [stC_qk, stC_exp, stC_pv], [0, 0, 2])
        P.es = es
        P.barrier()
        if stop_after == 'C':
            P.emit()
            return nc, P

        esDE = ExitStack()
        es.enter_context(esDE)
        P.es = esDE
        attnB = P.sbuf("attnB", [128, NT, 512], BF16)
        attnBv = P.views("attnBv", attnB, NT)
        PA = P.sbuf("PA", [128, 2, D], BF16)
        PB = P.sbuf("PB", [128, 4, D], BF16)
        WO = P.sbuf("WO", [128, 8, D], BF16)

        def load_branch_weights(after):
            P.dma('pool', PA.ap[:], wa_d.rearrange("(c p) n -> p c n", p=128), after, [PA])
            P.dma('pool', PB.ap[:], wb_d.rearrange("(c p) n -> p c n", p=128), after, [PB])
            for c in range(8):
                P.dma('pool', WO.ap[:, c, :], wo_d[c * 128:(c + 1) * 128, :], after, [WO], group='wo')
        with ExitStack() as es4:
            P.es = es4
            q2 = ring("q2", 2, [128, S], BF16)
            k2 = ring("k2", 2, [128, S], BF16)
            Vn = P.sbuf("Vn", [128, NT, 8, 65], BF16)
            nmf = P.sbuf("nmf", [128, 21, 128], F32)
            nbf = ring("nbf", 1, [128, 21, 128], F32)
            Eh = ring("Eh", 3, [128, 21 * 128], BF16)
            ptn = ring("ptn", 4, [128, 640], BF16)
            rdn = ring("rdn", 4, [128, 1], F32)
            spn = ring("spn", 2, [128, 1024], F32, psum=True)
            pon = ring("pon", 3, [128, 512], F32, psum=True)
            P.dma('sp', nmf.ap[:], namask_d, [], [nmf])
            for q4 in range(4):
                P.dma('act', Vn.ap[:, q4 * 8:(q4 + 1) * 8].rearrange("p k h c -> p k (h c)"),
                      v_d[q4 * 1024:(q4 + 1) * 1024, 12 * 65:20 * 65].rearrange("(k p) c -> p k c", p=128), [v_b], [Vn],
                      group='vn')

            def loadD_pair(hp):
                k_ = hp % 2
                P.dma('sp', q2[k_].ap[:], qkT_d[1536 + hp * 128:1536 + hp * 128 + 128, :], [qkT_b], [q2[k_]])
                P.dma('sp', k2[k_].ap[:], qkT_d[2048 + hp * 128:2048 + hp * 128 + 128, :], [qkT_b], [k2[k_]])

            negm = P.sbuf("negm", [128, 21, 128], F32)
            P.ts('pool', negm.ap[:], nmf.ap[:], -1.0, 30000.0, ALU.add, ALU.mult, [nmf], [negm])

            def make_E(h):
                e_ = 0
                P.dma('sp', nbf[e_].ap[:], nabias_d[h], [], [nbf[e_]])
                P.stt(Eh[h % 3].ap[:].rearrange("p (t q) -> p t q", q=128), nbf[e_].ap[:], 8.0, negm.ap[:], ALU.mult, ALU.add,
                      [nbf[e_], negm], [Eh[h % 3]])

            itemsD = []
            for hp in range(4):
                for s_ in range(2):
                    for b in range(NT):
                        itemsD.append(dict(n=len(itemsD), hp=hp, s=s_, h=2 * hp + s_, b=b))
            loadD_pair(0)
            loadD_pair(1)
            make_E(0)
            make_E(1)

            def stD_qk(it_):
                n, hp, s_, h, b = it_['n'], it_['hp'], it_['s'], it_['h'], it_['b']
                k_ = hp % 2
                if b == 0 and h + 2 < 8:
                    make_E(h + 2)
                if b == 0 and s_ == 0 and 1 <= hp < 3:
                    loadD_pair(hp + 1)
                bs = slice(64 * s_, 64 * s_ + 64)
                kbs, tile0 = na_block_info(b)
                nk = len(kbs)
                sp_ = spn[n % 2]
                qsl = q2[k_].ap[bs, b * 128:(b + 1) * 128]
                for i, kb in enumerate(kbs):
                    P.mm(sp_.ap[:, i * 128:(i + 1) * 128], k2[k_].ap[bs, kb * 128:(kb + 1) * 128], qsl, i == 0 or i == 4, False,
                         [k2[k_], q2[k_]], [sp_])
                n1 = min(nk, 4) * 128
                P.mm(sp_.ap[:, 0:n1], ident_b, Eh[h % 3].ap[:, tile0 * 128:tile0 * 128 + n1], False, True, [Eh[h % 3], cstb], [sp_])
                if nk == 5:
                    P.mm(sp_.ap[:, 512:640], ident_b, Eh[h % 3].ap[:, (tile0 + 4) * 128:(tile0 + 5) * 128], False, True,
                         [Eh[h % 3], cstb], [sp_])

            def stD_exp(it_):
                n, h, b = it_['n'], it_['h'], it_['b']
                kbs, tile0 = na_block_info(b)
                nk = len(kbs)
                sp_ = spn[n % 2]
                p_ = ptn[n % 4]
                n1 = min(nk, 4) * 128
                P.act(p_.ap[:, 0:n1], sp_.ap[:, 0:n1], AF.Exp, [sp_], [p_], scale=0.125)
                if nk == 5:
                    P.act(p_.ap[:, 512:640], sp_.ap[:, 512:640], AF.Exp, [sp_], [p_], scale=0.125)

            def stD_pv(it_):
                n, h, b = it_['n'], it_['h'], it_['b']
                kbs, tile0 = na_block_info(b)
                nk = len(kbs)
                p_ = ptn[n % 4]
                o_ = pon[n % 3]
                ocol = 0
                rd_ = rdn[n % 4]
                for i, kb in enumerate(kbs):
                    P.mm(o_.ap[:, ocol:ocol + 65], p_.ap[:, i * 128:(i + 1) * 128], Vn.ap[:, kb, h, :], i == 0,
                         i == nk - 1, [p_, Vn], [o_])
                P.op('dve', lambda e, o=rd_.ap[:], i_=o_.ap[:, ocol + 64:ocol + 65]: e.reciprocal(out=o, in_=i_),
                     [o_], [rd_])
                P.act(attnB.ap[:, b, h * 64:(h + 1) * 64], o_.ap[:, ocol:ocol + 64], AF.Copy, [o_, rd_], [attnBv[b]],
                      scale=rd_.ap[:, 0:1])
                if n == 24:
                    load_branch_weights([attnBv[b]])

            pipeline(itemsD, [stD_qk, stD_exp, stD_pv], [0, 0, 2])
        P.es = esDE
        P.barrier()
        if debug:
            dbgB_d = nc.dram_tensor("dbg_attnB", [128, NT, 512], BF16, kind="ExternalOutput").ap()
            P.dma('sp', dbgB_d, attnB.ap[:], attnBv, [])
        if stop_after == 'D':
            P.emit()
            return nc, P

        with ExitStack() as es5:
            P.es = es5
            wr = P.sbuf("wr", [128, 8, NE], F32)
            g2r = P.sbuf("g2r", [128, D], F32)
            P.dma('sp', wr.ap[:], wr_d.rearrange("(c p) e -> p c e", p=128), [], [wr])
            P.dma('sp', g2r.ap[:], g2_d, [], [g2r])
            d3 = ring("d3", 3, [128, 3, 260], F32)
            s01 = ring("s01", 3, [128, 260], F32)
            rd4 = ring("rd4", 3, [128, 4], F32)
            yAt = ring("yAt", 3, [128, 4, 64], BF16)
            tpE = ring("tpE", 1, [128, 8, 128], BF16, psum=True)
            yT = ring("yT", 2, [128, 6, 512], BF16)
            sg = ring("sg", 2, [128, 16, 512], BF16)
            psE = ring("psE", 2, [128, 512], F32, psum=True)
            psX = ring("psX", 2, [128, 512], F32, psum=True)
            lgp = P.psum("lgp", [128, 512], F32)
            m1 = ring("m1", 2, [128, 512], F32)
            m2 = ring("m2", 2, [128, 512], F32)
            mT = ring("mT", 2, [128, 8, 512], BF16)
            xt2 = ring("xt2", 2, [128, D], F32)
            x1 = ring("x1", 2, [128, D], F32)
            junk2 = P.sbuf("junk2", [128, D], BF16)
            ss2 = ring("ss2", 3, [128, 1], F32)
            rs2 = ring("rs2", 3, [128, 1], F32)
            rr2 = ring("rr2", 3, [128, 1], F32)
            h2f = ring("h2f", 2, [128, D], F32)
            h2b = ring("h2b", 2, [128, D], BF16)
            tpF = ring("tpF", 2, [128, 4, 128], F32, psum=True)
            h2T = ring("h2T", 2, [128, 8, 128], F32)
            pe_i = [0]

            def nxt():
                p_ = psE[pe_i[0] % 3]
                pe_i[0] += 1
                return p_

            def T0(t):
                k_ = t % 3
                P.dma('sp', d3[k_].ap[:], dout_d[:, t * 128:(t + 1) * 128, :].rearrange("g p c -> p g c"), [dout_b], [d3[k_]])

            def SGL(tb):
                P.dma('act', sg[tb % 2].ap[:], sgT_d[:, tb * 512:(tb + 1) * 512].rearrange("(o p) t -> p o t", p=128),
                      [sgT_b], [sg[tb % 2]])

            def T1(t):
                k_ = t % 3
                P.tt('pool', s01[k_].ap[:], d3[k_].ap[:, 0, :], d3[k_].ap[:, 1, :], ALU.add, [d3[k_]], [s01[k_]])
                P.tt('pool', s01[k_].ap[:], s01[k_].ap[:], d3[k_].ap[:, 2, :], ALU.add, [s01[k_], d3[k_]], [s01[k_]])

            def T2(t):
                k_ = t % 3
                s3 = s01[k_].ap[:].rearrange("p (h c) -> p h c", c=65)
                P.op('dve', lambda e, o=rd4[k_].ap[:], i_=s3[:, :, 64]: e.reciprocal(out=o, in_=i_), [s01[k_]], [rd4[k_]])
                P.tt('dve', yAt[k_].ap[:], s3[:, :, 0:64], rd4[k_].ap[:, :, None].to_broadcast([128, 4, 64]), ALU.mult,
                     [s01[k_], rd4[k_]], [yAt[k_]])

            def T3(t):
                tb, a = t // 4, t % 4
                y_ = yT[tb % 2]
                k_ = t % 3
                yA2 = yAt[k_].ap[:].rearrange("p h c -> p (h c)")
                tp_ = tpE[0]
                for c in range(2):
                    P.tr(tp_.ap[:, c, :], yA2[:, c * 128:(c + 1) * 128], ident_b, [yAt[k_], cstb], [tp_])
                for c in range(4):
                    P.tr(tp_.ap[:, 2 + c, :], attnB.ap[:, t, c * 128:(c + 1) * 128], ident_b, [attnBv[t], cstb], [tp_])
                P.cp('act', y_.ap[:, :, a * 128:(a + 1) * 128], tp_.ap[:, 0:6, :], [tp_], [y_], group=('yT', tb))

            def E2a(tb, j):
                ob, hf = j // 2, j % 2
                y_ = yT[tb % 2]
                bk = psE[j % 2]
                osl = slice(ob * 128, (ob + 1) * 128)
                hsl = slice(hf * 256, (hf + 1) * 256)
                for c in range(2):
                    P.mm(bk.ap[:, 0:256], PA.ap[:, c, osl], y_.ap[:, c, hsl], c == 0, c == 1, [PA, y_], [bk])
                for c in range(4):
                    P.mm(bk.ap[:, 256:512], PB.ap[:, c, osl], y_.ap[:, 2 + c, hsl], c == 0, c == 3, [PB, y_], [bk])

            def E2b(tb, j):
                ob, hf = j // 2, j % 2
                sg_ = sg[tb % 2]
                bk = psE[j % 2]
                hsl = slice(hf * 256, (hf + 1) * 256)
                k_ = j % 2
                P.tt('dve', m1[k_].ap[:, 0:256], bk.ap[:, 0:256], sg_.ap[:, ob, hsl], ALU.mult, [bk, sg_], [m1[k_]])
                P.tt('dve', m2[k_].ap[:, 0:256], bk.ap[:, 256:512], sg_.ap[:, 8 + ob, hsl], ALU.mult, [bk, sg_], [m2[k_]])

            def E2c(tb, j):
                ob, hf = j // 2, j % 2
                m_ = mT[tb % 2]
                k_ = j % 2
                hsl = slice(hf * 256, (hf + 1) * 256)
                P.tt('pool', m_.ap[:, ob, hsl], m1[k_].ap[:, 0:256], m2[k_].ap[:, 0:256], ALU.add, [m1[k_], m2[k_]], [m_],
                     group=('mT', tb))

            def XL(t):
                P.dma('sp', xt2[t % 2].ap[:], x_d[t * 128:(t + 1) * 128, :], [], [xt2[t % 2]])

            def X0(t):
                tb, a = t // 4, t % 4
                m_ = mT[tb % 2]
                for ch in range(2):
                    px = psX[(2 * t + ch) % 2]
                    csl = slice(ch * 512, (ch + 1) * 512)
                    for c in range(8):
                        P.mm(px.ap[:], m_.ap[:, c, a * 128:(a + 1) * 128], WO.ap[:, c, csl], c == 0, c == 7, [m_, WO], [px])

            def X0b(t):
                for ch in range(2):
                    px = psX[(2 * t + ch) % 2]
                    csl = slice(ch * 512, (ch + 1) * 512)
                    P.tt('dve', x1[t % 2].ap[:, csl], px.ap[:], xt2[t % 2].ap[:, csl], ALU.add, [px, xt2[t % 2]], [x1[t % 2]],
                         group=('x1', t))
                P.dma('sp', out_d[t * 128:(t + 1) * 128, :], x1[t % 2].ap[:], [x1[t % 2]], [out_b], group='fill')

            def X0c(t):
                k_ = t % 3
                P.act(junk2.ap[:], x1[t % 2].ap[:], AF.Square, [x1[t % 2]], [junk2, ss2[k_]], accum_out=ss2[k_].ap[:])
                P.act(rs2[k_].ap[:], ss2[k_].ap[:], AF.Sqrt, [ss2[k_]], [rs2[k_]], scale=1.0 / D, bias=EPS)

            def X1(t):
                k_ = t % 3
                P.op('dve', lambda e, o=rr2[k_].ap[:], i_=rs2[k_].ap[:]: e.reciprocal(out=o, in_=i_), [rs2[k_]], [rr2[k_]])
                P.stt(h2f[t % 2].ap[:], x1[t % 2].ap[:], rr2[k_].ap[:, 0:1], g2r.ap[:], ALU.mult, ALU.mult,
                      [x1[t % 2], rr2[k_], g2r], [h2f[t % 2]])

            def X1b(t):
                P.cp('act', h2b[t % 2].ap[:], h2f[t % 2].ap[:], [h2f[t % 2]], [h2b[t % 2]])
                P.dma('sp', h2_d[t * 128:(t + 1) * 128, :], h2b[t % 2].ap[:], [h2b[t % 2]], [h2_b], group='fill')
                for hf in range(2):
                    tf = tpF[hf]
                    for c in range(4):
                        cc_ = hf * 4 + c
                        P.tr(tf.ap[:, c, :], h2f[t % 2].ap[:, cc_ * 128:(cc_ + 1) * 128], ident_f, [h2f[t % 2], cst], [tf])

            def X2(t):
                for hf in range(2):
                    tf = tpF[hf]
                    P.cp('act', h2T[t % 2].ap[:, hf * 4:hf * 4 + 4, :], tf.ap[:], [tf], [h2T[t % 2]], group=('h2T', t))

            def X3(t):
                for c in range(8):
                    P.mm(lgp.ap[:, t * NE:(t + 1) * NE], h2T[t % 2].ap[:, c, :], wr.ap[:, c, :], c == 0, c == 7, [h2T[t % 2], wr], [lgp])

            ev = []
            for t in range(NT):
                tb, a = t // 4, t % 4
                ev += [(4 * t, 0, T0, (t,)), (4 * t + 2, 1, T1, (t,)), (4 * t + 4, 2, T2, (t,)), (4 * t + 6, 3, T3, (t,))]
                x0 = 16 * tb + 40 + 4 * a
                ev += [(x0 - 4, 7, XL, (t,)), (x0, 8, X0, (t,)), (x0 + 1, 9, X0b, (t,)), (x0 + 2, 10, X0c, (t,)),
                       (x0 + 4, 11, X1, (t,)), (x0 + 6, 12, X1b, (t,)), (x0 + 8, 13, X2, (t,)), (x0 + 10, 14, X3, (t,))]
            for tb in range(8):
                ev.append((16 * tb + 6 if tb >= 2 else 0, 0.5, SGL, (tb,)))
                for j in range(16):
                    u = 16 * tb + 20 + j
                    ev += [(u, 4, E2a, (tb, j)), (u + 1, 5, E2b, (tb, j)), (u + 2, 6, E2c, (tb, j))]
            ev.sort(key=lambda x: (x[0], -x[1]))
            for _, _, f_, args_ in ev:
                f_(*args_)
            P.cp('dve', lg_all.ap[:].rearrange("p t e -> p (t e)"), lgp.ap[:], [lgp], [lg_all])
        P.es = es
        esDE.close()
        P.barrier()
        if debug:
            dbgL_d = nc.dram_tensor("dbg_lg", [128, NT, NE], F32, kind="ExternalOutput").ap()
            P.dma('sp', dbgL_d, lg_all.ap[:], [lg_all], [])
        if stop_after == 'E':
            P.emit()
            return nc, P

        with ExitStack() as es7:
            P.es = es7
            NWB = 6
            wbg = ring("wbg", NWB, [128, 8, 512], BF16)
            xg = ring("xg", 12, [128, D], BF16)
            xinT = ring("xinT", 2, [128, 8, 512], BF16)
            sa = ring("sa", 3, [128, 512], F32)
            actT = ring("actT", 2, [128, 8, 512], BF16)
            yst = ring("yst", 4, [128, D], F32)
            tpG = ring("tpG", 2, [128, 8, 128], BF16, psum=True)
            psG = ring("psG", 5, [128, 512], F32, psum=True)
            cpsb = P.psum("cpsb", [128, 512], F32)
            es6 = ExitStack()
            es7.enter_context(es6)
            fdbg = emit_phase_F(P, ring, cst, ones_f, triu_f, ident_f, lg_all, idx_all, gate_all,
                                banks=[psG[0], psG[1], psG[2], cpsb], es_persist=es7, es_tmp=es6,
                                idx_v=idx_v, gate_v=gate_v)
            compact = fdbg['compact']
            P.es = es7
            es6.close()
            if stop_after == 'F':
                for e_ in range(NE):
                    compact(e_)
                dbgI_d = nc.dram_tensor("dbg_idx", [128, NE, 4], I32, kind="ExternalOutput").ap()
                dbgG_d = nc.dram_tensor("dbg_gate", [128, NE, 4], F32, kind="ExternalOutput").ap()
                P.dma('sp', dbgI_d, idx_all.ap[:], idx_v, [])
                P.dma('sp', dbgG_d, gate_all.ap[:], gate_v, [])
                P.emit()
                return nc, P
            pi_ = [0]
            wchunks = []
            for e_ in range(NE):
                for fh in range(2):
                    wchunks.append((e_, 'g', fh))
                    wchunks.append((e_, 'u', fh))
                for ch in range(2):
                    wchunks.append((e_, 'd', ch))
            PF = 4

            def load_w(k):
                if k >= len(wchunks):
                    return
                e_, kind, h_ = wchunks[k]
                src_t = {'g': wg_d, 'u': wu_d, 'd': wd_d}[kind]
                hsl = slice(h_ * 512, (h_ + 1) * 512)
                wb_ = wbg[k % NWB]
                P.dma('pool', wb_.ap[:], src_t[e_].rearrange("(c p) f -> p c f", p=128)[:, :, hsl], [], [wb_])

            def nps():
                p_ = psG[pi_[0] % 5]
                pi_[0] += 1
                return p_

            def prep_gather(e_):
                if e_ >= NE:
                    return
                for sc in range(4):
                    g_ = xg[(e_ * 4 + sc) % 12]
                    P.op('pool', lambda e, o=g_.ap[:], ix=idx_all.ap[:, e_, sc:sc + 1]: e.indirect_dma_start(
                        out=o, out_offset=None, in_=h2_d, in_offset=bass.IndirectOffsetOnAxis(ap=ix, axis=0)),
                        [idx_v[e_], h2_b], [g_], dma=True)

            def prep_tr(e_):
                if e_ >= NE:
                    return
                xT = xinT[e_ % 2]
                for sc in range(4):
                    g_ = xg[(e_ * 4 + sc) % 12]
                    tp_ = tpG[sc % 2]
                    for c in range(8):
                        P.tr(tp_.ap[:, c, :], g_.ap[:, c * 128:(c + 1) * 128], ident_b, [g_, cstb], [tp_])
                    P.cp('act' if sc % 2 == 0 else 'dve', xT.ap[:, :, sc * 128:(sc + 1) * 128], tp_.ap[:], [tp_], [xT],
                         group=('xT', e_))

            for k in range(PF):
                load_w(k)
            compact(0)
            prep_gather(0)
            compact(1)
            prep_gather(1)
            prep_tr(0)
            for k, (e_, kind, h_) in enumerate(wchunks):
                load_w(k + PF)
                xT = xinT[e_ % 2]
                aT = actT[e_ % 2]
                if kind == 'g':
                    if h_ == 0 and e_ + 2 < NE:
                        compact(e_ + 2)
                        prep_gather(e_ + 2)
                    continue
                if kind == 'u':
                    fh = h_
                    wg_ = wbg[(k - 1) % NWB]
                    wu_ = wbg[k % NWB]
                    for fc in range(4):
                        pa = nps()
                        pu = nps()
                        for c in range(8):
                            P.mm(pa.ap[:], wg_.ap[:, c, fc * 128:(fc + 1) * 128], xT.ap[:, c, :], c == 0, c == 7, [wg_, xT], [pa])
                        for c in range(8):
                            P.mm(pu.ap[:], wu_.ap[:, c, fc * 128:(fc + 1) * 128], xT.ap[:, c, :], c == 0, c == 7, [wu_, xT], [pu])
                        s__ = sa[fc % 3]
                        P.act(s__.ap[:], pa.ap[:], AF.Silu, [pa], [s__])
                        P.tt('dve', aT.ap[:, fh * 4 + fc, :], s__.ap[:], pu.ap[:], ALU.mult, [s__, pu], [aT], group=('aT', e_))
                    if fh == 1:
                        prep_tr(e_ + 1)
                    continue
                ch = h_
                csl = slice(ch * 512, (ch + 1) * 512)
                wd_ = wbg[k % NWB]
                for sc in range(4):
                    py = nps()
                    y_ = yst[sc]
                    for fc in range(8):
                        P.mm(py.ap[:], aT.ap[:, fc, sc * 128:(sc + 1) * 128], wd_.ap[:, fc, :], fc == 0, fc == 7, [aT, wd_], [py])
                    P.act(y_.ap[:, csl], py.ap[:], AF.Copy, [py, gate_v[e_]], [y_], group=('y', e_, sc),
                          scale=gate_all.ap[:, e_, sc:sc + 1])
                if ch == 1:
                    for sc in range(4):
                        y_ = yst[sc]
                        P.op('pool', lambda e, i_=y_.ap[:], ix=idx_all.ap[:, e_, sc:sc + 1]: e.indirect_dma_start(
                            out=out_d, out_offset=bass.IndirectOffsetOnAxis(ap=ix, axis=0), in_=i_, in_offset=None,
                            compute_op=ALU.add), [idx_v[e_], y_], [out_b], dma=True, group=('scat', e_))
        P.es = es

        P.emit()
    return nc, P


_CACHE = {}


def kernel(x, norm1_g, w_in, dil_q_norm_g, dil_k_norm_g, na_q_norm_g, na_k_norm_g, na_rpb,
           w_dil_branch, w_na_branch, w_out, norm2_g, w_router, w_gate, w_up, w_down, _debug=False,
           _stop_after=None, _cores=8):
    x = np.asarray(x, np.float32)
    consts, cosT, sinT, namask, dri, dci = host_consts()
    f = lambda a: np.ascontiguousarray(np.asarray(a, np.float32))
    g1 = f(np.asarray(norm1_g)[0].reshape(8, 128).T)
    gv = f(np.stack([np.tile(np.asarray(g)[0], 2) for g in (dil_q_norm_g, dil_k_norm_g, na_q_norm_g, na_k_norm_g)], axis=1))
    g2rep = f(np.broadcast_to(np.asarray(norm2_g)[0][None, :], (128, D)))
    rpb = np.asarray(na_rpb, np.float32)[0]
    nabias = f(rpb[:, dri, dci].transpose(0, 2, 1, 3))
    shared = dict(w_in=f(w_in[0]), g1=g1, gv=gv, g2rep=g2rep, consts=consts, cosT=cosT, sinT=sinT,
                  namask=namask, nabias=nabias, w_dil_branch=f(w_dil_branch[0]), w_na_branch=f(w_na_branch[0]),
                  w_out=f(w_out[0]), w_router=f(w_router[0]), w_gate=f(w_gate[0]), w_up=f(w_up[0]),
                  w_down=f(w_down[0]))
    key = (_debug, _stop_after)
    import time as _time
    _t0 = _time.time()
    if key not in _CACHE:
        _CACHE[key] = build_program(debug=_debug, stop_after=_stop_after)
    nc, P = _CACHE[key]
    if _debug:
        print("build_program s:", _time.time() - _t0, P.stats, flush=True)
    if _stop_after not in (None, 'G'):
        for k_ in ("w_gate", "w_up", "w_down"):
            shared.pop(k_)
    in_maps = []
    for c in range(_cores):
        m = dict(shared)
        m["x"] = np.ascontiguousarray(x[c])
        in_maps.append(m)
    _t0 = _time.time()
    res = run_bass_kernel_spmd(nc, in_maps, core_ids=list(range(_cores)))
    if _debug:
        print("run s:", _time.time() - _t0, flush=True)
        return res.results
    return np.stack([r["out"] for r in res.results], axis=0)
```
